# Optimizing a Trainium2 kernel written in Bass

```python
import jax, jax.numpy as jnp
from jax import lax
import numpy as np

D_MODEL = 1024
BATCH = 32
SEQ = 2048
DEPTH = 2

CHUNK = 64
HG_HEADS = 4
HG_DK = 128
HG_DV = 128
HG_WIDTH = HG_HEADS * HG_DK
ML_HEADS = 4
ML_DV = 128
ML_DQK = 64
ML_WIDTH = ML_HEADS * ML_DV
CONV_K = 4
N_BRANCH = 2
IN_SIZES = (HG_WIDTH, HG_WIDTH, HG_WIDTH, HG_WIDTH, ML_WIDTH, ML_WIDTH, ML_WIDTH, ML_HEADS, ML_HEADS, D_MODEL, D_MODEL)
IN_COLS = 4 * HG_WIDTH + 3 * ML_WIDTH + 2 * ML_HEADS + N_BRANCH * D_MODEL
D_FF = 2816
N_EXPERTS = 8
TOP_K = 2
D_FF_EXPERT = 1408
N_DENSE = (DEPTH + 1) // 2
N_MOE = DEPTH // 2
EPS = 1e-6

kernel_name = "hgrn2_mlstm_gated_hybrid_moe"


def rms_norm(x, g):
    xf = x.astype(jnp.float32)
    y = xf * lax.rsqrt(jnp.mean(xf * xf, axis=-1, keepdims=True) + EPS)
    return (y * g.astype(jnp.float32)).astype(x.dtype)


def head_rms_norm(o, g):
    B, S, H, d = o.shape
    y = o * lax.rsqrt(jnp.mean(o * o, axis=-1, keepdims=True) + EPS)
    return y.reshape(B, S, H * d) * g.astype(jnp.float32)


def ada_params(c_act, w, b):
    mod = c_act @ w + b
    shift, scale, gate = jnp.split(mod, 3, axis=-1)
    return shift[:, None, :], scale[:, None, :], gate[:, None, :]


def modulate(h, shift, scale):
    return h * (1 + scale) + shift


def to_chunks(t):
    B, S, H, d = t.shape
    return t.reshape(B, S // CHUNK, CHUNK, H, d).transpose(1, 0, 3, 2, 4)


def from_chunks(t):
    nc, B, H, C, d = t.shape
    return t.transpose(1, 0, 3, 2, 4).reshape(B, nc * C, H, d)


def hgrn2_chunkwise(q, k, v, log_f):
    B, S, H, DK = q.shape
    DV = v.shape[-1]
    qc, kc, vc, lfc = (to_chunks(t.astype(jnp.float32)) for t in (q, k, v, log_f))
    causal = jnp.tril(jnp.ones((CHUNK, CHUNK), dtype=bool))

    def step(state, inp):
        qb, kb, vb, lfb = inp
        A = jnp.cumsum(lfb, axis=2)
        diff = A[:, :, :, None, :] - A[:, :, None, :, :]
        decay = jnp.exp(jnp.where(causal[:, :, None], diff, -jnp.inf))
        scores = jnp.einsum("bhtk,bhsk,bhtsk->bhts", qb, kb, decay)
        o_intra = jnp.einsum("bhts,bhsv->bhtv", scores, vb)
        o_inter = jnp.einsum("bhtk,bhkv->bhtv", qb * jnp.exp(A), state)
        A_last = A[:, :, -1:, :]
        k_dec = kb * jnp.exp(A_last - A)
        new_state = jnp.exp(A_last[:, :, 0, :])[..., None] * state + jnp.einsum("bhsk,bhsv->bhkv", k_dec, vb)
        return new_state, o_intra + o_inter

    s0 = jnp.zeros((B, H, DK, DV), jnp.float32)
    _, o = lax.scan(step, s0, (qc, kc, vc, lfc))
    return from_chunks(o)


def mlstm_chunkwise(q, k, v, i_pre, log_f):
    B, S, H, DQK = q.shape
    DV = v.shape[-1]
    qc, kc, vc = (to_chunks(t.astype(jnp.float32)) for t in (q, k, v))
    ic, fc = (to_chunks(t.astype(jnp.float32)[..., None])[..., 0] for t in (i_pre, log_f))
    causal = jnp.tril(jnp.ones((CHUNK, CHUNK), dtype=bool))

    def step(carry, inp):
        C_prev, n_prev, m_prev = carry
        qb, kb, vb, ib, lfb = inp
        b = jnp.cumsum(lfb, axis=-1)
        d_log = jnp.where(causal, b[..., :, None] - b[..., None, :] + ib[..., None, :], -jnp.inf)
        inter_log = b + m_prev[..., None]
        m_t = jnp.maximum(jnp.max(d_log, axis=-1), inter_log)
        w = jnp.exp(d_log - m_t[..., None])
        w_inter = jnp.exp(inter_log - m_t)
        qk = jnp.einsum("bhtd,bhsd->bhts", qb, kb) * w
        num = jnp.einsum("bhts,bhsv->bhtv", qk, vb) + w_inter[..., None] * jnp.einsum("bhtd,bhdv->bhtv", qb, C_prev)
        den = jnp.sum(qk, axis=-1) + w_inter * jnp.einsum("bhtd,bhd->bht", qb, n_prev)
        h = num / jnp.maximum(jnp.abs(den), jnp.exp(-m_t))[..., None]
        b_last = b[..., -1]
        state_log = b_last[..., None] - b + ib
        m_new = jnp.maximum(b_last + m_prev, jnp.max(state_log, axis=-1))
        w_s = jnp.exp(state_log - m_new[..., None])
        decay_prev = jnp.exp(b_last + m_prev - m_new)
        C_new = decay_prev[..., None, None] * C_prev + jnp.einsum("bhs,bhsd,bhsv->bhdv", w_s, kb, vb)
        n_new = decay_prev[..., None] * n_prev + jnp.einsum("bhs,bhsd->bhd", w_s, kb)
        return (C_new, n_new, m_new), h

    carry0 = (jnp.zeros((B, H, DQK, DV), jnp.float32), jnp.zeros((B, H, DQK), jnp.float32), jnp.zeros((B, H), jnp.float32))
    _, h = lax.scan(step, carry0, (qc, kc, vc, ic, fc))
    return from_chunks(h)


def causal_dwconv(u, w, b):
    S = u.shape[1]
    up = jnp.pad(u, ((0, 0), (CONV_K - 1, 0), (0, 0)))
    y = b + w[0] * up[:, 0:S]
    for j in range(1, CONV_K):
        y = y + w[j] * up[:, j:j + S]
    return y


def hybrid_mixer(h, w_in, lb, hg_norm_g, conv_w, conv_b, wq, wk, fbias, ml_norm_g, w_br_hg, w_br_ml, w_out):
    B, S, _ = h.shape
    proj = h @ w_in
    offs = []
    acc = 0
    for sz in IN_SIZES[:-1]:
        acc += sz
        offs.append(acc)
    hq, hf, hi, hg, mu, mv, mo, mi, mf, ga, gb = jnp.split(proj, offs, axis=-1)

    zf = hf.astype(jnp.float32)
    log_f = jnp.logaddexp(jnp.log(lb), jnp.log1p(-lb) + jax.nn.log_sigmoid(zf))
    k_hg = (1 - lb) * jax.nn.sigmoid(-zf)
    q_hg = jax.nn.silu(hq.astype(jnp.float32))
    hs = lambda t, d: t.reshape(B, S, HG_HEADS, d)
    o_hg = hgrn2_chunkwise(hs(q_hg, HG_DK), hs(k_hg, HG_DK), hs(hi, HG_DV), hs(log_f, HG_DK))
    y_hg = head_rms_norm(o_hg, hg_norm_g) * jax.nn.silu(hg.astype(jnp.float32))

    uc = jax.nn.silu(causal_dwconv(mu, conv_w, conv_b)).reshape(B, S, ML_HEADS, ML_DV)
    q_ml = jnp.einsum("bshd,hde->bshe", uc, wq) * (ML_DQK ** -0.5)
    k_ml = jnp.einsum("bshd,hde->bshe", uc, wk)
    v_ml = mv.reshape(B, S, ML_HEADS, ML_DV)
    log_f_ml = jax.nn.log_sigmoid(mf.astype(jnp.float32) + fbias)
    o_ml = mlstm_chunkwise(q_ml, k_ml, v_ml, mi, log_f_ml)
    y_ml = jax.nn.sigmoid(mo.astype(jnp.float32)) * head_rms_norm(o_ml, ml_norm_g)

    y_hg = y_hg.astype(h.dtype) @ w_br_hg
    y_ml = y_ml.astype(h.dtype) @ w_br_ml
    merged = jax.nn.sigmoid(ga) * y_hg + jax.nn.sigmoid(gb) * y_ml
    return merged @ w_out


def swiglu(h, w1, w3, w2):
    return (jax.nn.silu(h @ w1) * (h @ w3)) @ w2


def moe_swiglu(h, router_w, router_b, w1, w3, w2):
    B, S, D = h.shape
    t = h.reshape(B * S, D)
    logits = (t @ router_w).astype(jnp.float32) + router_b.astype(jnp.float32)
    top_v, top_i = lax.top_k(logits, TOP_K)
    gates = jax.nn.softmax(top_v, axis=-1)
    combine = jnp.sum(jax.nn.one_hot(top_i, N_EXPERTS, dtype=jnp.float32) * gates[..., None], axis=1)
    combine = combine.astype(h.dtype)
    out = combine[:, 0:1] * swiglu(t, w1[0], w3[0], w2[0])
    for e in range(1, N_EXPERTS):
        out = out + combine[:, e:e + 1] * swiglu(t, w1[e], w3[e], w2[e])
    return out.reshape(B, S, D)


def setup_inputs(seed: int = 0) -> dict:
    key = jax.random.key(seed)
    ks = jax.random.split(key, 32)
    D = D_MODEL

    def nrm(k, shape, s):
        return jax.random.normal(k, shape, jnp.float32) * s

    return {
        "x": nrm(ks[0], (BATCH, SEQ, D), 1.0),
        "c": nrm(ks[1], (BATCH, D), 1.0),
        "norm_mix_g": 1.0 + nrm(ks[2], (DEPTH, D), 0.1),
        "ada_mix_w": nrm(ks[3], (DEPTH, D, 3 * D), 0.5 * D ** -0.5),
        "ada_mix_b": nrm(ks[4], (DEPTH, 3 * D), 0.02),
        "w_in": nrm(ks[5], (DEPTH, D, IN_COLS), D ** -0.5),
        "hg_lb_logits": nrm(ks[6], (DEPTH, HG_WIDTH), 1.0),
        "hg_norm_g": 1.0 + nrm(ks[7], (DEPTH, HG_WIDTH), 0.1),
        "ml_conv_w": nrm(ks[8], (DEPTH, CONV_K, ML_WIDTH), CONV_K ** -0.5),
        "ml_conv_b": nrm(ks[9], (DEPTH, ML_WIDTH), 0.02),
        "ml_wq": nrm(ks[10], (DEPTH, ML_HEADS, ML_DV, ML_DQK), ML_DV ** -0.5),
        "ml_wk": nrm(ks[11], (DEPTH, ML_HEADS, ML_DV, ML_DQK), ML_DV ** -0.5),
        "ml_fbias": 3.0 + 3.0 * jax.random.uniform(ks[12], (DEPTH, ML_HEADS), jnp.float32),
        "ml_norm_g": 1.0 + nrm(ks[13], (DEPTH, ML_WIDTH), 0.1),
        "w_br_hg": nrm(ks[14], (DEPTH, HG_WIDTH, D), HG_WIDTH ** -0.5),
        "w_br_ml": nrm(ks[15], (DEPTH, ML_WIDTH, D), ML_WIDTH ** -0.5),
        "w_out": nrm(ks[16], (DEPTH, D, D), D ** -0.5),
        "norm_ffn_g": 1.0 + nrm(ks[17], (DEPTH, D), 0.1),
        "ada_ffn_w": nrm(ks[18], (DEPTH, D, 3 * D), 0.5 * D ** -0.5),
        "ada_ffn_b": nrm(ks[19], (DEPTH, 3 * D), 0.02),
        "ffn_w1": nrm(ks[20], (N_DENSE, D, D_FF), D ** -0.5),
        "ffn_w3": nrm(ks[21], (N_DENSE, D, D_FF), D ** -0.5),
        "ffn_w2": nrm(ks[22], (N_DENSE, D_FF, D), D_FF ** -0.5),
        "moe_router_w": nrm(ks[23], (N_MOE, D, N_EXPERTS), D ** -0.5),
        "moe_router_b": nrm(ks[24], (N_MOE, N_EXPERTS), 0.01),
        "moe_w1": nrm(ks[25], (N_MOE, N_EXPERTS, D, D_FF_EXPERT), D ** -0.5),
        "moe_w3": nrm(ks[26], (N_MOE, N_EXPERTS, D, D_FF_EXPERT), D ** -0.5),
        "moe_w2": nrm(ks[27], (N_MOE, N_EXPERTS, D_FF_EXPERT, D), D_FF_EXPERT ** -0.5),
        "final_norm_g": 1.0 + nrm(ks[28], (D,), 0.1),
    }


def reference(x, c, norm_mix_g, ada_mix_w, ada_mix_b, w_in, hg_lb_logits, hg_norm_g,
              ml_conv_w, ml_conv_b, ml_wq, ml_wk, ml_fbias, ml_norm_g, w_br_hg, w_br_ml, w_out,
              norm_ffn_g, ada_ffn_w, ada_ffn_b, ffn_w1, ffn_w3, ffn_w2,
              moe_router_w, moe_router_b, moe_w1, moe_w3, moe_w2, final_norm_g):
    c_act = jax.nn.silu(c)
    lb_all = jnp.cumsum(jax.nn.softmax(hg_lb_logits.astype(jnp.float32), axis=0), axis=0)
    lb_all = lb_all - lb_all[0:1]
    for l in range(DEPTH):
        shift, scale, gate = ada_params(c_act, ada_mix_w[l], ada_mix_b[l])
        h = modulate(rms_norm(x, norm_mix_g[l]), shift, scale)
        mix = hybrid_mixer(h, w_in[l], lb_all[l], hg_norm_g[l], ml_conv_w[l], ml_conv_b[l],
                           ml_wq[l], ml_wk[l], ml_fbias[l], ml_norm_g[l], w_br_hg[l], w_br_ml[l], w_out[l])
        x = x + gate * mix
        shift, scale, gate = ada_params(c_act, ada_ffn_w[l], ada_ffn_b[l])
        h = modulate(rms_norm(x, norm_ffn_g[l]), shift, scale)
        j = l // 2
        if l % 2 == 0:
            ff = swiglu(h, ffn_w1[j], ffn_w3[j], ffn_w2[j])
        else:
            ff = moe_swiglu(h, moe_router_w[j], moe_router_b[j], moe_w1[j], moe_w3[j], moe_w2[j])
        x = x + gate * ff
    return rms_norm(x, final_norm_g)
```

```python
import contextlib
import numpy as np
import concourse.bass as bass
import concourse.mybir as mybir
from concourse.bass_utils import run_bass_kernel_spmd

F32 = mybir.dt.float32
BF16 = mybir.dt.bfloat16
AF = mybir.ActivationFunctionType
ALU = mybir.AluOpType
AX = mybir.AxisListType

D = 1024
DEPTH = 2
INC = 5640
C_Q, C_F, C_I, C_G, C_MU, C_MV, C_MO, C_MI, C_MF, C_GA, C_GB = 0, 512, 1024, 1536, 2048, 2560, 3072, 3584, 3588, 3592, 4616
DFF = 2816
NE = 8
DFE = 1408
EPS = 1e-6
N_CORES = 8
PE_DRAIN = False
SPARSE_MOE = True

PV_NMG, PV_AMB, PV_NFG, PV_AFB, PV_LB, PV_HNG, PV_CW, PV_CB, PV_MNG, PV_FB, PV_RB = 0, 8, 32, 40, 64, 68, 72, 88, 92, 96, 100
PVL = 108


class Buf:
    __slots__ = ("name", "w", "r")

    def __init__(self, name):
        self.name = name
        self.w = []
        self.r = []


class _Rec:
    def __init__(self):
        self.call = None

    def __getattr__(self, name):
        def f(*a, **k):
            self.call = (name, a, k)
            return self
        return f


def _record(fn):
    r = _Rec()
    fn(r)
    return r.call


class Prog:
    ENGS = ("pe", "act", "dve", "pool", "sp")

    def __init__(self, nc, n_dma_sems=8):
        self.nc = nc
        self.q = {e: [] for e in self.ENGS}
        self.cnt = {e: 0 for e in self.ENGS}
        self.waited = {e: {} for e in self.ENGS}
        self.n_dma_sems = n_dma_sems
        self.dma_rr = {"sp": 0, "pool": 0, "act": 0}
        self.dma_cnt = {}
        self.n_ins = 0

    def _need(self, eng, tok, waits):
        if tok is None:
            return
        key, val = tok
        if key == ("e", eng) and eng == "pe":
            return
        if self.waited[eng].get(key, 0) >= val:
            return
        self.waited[eng][key] = val
        waits.append((key, val))

    def _deps(self, eng, reads, writes):
        waits = []
        for b in reads:
            for t in b.w:
                self._need(eng, t, waits)
        for b in writes:
            for t in b.w:
                self._need(eng, t, waits)
            for t in b.r:
                self._need(eng, t, waits)
        return waits

    def _commit(self, tok, reads, writes):
        for b in reads:
            b.r.append(tok)
        for b in writes:
            b.w = [tok]
            b.r = []

    def op(self, eng, fn, reads=(), writes=()):
        waits = self._deps(eng, reads, writes)
        self.cnt[eng] += 1
        tok = (("e", eng), self.cnt[eng])
        self.q[eng].append((waits, _record(fn), ("e", eng), 1))
        self._commit(tok, reads, writes)
        self.n_ins += 1
        return tok

    def dma(self, qeng, fn, reads=(), writes=(), group=None):
        if group is not None:
            waits = []
            for t in group["deps"]:
                self._need(qeng, t, waits)
        else:
            waits = self._deps(qeng, reads, writes)
        k = self.dma_rr[qeng]
        self.dma_rr[qeng] = (k + 1) % self.n_dma_sems
        key = ("d", qeng, k)
        prev = self.dma_cnt.get(key, 0)
        if prev:
            self._need(qeng, (key, prev), waits)
        val = prev + 16
        self.dma_cnt[key] = val
        tok = (key, val)
        self.q[qeng].append((waits, _record(fn), key, 16))
        if group is not None:
            group["toks"].append(tok)
        else:
            self._commit(tok, reads, writes)
        self.n_ins += 1
        return tok

    def group_begin(self, bufs):
        deps = []
        for b in bufs:
            deps += list(b.w) + list(b.r)
        return {"deps": deps, "toks": [], "bufs": bufs}

    def group_end(self, g):
        for b in g["bufs"]:
            b.w = list(g["toks"])
            b.r = []

    def final_wait(self, eng, bufs):
        waits = []
        for b in bufs:
            for t in b.w:
                self._need(eng, t, waits)
        self.q[eng].append((waits, None, None, 0))

    def emit(self):
        nc = self.nc
        with contextlib.ExitStack() as st:
            sems = {}
            for e in self.ENGS:
                sems[("e", e)] = st.enter_context(nc.semaphore("s_" + e))
            for key in self.dma_cnt:
                sems[key] = st.enter_context(nc.semaphore("d_%s_%d" % (key[1], key[2])))
            block = st.enter_context(nc.Block())

            def run(eh, lst, drain=False):
                for waits, fn, skey, inc in lst:
                    for key, val in waits:
                        eh.wait_ge(sems[key], val)
                    if drain and waits:
                        eh.drain()
                    if fn is not None:
                        name, a, k = fn
                        getattr(eh, name)(*a, **k).then_inc(sems[skey], inc)

            block.tensor(lambda t: run(t, self.q["pe"], PE_DRAIN))
            block.scalar(lambda t: run(t, self.q["act"]))
            block.vector(lambda t: run(t, self.q["dve"]))
            block.gpsimd(lambda t: run(t, self.q["pool"]))
            block.sync(lambda t: run(t, self.q["sp"]))


def build(NSEQ, S, phases="ABCD", debug=False):
    NT = S // 128
    NTILES = NSEQ * NT
    nc = bass.Bass("TRN2", target_bir_lowering=False)
    dt_in = lambda name, shape: nc.dram_tensor(name, list(shape), F32, kind="ExternalInput").ap()
    x_d = dt_in("x", [NSEQ * S, D])
    cT_d = dt_in("cT", [128, 8, NSEQ])
    pv_d = dt_in("pv", [128, DEPTH * PVL])
    cst_d = dt_in("cst", [128, 1024])
    rst_d = dt_in("rst", [128, 512])
    gb_d = dt_in("gbias", [2 * DEPTH, 128, D])
    fng_d = dt_in("fng", [128, D])
    ada_mix_w = dt_in("ada_mix_w", [DEPTH, D, 3 * D])
    ada_ffn_w = dt_in("ada_ffn_w", [DEPTH, D, 3 * D])
    w_in = dt_in("w_in", [DEPTH, D, INC])
    ml_wq = dt_in("ml_wq", [DEPTH, 4, 128, 64])
    ml_wk = dt_in("ml_wk", [DEPTH, 4, 128, 64])
    w_br_hg = dt_in("w_br_hg", [DEPTH, 512, D])
    w_br_ml = dt_in("w_br_ml", [DEPTH, 512, D])
    w_out = dt_in("w_out", [DEPTH, D, D])
    ffn_w1 = dt_in("ffn_w1", [1, D, DFF])
    ffn_w3 = dt_in("ffn_w3", [1, D, DFF])
    ffn_w2 = dt_in("ffn_w2", [1, DFF, D])
    moe_rw = dt_in("moe_router_w", [1, D, NE])
    moe_w1 = dt_in("moe_w1", [1, NE, D, DFE])
    moe_w3 = dt_in("moe_w3", [1, NE, D, DFE])
    moe_w2 = dt_in("moe_w2", [1, NE, DFE, D])
    out_d = nc.dram_tensor("out", [NSEQ * S, D], F32, kind="ExternalOutput").ap()
    cnt_d = nc.dram_tensor("cnt", [128, NE], F32, kind="ExternalOutput").ap()
    hsc_d = nc.dram_tensor("hsc", [NTILES, 128, 8 * 128], BF16, kind="Internal").ap()
    csc_d = nc.dram_tensor("csc", [NTILES, 128, NE], F32, kind="Internal").ap()
    CAP = max(128, ((NSEQ * S * 2 // NE) * 15 // 8 + 127) // 128 * 128)
    hg_d = nc.dram_tensor("hg", [NE * CAP, D], BF16, kind="Internal").ap()
    y_d = nc.dram_tensor("yex", [NE * CAP, D], F32, kind="Internal").ap()

    dbg = {}
    if debug:
        for nm, shp in (("dbg_hT", [128, 1024]), ("dbg_mod", [128, 16]), ("dbg_gate", [128, 1024]), ("dbg_act", [128, 1408]), ("dbg_xn", [128, 1024]), ("dbg_ob", [128, 1024])):
            dbg[nm] = nc.dram_tensor(nm, shp, F32, kind="ExternalOutput").ap()
    dbgB = []
    P = Prog(nc)
    st = contextlib.ExitStack()
    with st:
        def sb(name, shape, dt=F32):
            return st.enter_context(nc.sbuf_tensor("sb_" + name, list(shape), dt))

        ARENA_N = 67584
        arena = sb("arena", [128, ARENA_N], BF16)
        slotB = [Buf("slot0"), Buf("slot1")]
        X = [sb("X0", [128, D]), sb("X1", [128, D])]
        XB = [Buf("X0"), Buf("X1")]
        xn = sb("xn", [128, D], BF16); xnB = Buf("xn")
        hT = sb("hT", [128, 8, 128], BF16); hTB = Buf("hT")
        cst = sb("cst", [128, 1024]); cstB = Buf("cst")
        identb = sb("identb", [128, 128], BF16)
        trib = sb("trib", [128, 128], BF16)
        bonesb = sb("bonesb", [128, 128], BF16)
        onesb = sb("onesb", [128, 128], BF16)
        caus = sb("caus", [128, 128], BF16)
        m64 = sb("m64", [128, 64])
        rst = sb("rst", [128, 512], BF16);
        pv = sb("pv", [128, DEPTH * PVL]); pvB = Buf("pv")
        lbc = sb("lbc", [128, 2, 4]); lbB = Buf("lbc")
        cT = sb("cT", [128, 8, NSEQ])
        cact = sb("cact", [128, 8, NSEQ], BF16)
        cbc = sb("cbc", [128, 8, 128], BF16); cbcB = Buf("cbc")
        MS = sb("MS", [128, NSEQ, 8]); SH = sb("SH", [128, NSEQ, 8]); modB = Buf("mod")
        GATEb = sb("GATEb", [128, D]); gateB = Buf("gate")
        small = sb("small", [128, 64]);
        T = [sb("T%d" % i, [128, 512]) for i in range(6)]
        TB = [Buf("T%d" % i) for i in range(6)]
        constB = Buf("consts")

        psum_all = st.enter_context(nc.psum_tensor("psum_all", [128, 4096], F32))
        banks = [psum_all[:, i * 512:(i + 1) * 512] for i in range(8)]
        bankB = [Buf("bank%d" % i) for i in range(8)]
        bank_rr = [0]
        bank_n = [6]

        def nb():
            i = bank_rr[0] % bank_n[0]
            bank_rr[0] = (i + 1) % bank_n[0]
            return banks[i], bankB[i]

        def dve(fn, r=(), w=()):
            return P.op("dve", fn, r, w)

        def act(fn, r=(), w=()):
            return P.op("act", fn, r, w)

        def pe(fn, r=(), w=()):
            return P.op("pe", fn, r, w)

        def pool(fn, r=(), w=()):
            return P.op("pool", fn, r, w)

        P.dma("sp", lambda e: e.dma_start(out=cst[:], in_=cst_d), writes=[cstB])
        P.dma("pool", lambda e: e.dma_start(out=rst[:], in_=rst_d), writes=[constB])
        P.dma("sp", lambda e: e.dma_start(out=pv[:], in_=pv_d), writes=[pvB])
        P.dma("sp", lambda e: e.dma_start(out=cT[:], in_=cT_d), writes=[modB])
        dve(lambda e: e.tensor_copy(out=identb[:], in_=cst[:, 0:128]), [cstB], [constB])
        dve(lambda e: e.tensor_copy(out=trib[:], in_=cst[:, 128:256]), [cstB], [constB])
        dve(lambda e: e.tensor_copy(out=bonesb[:], in_=cst[:, 256:384]), [cstB], [constB])
        dve(lambda e: e.tensor_copy(out=caus[:], in_=cst[:, 384:512]), [cstB], [constB])
        dve(lambda e: e.tensor_copy(out=m64[:], in_=cst[:, 512:576]), [cstB], [constB])
        dve(lambda e: e.memset(onesb[:], 1.0), [], [constB])
        strilb = sb("strilb", [128, 128], BF16)
        dve(lambda e: e.tensor_copy(out=strilb[:], in_=cst[:, 576:704]), [cstB], [constB])
        act(lambda e: e.activation(out=cact[:], in_=cT[:], func=AF.Silu), [modB], [modB])

        def wload(dst_ap, src_ap, grp, ncols):
            K = dst_ap.shape[1]
            for k in range(K):
                for c0 in range(0, ncols, 1024):
                    c1 = min(ncols, c0 + 1024)
                    P.dma("pool", (lambda k, c0, c1: (lambda e: e.dma_start(out=dst_ap[:, k, c0:c1], in_=src_ap[:, k, c0:c1])))(k, c0, c1),
                          group=grp)

        def kview(w2d):
            return w2d.rearrange("(k p) n -> p k n", p=128)

        def ada_setup(ada_w_l, l, which, norm_g_col, ada_b_col, gb_row):
            aw = arena[:, 33792:33792 + 8 * 3072].rearrange("p (k n) -> p k n", k=8)
            g = P.group_begin([slotB[1]])
            wload(aw, kview(ada_w_l), g, 3072)
            P.group_end(g)
            bk, bb = nb()
            for j in range(16):
                for k in range(8):
                    pe((lambda j, k: (lambda e: e.matmul(bk[:, j * NSEQ:(j + 1) * NSEQ], lhsT=aw[:, k, j * 128:(j + 1) * 128], rhs=cact[:, k, :], start=(k == 0), stop=(k == 7))))(j, k),
                       [slotB[1], modB], [bb])
            bv = bk[:, 0:16 * NSEQ].rearrange("p (j s) -> p j s", s=NSEQ)
            for s in range(NSEQ):
                dve((lambda s: (lambda e: e.tensor_tensor(out=SH[:, s, :], in0=bv[:, 0:8, s], in1=pv[:, l * PVL + ada_b_col: l * PVL + ada_b_col + 8], op=ALU.add)))(s), [bb, pvB], [modB])
                dve((lambda s: (lambda e: e.tensor_tensor(out=MS[:, s, :], in0=bv[:, 8:16, s], in1=pv[:, l * PVL + ada_b_col + 8: l * PVL + ada_b_col + 16], op=ALU.add)))(s), [bb, pvB], [modB])
                dve((lambda s: (lambda e: e.scalar_tensor_tensor(out=MS[:, s, :], in0=MS[:, s, :], scalar=1.0, in1=pv[:, l * PVL + norm_g_col: l * PVL + norm_g_col + 8], op0=ALU.add, op1=ALU.mult)))(s), [pvB], [modB])
            return aw

        def gate_setup(aw, s, gb_row):
            dve(lambda e: e.tensor_copy(out=cbc[:], in_=cact[:, :, s:s + 1].broadcast_to([128, 8, 128])), [modB], [cbcB])
            P.dma("sp", lambda e: e.dma_start(out=GATEb[:], in_=gb_d[gb_row]), writes=[gateB])
            for half in range(2):
                bk, bb = nb()
                for k in range(8):
                    pe((lambda k, half: (lambda e: e.matmul(bk[:], lhsT=cbc[:, k, :], rhs=aw[:, k, 2048 + half * 512: 2048 + (half + 1) * 512], start=(k == 0), stop=(k == 7))))(k, half),
                       [cbcB, slotB[1]], [bb])
                dve((lambda half, bk: (lambda e: e.tensor_tensor(out=GATEb[:, half * 512:(half + 1) * 512], in0=GATEb[:, half * 512:(half + 1) * 512], in1=bk[:], op=ALU.add)))(half, bk), [bb], [gateB])

        def load_x(ti, slot, src):
            P.dma("sp", lambda e: e.dma_start(out=X[slot][:], in_=src[ti * 128:(ti + 1) * 128, :]), writes=[XB[slot]])

        def norm_mod(slot, s):
            Xs, Xb = X[slot], XB[slot]
            act(lambda e: e.activation(out=xn[:], in_=Xs[:], func=AF.Square, accum_out=small[:, 0:1]), [Xb], [xnB])
            dve(lambda e: e.tensor_scalar(out=small[:, 1:2], in0=small[:, 0:1], scalar1=1.0 / D, scalar2=EPS, op0=ALU.mult, op1=ALU.add), [xnB], [xnB])
            act(lambda e: e.activation(out=small[:, 1:2], in_=small[:, 1:2], func=AF.Ln), [xnB], [xnB])
            act(lambda e: e.activation(out=small[:, 2:3], in_=small[:, 1:2], func=AF.Exp, scale=-0.5), [xnB], [xnB])
            act(lambda e: e.activation(out=xn[:], in_=Xs[:], func=AF.Copy, scale=small[:, 2:3]), [Xb, xnB], [xnB])
            bk, bb = nb()
            bkb = bk[:].bitcast(BF16)
            for k in range(8):
                pe((lambda k: (lambda e: e.transpose(out=bkb[:, k * 128:(k + 1) * 128], in_=xn[:, k * 128:(k + 1) * 128], identity=identb[:])))(k), [xnB, constB], [bb])
            bv = bkb[:, 0:1024].rearrange("p (k t) -> p k t", k=8)
            dve(lambda e: e.tensor_tensor(out=T[0][:].rearrange("p (k t) -> p k t", k=8)[:, :, 0:64], in0=bv[:, :, 0:64], in1=MS[:, s, :].unsqueeze(2).broadcast_to([128, 8, 64]), op=ALU.mult), [bb, modB], [TB[0]])
            dve(lambda e: e.tensor_tensor(out=T[1][:].rearrange("p (k t) -> p k t", k=8)[:, :, 0:64], in0=bv[:, :, 64:128], in1=MS[:, s, :].unsqueeze(2).broadcast_to([128, 8, 64]), op=ALU.mult), [bb, modB], [TB[1]])
            dve(lambda e: e.tensor_tensor(out=hT[:, :, 0:64], in0=T[0][:].rearrange("p (k t) -> p k t", k=8), in1=SH[:, s, :].unsqueeze(2).broadcast_to([128, 8, 64]), op=ALU.add), [TB[0], modB], [hTB])
            dve(lambda e: e.tensor_tensor(out=hT[:, :, 64:128], in0=T[1][:].rearrange("p (k t) -> p k t", k=8), in1=SH[:, s, :].unsqueeze(2).broadcast_to([128, 8, 64]), op=ALU.add), [TB[1], modB], [hTB])

        actT = sb("actT", [128, 11, 128], BF16); actTB = Buf("actT")
        actT_l = [actT, sb("actT1", [128, 11, 128], BF16)]; actTB_l = [actTB, Buf("actT1")]
        hT_l = [hT, sb("hT1", [128, 8, 128], BF16)]; hTB_l = [hTB, Buf("hT1")]
        ffn_grp = [0]

        def ffn_views(slot):
            base = slot * 33792
            w1 = arena[:, base:base + 8 * DFE].rearrange("p (k n) -> p k n", k=8)
            w3 = arena[:, base + 8 * DFE: base + 16 * DFE].rearrange("p (k n) -> p k n", k=8)
            w2 = arena[:, base + 16 * DFE: base + 16 * DFE + 11 * D].rearrange("p (k n) -> p k n", k=11)
            return w1, w3, w2

        def ffn_load(slot, w1src, w3src, w2src):
            w1, w3, w2 = ffn_views(slot)
            g = P.group_begin([slotB[slot]])
            wload(w1, kview(w1src), g, DFE)
            wload(w3, kview(w3src), g, DFE)
            wload(w2, kview(w2src), g, D)
            P.group_end(g)

        def ffn_expert(slot, ob, obB, first, last):
            w1, w3, w2 = ffn_views(slot)
            for g0 in range(0, 11, 4):
                nblk = min(4, 11 - g0)
                ffn_grp[0] += 1
                ts = 2 if ffn_grp[0] % 2 == 0 else 5
                b1, b1B = nb()
                b3, b3B = nb()
                for jj in range(nblk):
                    j = g0 + jj
                    for k in range(8):
                        pe((lambda j, jj, k: (lambda e: e.matmul(b1[:, jj * 128:(jj + 1) * 128], lhsT=w1[:, k, j * 128:(j + 1) * 128], rhs=hT[:, k, :], start=(k == 0), stop=(k == 7))))(j, jj, k), [slotB[slot], hTB], [b1B])
                    for k in range(8):
                        pe((lambda j, jj, k: (lambda e: e.matmul(b3[:, jj * 128:(jj + 1) * 128], lhsT=w3[:, k, j * 128:(j + 1) * 128], rhs=hT[:, k, :], start=(k == 0), stop=(k == 7))))(j, jj, k), [slotB[slot], hTB], [b3B])
                n = nblk * 128
                act((lambda n, b1: (lambda e: e.activation(out=T[ts][:, 0:n], in_=b1[:, 0:n], func=AF.Silu)))(n, b1), [b1B], [TB[ts]])
                dve((lambda n, b3, g0: (lambda e: e.tensor_tensor(out=actT[:, g0:g0 + n // 128, :].rearrange("p j t -> p (j t)"), in0=T[ts][:, 0:n], in1=b3[:, 0:n], op=ALU.mult)))(n, b3, g0), [TB[ts], b3B], [actTB])
                for jj in range(nblk):
                    j = g0 + jj
                    for half in range(2):
                        pe((lambda j, half: (lambda e: e.matmul(ob[half][:], lhsT=actT[:, j, :], rhs=w2[:, j, half * 512:(half + 1) * 512], start=(first and j == 0), stop=(last and j == 10))))(j, half),
                           [actTB, slotB[slot]], [obB[half]])

        def residual(slot, ob, obB, comb_ap=None, combB=None):
            Xs, Xb = X[slot], XB[slot]
            for half in range(2):
                sl = slice(half * 512, (half + 1) * 512)
                if comb_ap is None:
                    dve((lambda half, sl: (lambda e: e.tensor_tensor(out=T[3 + half][:], in0=ob[half][:], in1=GATEb[:, sl], op=ALU.mult)))(half, sl), [obB[half], gateB], [TB[3 + half]])
                else:
                    dve((lambda half, sl: (lambda e: e.scalar_tensor_tensor(out=T[3 + half][:], in0=ob[half][:], scalar=comb_ap, in1=GATEb[:, sl], op0=ALU.mult, op1=ALU.mult)))(half, sl), [obB[half], gateB, combB], [TB[3 + half]])
                pool((lambda half, sl: (lambda e: e.tensor_tensor(out=Xs[:, sl], in0=Xs[:, sl], in1=T[3 + half][:], op=ALU.add)))(half, sl), [TB[3 + half], Xb], [Xb])

        def store_x(ti, slot, dstB):
            P.dma("sp", lambda e: e.dma_start(out=out_d[ti * 128:(ti + 1) * 128, :], in_=X[slot][:]), reads=[XB[slot]], writes=[dstB])

        xdB = [Buf("xd%d" % i) for i in range(NTILES)]

        def phase_dense(l, src):
            aw = ada_setup(ada_ffn_w[l], l, "ffn", PV_NFG, PV_AFB, 2 * l + 1)
            gate_rows(aw, 2 * l + 1)
            ffn_load(0, ffn_w1[0][:, 0:DFE], ffn_w3[0][:, 0:DFE], ffn_w2[0][0:DFE, :])
            ffn_load(1, ffn_w1[0][:, DFE:DFF], ffn_w3[0][:, DFE:DFF], ffn_w2[0][DFE:DFF, :])
            bank_n[0] = 4
            load_x(0, 0, src)
            for ti in range(NTILES):
                s = ti // NT
                slot = ti % 2
                set_par(slot)
                if ti + 1 < NTILES:
                    load_x(ti + 1, (ti + 1) % 2, src)
                if ti % NT == 0:
                    gate_fetch(s)
                norm_mod(slot, s)
                ob0, ob0B = banks[4 + 2 * slot], bankB[4 + 2 * slot]
                ob1, ob1B = banks[5 + 2 * slot], bankB[5 + 2 * slot]
                ffn_expert(0, [ob0, ob1], [ob0B, ob1B], True, False)
                ffn_expert(1, [ob0, ob1], [ob0B, ob1B], False, True)
                residual(slot, [ob0, ob1], [ob0B, ob1B])
                store_x(ti, slot, xdB[ti])
            bank_n[0] = 6

        gsc_d = nc.dram_tensor("gsc", [NSEQ, 128, D], F32, kind="Internal").ap()
        gscB = [Buf("gsc%d" % s) for s in range(NSEQ)]

        def gate_rows(aw, gb_row):
            for s in range(NSEQ):
                gate_setup(aw, s, gb_row)
                P.dma("sp", (lambda s: (lambda e: e.dma_start(out=gsc_d[s], in_=GATEb[:])))(s), reads=[gateB], writes=[gscB[s]])

        def gate_fetch(s):
            P.dma("sp", lambda e: e.dma_start(out=GATEb[:], in_=gsc_d[s]), reads=[gscB[s]], writes=[gateB])

        def load_x_dep(ti, slot, src):
            if src is out_d:
                P.dma("sp", lambda e: e.dma_start(out=X[slot][:], in_=src[ti * 128:(ti + 1) * 128, :]), reads=[xdB[ti]], writes=[XB[slot]])
            else:
                P.dma("sp", lambda e: e.dma_start(out=X[slot][:], in_=src[ti * 128:(ti + 1) * 128, :]), writes=[XB[slot]])
        load_x = load_x_dep

        rw = sb("rw", [128, 8, 2 * NE], BF16); rwB = Buf("rw")
        rw32 = sb("rw32", [128, 8, NE])
        hlo = sb("hlo_mrg", [128, 8, 128], BF16); hloB = Buf("hlo")
        comb = sb("comb", [128, 4 * NE]); combB = Buf("comb")
        comb_l = [comb, sb("comb1", [128, 4 * NE])]; combB_l = [combB, Buf("comb1")]
        rt = sb("rt", [128, 64]); rtB = Buf("rt")
        basec = sb("basec", [128, NE])
        capmax = sb("capmax", [128, NE])
        Mb = sb("Mb", [128, NE], BF16)
        RT = sb("RT", [128, NTILES, 4])
        idxu = sb("idxu", [128, NTILES, 2], mybir.dt.uint32)

        def set_par(p):
            nonlocal hT, hTB, actT, actTB, comb, combB
            hT, hTB = hT_l[p], hTB_l[p]
            actT, actTB = actT_l[p], actTB_l[p]
            comb, combB = comb_l[p], combB_l[p]
        fng = cst; fngB = cstB
        hT32 = None

        def router(slot, s, ti):
            for hf in range(2):
                dve((lambda hf: (lambda e: e.tensor_tensor(out=T[hf][:].rearrange("p (k t) -> p k t", k=8), in0=T[hf][:].rearrange("p (k t) -> p k t", k=8), in1=SH[:, s, :].unsqueeze(2).broadcast_to([128, 8, 64]), op=ALU.add)))(hf), [modB, hTB], [TB[hf]])
                dve((lambda hf: (lambda e: e.tensor_tensor(out=hlo[:, :, hf * 64:(hf + 1) * 64], in0=T[hf][:].rearrange("p (k t) -> p k t", k=8), in1=hT[:, :, hf * 64:(hf + 1) * 64], op=ALU.subtract)))(hf), [TB[hf], hTB], [hloB])
            bk, bb = nb()
            n = 0
            for (lh, rc) in ((hT, 0), (hT, NE), (hlo, 0)):
                for k in range(8):
                    pe((lambda lh, rc, k, n: (lambda e: e.matmul(bk[:, 0:NE], lhsT=lh[:, k, :], rhs=rw[:, k, rc:rc + NE], start=(n == 0), stop=(n == 23))))(lh, rc, k, n), [hTB, hloB, rwB], [bb])
                    n += 1
            lg = comb[:, 8:16]
            dve(lambda e: e.tensor_tensor(out=lg, in0=bk[:, 0:NE], in1=pv[:, PVL + PV_RB: PVL + PV_RB + NE], op=ALU.add), [bb, pvB], [combB])
            dve(lambda e: e.max(out=comb[:, 16:24], in_=lg), [], [combB])
            dve(lambda e: e.tensor_scalar(out=comb[:, 24:32], in0=lg, scalar1=comb[:, 16:17], scalar2=None, op0=ALU.subtract), [], [combB])
            act(lambda e: e.activation(out=comb[:, 24:32], in_=comb[:, 24:32], func=AF.Exp), [combB], [combB])
            dve(lambda e: e.tensor_scalar(out=comb[:, 0:8], in0=lg, scalar1=comb[:, 17:18], scalar2=None, op0=ALU.is_ge), [combB], [combB])
            dve(lambda e: e.tensor_tensor(out=comb[:, 0:8], in0=comb[:, 0:8], in1=comb[:, 24:32], op=ALU.mult), [], [combB])
            dve(lambda e: e.reduce_sum(out=small[:, 4:5], in_=comb[:, 0:8], axis=AX.X), [], [combB])
            dve(lambda e: e.reciprocal(out=small[:, 5:6], in_=small[:, 4:5]), [], [combB])
            dve(lambda e: e.tensor_scalar(out=comb[:, 0:8], in0=comb[:, 0:8], scalar1=small[:, 5:6], scalar2=None, op0=ALU.mult), [], [combB])

        hscB = [Buf("hsc%d" % i) for i in range(NTILES)]
        cscB = [Buf("csc%d" % i) for i in range(NTILES)]
        outB = [Buf("out%d" % i) for i in range(NTILES)]

        def phase_moe(l, src):
            aw = ada_setup(ada_ffn_w[l], l, "ffn", PV_NFG, PV_AFB, 2 * l + 1)
            gate_rows(aw, 2 * l + 1)
            P.dma("sp", lambda e: e.dma_start(out=rw32[:], in_=kview(moe_rw[0])), writes=[rwB])
            dve(lambda e: e.tensor_copy(out=rw[:, :, 0:NE], in_=rw32[:]), [rwB], [rwB])
            dve(lambda e: e.tensor_tensor(out=rw32[:], in0=rw32[:], in1=rw[:, :, 0:NE], op=ALU.subtract), [], [rwB])
            dve(lambda e: e.tensor_copy(out=rw[:, :, NE:2 * NE], in_=rw32[:]), [], [rwB])
            P.dma("sp", lambda e: e.dma_start(out=fng[:], in_=fng_d), writes=[fngB])
            ffn_load(0, moe_w1[0, 0], moe_w3[0, 0], moe_w2[0, 0])
            bank_n[0] = 4
            units = [(ex, ti) for ex in range(NE) for ti in range(NTILES)]

            def loads(u):
                ex, ti = units[u]
                p = u % 2
                set_par(p)
                load_x(ti, p, src if ex == 0 else out_d)
                if ex > 0:
                    P.dma("sp", lambda e: e.dma_start(out=hT[:].rearrange("p k t -> p (k t)"), in_=hsc_d[ti]), reads=[hscB[ti]], writes=[hTB])
                    P.dma("sp", lambda e: e.dma_start(out=comb[:, 0:NE], in_=csc_d[ti]), reads=[cscB[ti]], writes=[combB])

            loads(0)
            for u, (ex, ti) in enumerate(units):
                slot_w = ex % 2
                if ti == 0 and ex + 1 < NE:
                    ffn_load((ex + 1) % 2, moe_w1[0, ex + 1], moe_w3[0, ex + 1], moe_w2[0, ex + 1])
                s = ti // NT
                slot = u % 2
                if u + 1 < len(units):
                    loads(u + 1)
                set_par(slot)
                if ti % NT == 0:
                    gate_fetch(s)
                if ex == 0:
                    norm_mod(slot, s)
                    router(slot, s, ti)
                    P.dma("sp", lambda e: e.dma_start(out=hsc_d[ti], in_=hT[:].rearrange("p k t -> p (k t)")), reads=[hTB], writes=[hscB[ti]])
                    P.dma("sp", lambda e: e.dma_start(out=csc_d[ti], in_=comb[:, 0:NE]), reads=[combB], writes=[cscB[ti]])
                ob0, ob0B = banks[4 + 2 * slot], bankB[4 + 2 * slot]
                ob1, ob1B = banks[5 + 2 * slot], bankB[5 + 2 * slot]
                ffn_expert(slot_w, [ob0, ob1], [ob0B, ob1B], True, True)
                residual(slot, [ob0, ob1], [ob0B, ob1B], comb_ap=comb[:, ex:ex + 1], combB=combB)
                if ex < NE - 1:
                    store_x(ti, slot, xdB[ti])
                else:
                    final_norm(ti, slot)
            bank_n[0] = 6

        def phase_moe_sparse(l, src):
            from concourse.bass import IndirectOffsetOnAxis
            U32 = mybir.dt.uint32
            aw = ada_setup(ada_ffn_w[l], l, "ffn", PV_NFG, PV_AFB, 2 * l + 1)
            gate_rows(aw, 2 * l + 1)
            P.dma("sp", lambda e: e.dma_start(out=rw32[:], in_=kview(moe_rw[0])), writes=[rwB])
            dve(lambda e: e.tensor_copy(out=rw[:, :, 0:NE], in_=rw32[:]), [rwB], [rwB])
            dve(lambda e: e.tensor_tensor(out=rw32[:], in0=rw32[:], in1=rw[:, :, 0:NE], op=ALU.subtract), [], [rwB])
            dve(lambda e: e.tensor_copy(out=rw[:, :, NE:2 * NE], in_=rw32[:]), [], [rwB])
            ffn_load(0, moe_w1[0, 0], moe_w3[0, 0], moe_w2[0, 0])
            bank_n[0] = 4
            for e_ in range(NE):
                dve((lambda e_: (lambda e: e.memset(basec[:, e_:e_ + 1], float(e_ * CAP))))(e_), [], [rtB])
                dve((lambda e_: (lambda e: e.memset(capmax[:, e_:e_ + 1], float(e_ * CAP + CAP - 1))))(e_), [], [rtB])
            hgB = Buf("Hg")
            hg_grp = P.group_begin([hgB])
            load_x(0, 0, src)
            for ti in range(NTILES):
                s = ti // NT
                slot = ti % 2
                set_par(slot)
                if ti + 1 < NTILES:
                    load_x(ti + 1, (ti + 1) % 2, src)
                norm_mod(slot, s)
                for hf in range(2):
                    dve((lambda hf: (lambda e: e.tensor_tensor(out=T[hf][:].rearrange("p (k t) -> p k t", k=8), in0=T[hf][:].rearrange("p (k t) -> p k t", k=8), in1=SH[:, s, :].unsqueeze(2).broadcast_to([128, 8, 64]), op=ALU.add)))(hf), [modB, hTB], [TB[hf]])
                    dve((lambda hf: (lambda e: e.tensor_tensor(out=hlo[:, :, hf * 64:(hf + 1) * 64], in0=T[hf][:].rearrange("p (k t) -> p k t", k=8), in1=hT[:, :, hf * 64:(hf + 1) * 64], op=ALU.subtract)))(hf), [TB[hf], hTB], [hloB])
                bk, bb = nb()
                n = 0
                for (lh, rc) in ((hT, 0), (hT, NE), (hlo, 0)):
                    for k in range(8):
                        pe((lambda lh, rc, k, n: (lambda e: e.matmul(bk[:, 0:NE], lhsT=lh[:, k, :], rhs=rw[:, k, rc:rc + NE], start=(n == 0), stop=(n == 23))))(lh, rc, k, n), [hTB, hloB, rwB], [bb])
                        n += 1
                lg = rt[:, 48:56]
                dve(lambda e: e.tensor_tensor(out=lg, in0=bk[:, 0:NE], in1=pv[:, PVL + PV_RB: PVL + PV_RB + NE], op=ALU.add), [bb, pvB], [rtB])
                dve(lambda e: e.max(out=rt[:, 0:8], in_=lg), [], [rtB])
                dve(lambda e: e.tensor_scalar(out=rt[:, 8:16], in0=lg, scalar1=rt[:, 0:1], scalar2=None, op0=ALU.is_equal), [], [rtB])
                dve(lambda e: e.tensor_scalar(out=rt[:, 16:24], in0=lg, scalar1=rt[:, 1:2], scalar2=None, op0=ALU.is_equal), [], [rtB])
                dve(lambda e: e.tensor_tensor(out=Mb[:], in0=rt[:, 8:16], in1=rt[:, 16:24], op=ALU.add), [], [rtB])
                bp, bpB = nb()
                pe(lambda e: e.matmul(bp[:, 0:8], lhsT=strilb[:], rhs=Mb[:], start=True, stop=True), [rtB, constB], [bpB])
                pe(lambda e: e.matmul(bp[:, 8:16], lhsT=onesb[:], rhs=Mb[:], start=True, stop=True), [rtB, constB], [bpB])
                dve(lambda e: e.tensor_tensor(out=rt[:, 24:32], in0=bp[:, 0:8], in1=basec[:], op=ALU.add), [bpB], [rtB])
                dve(lambda e: e.tensor_tensor(out=rt[:, 24:32], in0=rt[:, 24:32], in1=capmax[:], op=ALU.min), [], [rtB])
                dve(lambda e: e.tensor_tensor(out=basec[:], in0=basec[:], in1=bp[:, 8:16], op=ALU.add), [bpB], [rtB])
                dve(lambda e: e.tensor_tensor(out=rt[:, 32:40], in0=rt[:, 8:16], in1=rt[:, 24:32], op=ALU.mult), [], [rtB])
                dve(lambda e: e.reduce_sum(out=RT[:, ti, 0:1], in_=rt[:, 32:40], axis=AX.X), [], [rtB])
                dve(lambda e: e.tensor_tensor(out=rt[:, 32:40], in0=rt[:, 16:24], in1=rt[:, 24:32], op=ALU.mult), [], [rtB])
                dve(lambda e: e.reduce_sum(out=RT[:, ti, 1:2], in_=rt[:, 32:40], axis=AX.X), [], [rtB])
                dve(lambda e: e.tensor_copy(out=idxu[:, ti, :], in_=RT[:, ti, 0:2]), [], [rtB])
                dve(lambda e: e.tensor_tensor(out=rt[:, 40:41], in0=rt[:, 1:2], in1=rt[:, 0:1], op=ALU.subtract), [], [rtB])
                act(lambda e: e.activation(out=rt[:, 41:42], in_=rt[:, 40:41], func=AF.Exp), [rtB], [rtB])
                dve(lambda e: e.tensor_scalar(out=rt[:, 42:43], in0=rt[:, 41:42], scalar1=1.0, scalar2=None, op0=ALU.add), [], [rtB])
                dve(lambda e: e.reciprocal(out=RT[:, ti, 2:3], in_=rt[:, 42:43]), [], [rtB])
                dve(lambda e: e.tensor_tensor(out=RT[:, ti, 3:4], in0=rt[:, 41:42], in1=RT[:, ti, 2:3], op=ALU.mult), [], [rtB])
                bt, btB = nb()
                btb = bt[:].bitcast(BF16)
                for k in range(8):
                    pe((lambda k: (lambda e: e.transpose(out=btb[:, k * 128:(k + 1) * 128], in_=hT[:, k, :], identity=identb[:])))(k), [hTB, constB], [btB])
                act(lambda e: e.activation(out=xn[:], in_=btb[:, 0:1024], func=AF.Copy), [btB], [xnB])
                for k in range(2):
                    P.dma("pool", (lambda k: (lambda e: e.indirect_dma_start(out=hg_d, out_offset=IndirectOffsetOnAxis(ap=idxu[:, ti, k:k + 1], axis=0), in_=xn[:], in_offset=None)))(k), group=hg_grp)
                    for t_ in list(xnB.w) + list(rtB.w):
                        P._need("pool", t_, P.q["pool"][-1][0])
                    xnB.r.append(hg_grp["toks"][-1])
                    rtB.r.append(hg_grp["toks"][-1])
            P.group_end(hg_grp)
            cntB = Buf("cnt"); dbgB.append(cntB)
            P.dma("sp", lambda e: e.dma_start(out=cnt_d, in_=basec[:]), reads=[rtB], writes=[cntB])
            yB = Buf("Y")
            y_grp = P.group_begin([yB])
            units = [(ex, t) for ex in range(NE) for t in range(CAP // 128)]
            stage = [xn[:], hlo[:].rearrange("p k t -> p (k t)")]
            stageB = [xnB, hloB]

            def loads2(u):
                ex, t = units[u]
                p = u % 2
                r0 = ex * CAP + t * 128
                P.dma("sp", lambda e: e.dma_start(out=stage[p], in_=hg_d[r0:r0 + 128, :]), reads=[hgB], writes=[stageB[p]])

            loads2(0)
            for u, (ex, t) in enumerate(units):
                slot_w = ex % 2
                if t == 0 and ex + 1 < NE:
                    ffn_load((ex + 1) % 2, moe_w1[0, ex + 1], moe_w3[0, ex + 1], moe_w2[0, ex + 1])
                p = u % 2
                if u + 1 < len(units):
                    loads2(u + 1)
                set_par(p)
                bt, btB = nb()
                btb = bt[:].bitcast(BF16)
                for k in range(8):
                    pe((lambda k: (lambda e: e.transpose(out=btb[:, k * 128:(k + 1) * 128], in_=stage[p][:, k * 128:(k + 1) * 128], identity=identb[:])))(k), [stageB[p], constB], [btB])
                act(lambda e: e.activation(out=hT[:].rearrange("p k t -> p (k t)"), in_=btb[:, 0:1024], func=AF.Copy), [btB], [hTB])
                ob0, ob0B = banks[4 + 2 * p], bankB[4 + 2 * p]
                ob1, ob1B = banks[5 + 2 * p], bankB[5 + 2 * p]
                ffn_expert(slot_w, [ob0, ob1], [ob0B, ob1B], True, True)
                act(lambda e: e.activation(out=X[p][:, 0:512], in_=ob0[:], func=AF.Copy), [ob0B], [XB[p]])
                dve(lambda e: e.tensor_copy(out=X[p][:, 512:1024], in_=ob1[:]), [ob1B], [XB[p]])
                r0 = ex * CAP + t * 128
                P.dma("sp", lambda e: e.dma_start(out=y_d[r0:r0 + 128, :], in_=X[p][:]), group=y_grp)
                for t_ in list(XB[p].w):
                    P._need("sp", t_, P.q["sp"][-1][0])
                XB[p].r.append(y_grp["toks"][-1])
            P.group_end(y_grp)
            bank_n[0] = 6
            P.dma("sp", lambda e: e.dma_start(out=fng[:], in_=fng_d), writes=[fngB])
            Yv = [[arena[:, (pp * 2 + k) * 2048:(pp * 2 + k + 1) * 2048].bitcast(F32) for k in range(2)] for pp in range(2)]
            YvB = [[Buf("Yv%d%d" % (pp, k)) for k in range(2)] for pp in range(2)]
            for pp in range(2):
                for k in range(2):
                    for sbuf_ in slotB:
                        YvB[pp][k].r += list(sbuf_.r) + list(sbuf_.w)

            def loads3(ti):
                p = ti % 2
                load_x(ti, p, src)
                for k in range(2):
                    P.dma("pool", (lambda k: (lambda e: e.indirect_dma_start(out=Yv[p][k], out_offset=None, in_=y_d, in_offset=IndirectOffsetOnAxis(ap=idxu[:, ti, k:k + 1], axis=0))))(k), reads=[yB, rtB], writes=[YvB[p][k]])

            loads3(0)
            for ti in range(NTILES):
                s = ti // NT
                p = ti % 2
                if ti + 1 < NTILES:
                    loads3(ti + 1)
                if ti % NT == 0:
                    gate_fetch(s)
                Xs, Xb = X[p], XB[p]
                for half in range(2):
                    sl = slice(half * 512, (half + 1) * 512)
                    dve((lambda sl, half: (lambda e: e.tensor_scalar(out=T[half][:], in0=Yv[p][0][:, sl], scalar1=RT[:, ti, 2:3], scalar2=None, op0=ALU.mult)))(sl, half), [YvB[p][0], rtB], [TB[half]])
                    dve((lambda sl, half: (lambda e: e.scalar_tensor_tensor(out=T[half][:], in0=Yv[p][1][:, sl], scalar=RT[:, ti, 3:4], in1=T[half][:], op0=ALU.mult, op1=ALU.add)))(sl, half), [YvB[p][1], rtB], [TB[half]])
                    dve((lambda sl, half: (lambda e: e.tensor_tensor(out=T[half][:], in0=T[half][:], in1=GATEb[:, sl], op=ALU.mult)))(sl, half), [gateB], [TB[half]])
                    pool((lambda sl, half: (lambda e: e.tensor_tensor(out=Xs[:, sl], in0=Xs[:, sl], in1=T[half][:], op=ALU.add)))(sl, half), [TB[half], Xb], [Xb])
                final_norm(ti, p)

        def final_norm(ti, slot):
            Xs, Xb = X[slot], XB[slot]
            act(lambda e: e.activation(out=xn[:], in_=Xs[:], func=AF.Square, accum_out=small[:, 0:1]), [Xb], [xnB])
            dve(lambda e: e.tensor_scalar(out=small[:, 1:2], in0=small[:, 0:1], scalar1=1.0 / D, scalar2=EPS, op0=ALU.mult, op1=ALU.add), [xnB], [xnB])
            act(lambda e: e.activation(out=small[:, 1:2], in_=small[:, 1:2], func=AF.Ln), [xnB], [xnB])
            act(lambda e: e.activation(out=small[:, 2:3], in_=small[:, 1:2], func=AF.Exp, scale=-0.5), [xnB], [xnB])
            dve(lambda e: e.scalar_tensor_tensor(out=Xs[:], in0=Xs[:], scalar=small[:, 2:3], in1=fng[:], op0=ALU.mult, op1=ALU.mult), [xnB, fngB], [Xb])
            P.dma("sp", lambda e: e.dma_start(out=out_d[ti * 128:(ti + 1) * 128, :], in_=Xs[:]), reads=[Xb], writes=[xdB[ti], outB[ti]])

        def mixer_bufs():
            pass

        if "A" in phases or "C" in phases:
            qt = sb("qt", [128, 512], BF16); kt = sb("kt", [128, 512], BF16); qE = sb("qE", [128, 512], BF16); kdT = sb("kdT", [128, 512], BF16)
            hgB = Buf("hgops")
            vtok = sb("vtok", [128, 512], BF16); vtokB = Buf("vtok")
            kdtok = sb("kdtok", [128, 512], BF16); kdtokB = Buf("kdtok")
            Pm = sb("Pm", [128, 4, 64], BF16); PmB = Buf("Pm")
            S32 = sb("S32", [128, 4, 128]); Sbf = sb("Sbf", [128, 4, 128], BF16); SB_ = Buf("S")
            Gs = sb("Gs", [128, 512], BF16); GsB = Buf("Gs")
            yhgT = sb("yhgT", [128, 4, 128], BF16); yhgB = Buf("yhgT")
            ymlT = sb("ymlT", [128, 4, 128], BF16); ymlB = Buf("ymlT")
            eA = sb("eA", [128, 16]); eAB = Buf("eA")
            U = sb("U", [128, 4, 131]); UB = Buf("U")
            ucT = sb("ucT", [128, 4, 128], BF16); ucB = Buf("ucT")
            qTm = sb("qTm", [64, 4, 128], BF16); kTm = sb("kTm", [64, 4, 128], BF16); qkB = Buf("qkm")
            ktok = sb("ktok", [128, 4, 64], BF16); ktokB = Buf("ktok")
            vaug = sb("vaug", [128, 4, 130], BF16); vaugB = Buf("vaug")
            mosg = sb("mosg", [128, 512], BF16); mosgB = Buf("mosg")
            _a1 = actT_l[1][:].rearrange("p j t -> p (j t)")
            Pml = _a1[:, 512:1024].rearrange("p (h t) -> p h t", h=4); PmlB = actTB_l[1]
            C32 = sb("C32", [64, 4, 130]); Cbf = sb("Cbf", [64, 4, 130], BF16); CB_ = Buf("C")
            gts = sb("gts", [128, 64]); gtsB = Buf("gts")
            gthl = sb("gthl", [128, 8], BF16);
            yml = _a1[:, 0:512]; ymlTokB = actTB_l[1]
            SGA = hT_l[1]; SGB = actT_l[0][:, 0:8, :]; sgB = hTB_l[1]; sgB2 = actTB_l[0]
            mrg = hlo; mrgB = hloB
            wqk = sb("wqk", [128, 2, 4, 64], BF16); wqkB = Buf("wqk")

        def phase_mixer(l, src):
            aw = ada_setup(ada_mix_w[l], l, "mix", PV_NMG, PV_AMB, 2 * l)
            gate_rows(aw, 2 * l)
            Win = arena[:, 0:8 * INC].rearrange("p (k n) -> p k n", k=8)
            o = 8 * INC
            Wbh = arena[:, o:o + 4096].rearrange("p (k n) -> p k n", k=4)
            Wbm = arena[:, o + 4096:o + 8192].rearrange("p (k n) -> p k n", k=4)
            Wo = arena[:, o + 8192:o + 16384].rearrange("p (k n) -> p k n", k=8)
            WB = slotB
            g = P.group_begin(WB + [wqkB])
            wload(Win, kview(w_in[l]), g, INC)
            wload(Wbh, kview(w_br_hg[l]), g, D)
            wload(Wbm, kview(w_br_ml[l]), g, D)
            wload(Wo, kview(w_out[l]), g, D)
            for hh in range(4):
                P.dma("pool", (lambda hh: (lambda e: e.dma_start(out=wqk[:, 0, hh, :], in_=ml_wq[l, hh])))(hh), group=g)
                P.dma("pool", (lambda hh: (lambda e: e.dma_start(out=wqk[:, 1, hh, :], in_=ml_wk[l, hh])))(hh), group=g)
            P.group_end(g)
            if l == 0:
                dve(lambda e: e.memset(lbc[:, 0, :], 0.0), [], [lbB])
                dve(lambda e: e.memset(lbc[:, 1, :], 1.0), [], [lbB])
            else:
                dve(lambda e: e.tensor_tensor(out=lbc[:, 0, :], in0=pv[:, PVL + PV_LB:PVL + PV_LB + 4], in1=pv[:, PV_LB:PV_LB + 4], op=ALU.subtract), [pvB], [lbB])
                act(lambda e: e.activation(out=lbc[:, 0, :], in_=lbc[:, 0, :], func=AF.Sigmoid), [], [lbB])
                dve(lambda e: e.tensor_scalar(out=lbc[:, 1, :], in0=lbc[:, 0, :], scalar1=-1.0, scalar2=1.0, op0=ALU.mult, op1=ALU.add), [], [lbB])
            pvl = l * PVL

            def proj_fm(c0, nblk, bk, bb):
                for j in range(nblk):
                    for k in range(8):
                        pe((lambda j, k: (lambda e: e.matmul(bk[:, j * 128:(j + 1) * 128], lhsT=Win[:, k, c0 + j * 128:c0 + (j + 1) * 128], rhs=hT[:, k, :], start=(k == 0), stop=(k == 7))))(j, k), [WB[0], hTB], [bb])

            def proj_tm(c0, n, bk, bb):
                for k in range(8):
                    pe((lambda k: (lambda e: e.matmul(bk[:, 0:n], lhsT=hT[:, k, :], rhs=Win[:, k, c0:c0 + n], start=(k == 0), stop=(k == 7))))(k), [WB[0], hTB], [bb])

            set_par(0)
            bank_n[0] = 6
            for ti in range(NTILES):
                s = ti // NT
                slot = ti % 2
                first = (ti % NT == 0)
                if first:
                    gate_fetch(s)
                    dve(lambda e: e.memset(S32[:], 0.0), [], [SB_])
                    dve(lambda e: e.memset(Sbf[:], 0.0), [], [SB_])
                    dve(lambda e: e.memset(C32[:], 0.0), [], [CB_])
                    dve(lambda e: e.memset(Cbf[:], 0.0), [], [CB_])
                    dve(lambda e: e.memset(U[:], 0.0), [], [UB])
                load_x(ti, slot, src)
                norm_mod(slot, s)

                bq, bqB = nb(); proj_fm(C_Q, 4, bq, bqB)
                bf, bfB = nb(); proj_fm(C_F, 4, bf, bfB)
                bg, bgB = nb(); proj_fm(C_G, 4, bg, bgB)
                bv, bvB = nb(); proj_tm(C_I, 512, bv, bvB)
                Q, Fb, LF, KK, E1, E2 = T[0], T[1], T[2], T[3], T[4], T[5]
                act(lambda e: e.activation(out=Q[:], in_=bq[:], func=AF.Silu), [bqB], [TB[0]])
                act(lambda e: e.activation(out=Gs[:], in_=bg[:], func=AF.Silu), [bgB], [GsB])
                act(lambda e: e.activation(out=Fb[:], in_=bf[:], func=AF.Sigmoid), [bfB], [TB[1]])
                act(lambda e: e.activation(out=vtok[:], in_=bv[:], func=AF.Copy), [bvB], [vtokB])
                for hh in range(4):
                    dve((lambda hh: (lambda e: e.tensor_scalar(out=Fb[:, hh * 128:(hh + 1) * 128], in0=Fb[:, hh * 128:(hh + 1) * 128], scalar1=lbc[:, 1, hh:hh + 1], scalar2=lbc[:, 0, hh:hh + 1], op0=ALU.mult, op1=ALU.add)))(hh), [lbB], [TB[1]])
                act(lambda e: e.activation(out=LF[:], in_=Fb[:], func=AF.Ln), [TB[1]], [TB[2]])
                dve(lambda e: e.tensor_scalar(out=KK[:], in0=Fb[:], scalar1=-1.0, scalar2=1.0, op0=ALU.mult, op1=ALU.add), [TB[1]], [TB[3]])
                A_ = Fb
                dve(lambda e: e.tensor_tensor_scan(out=A_[:], data0=rst[:], data1=LF[:], initial=0.0, op0=ALU.mult, op1=ALU.add), [TB[2], constB], [TB[1]])
                A4 = A_[:].rearrange("p (g t) -> p g t", t=64)
                Dd = LF
                dve(lambda e: e.tensor_tensor(out=Dd[:].rearrange("p (g t) -> p g t", t=64), in0=A4, in1=A4[:, :, 31:32].broadcast_to([128, 8, 64]), op=ALU.subtract), [TB[1]], [TB[2]])
                dve(lambda e: e.tensor_copy(out=eA[:, 0:8], in_=A4[:, :, 31]), [TB[1]], [eAB])
                dve(lambda e: e.tensor_copy(out=eA[:, 8:16], in_=Dd[:].rearrange("p (g t) -> p g t", t=64)[:, :, 63]), [TB[2]], [eAB])
                dve(lambda e: e.tensor_copy(out=small[:, 8:16], in_=A4[:, :, 63]), [TB[1]], [eAB])
                dve(lambda e: e.tensor_scalar(out=Dd[:], in0=Dd[:], scalar1=40.0, scalar2=-40.0, op0=ALU.min, op1=ALU.max), [eAB], [TB[2]])
                act(lambda e: e.activation(out=E1[:], in_=Dd[:], func=AF.Exp), [TB[2]], [TB[4]])
                act(lambda e: e.activation(out=E2[:], in_=Dd[:], func=AF.Exp, scale=-1.0), [TB[2]], [TB[5]])
                act(lambda e: e.activation(out=eA[:], in_=eA[:], func=AF.Exp), [eAB], [eAB])
                act(lambda e: e.activation(out=small[:, 8:16], in_=small[:, 8:16], func=AF.Exp), [eAB], [eAB])
                dve(lambda e: e.tensor_tensor(out=Q[:], in0=Q[:], in1=E1[:], op=ALU.mult), [TB[4]], [TB[0]])
                dve(lambda e: e.tensor_tensor(out=KK[:], in0=KK[:], in1=E2[:], op=ALU.mult), [TB[5]], [TB[3]])
                act(lambda e: e.activation(out=qt[:], in_=Q[:], func=AF.Copy), [TB[0]], [hgB])
                act(lambda e: e.activation(out=kt[:], in_=KK[:], func=AF.Copy), [TB[3]], [hgB])
                dve(lambda e: e.tensor_tensor(out=qE[:].rearrange("p (g t) -> p g t", t=64), in0=Q[:].rearrange("p (g t) -> p g t", t=64), in1=eA[:, 0:8].unsqueeze(2).broadcast_to([128, 8, 64]), op=ALU.mult), [TB[0], eAB], [hgB])
                dve(lambda e: e.tensor_tensor(out=kdT[:].rearrange("p (g t) -> p g t", t=64), in0=KK[:].rearrange("p (g t) -> p g t", t=64), in1=eA[:, 8:16].unsqueeze(2).broadcast_to([128, 8, 64]), op=ALU.mult), [TB[3], eAB], [hgB])
                if debug and ti == 1:
                    def dump2(nm, c0, ap, bufs):
                        b = Buf(nm + str(c0)); dbgB.append(b)
                        P.dma("pool", lambda e: e.dma_start(out=dbg[nm][:, c0:c0 + 512], in_=ap), reads=bufs, writes=[b])
                    dump2("dbg_hT", 0, A_[:], [TB[1]])
                    dump2("dbg_hT", 512, Dd[:], [TB[2]])
                    dump2("dbg_gate", 0, Q[:], [TB[0]])
                    dump2("dbg_gate", 512, KK[:], [TB[3]])
                    dump2("dbg_xn", 0, E1[:], [TB[4]])
                    dump2("dbg_xn", 512, E2[:], [TB[5]])
                bt, btB = nb()
                btb = bt[:].bitcast(BF16)
                for hh in range(4):
                    pe((lambda hh: (lambda e: e.transpose(out=btb[:, hh * 128:(hh + 1) * 128], in_=kdT[:, hh * 128:(hh + 1) * 128], identity=identb[:])))(hh), [hgB, constB], [btB])
                act(lambda e: e.activation(out=kdtok[:], in_=btb[:, 0:512], func=AF.Copy), [btB], [kdtokB])
                bs, bsB = nb()
                for hh in range(4):
                    for c in range(2):
                        g = hh * 2 + c
                        pe((lambda hh, c, g: (lambda e: e.matmul(bs[c * 64:(c + 1) * 64, hh * 64:(hh + 1) * 64], lhsT=kt[:, g * 64:(g + 1) * 64], rhs=qt[:, g * 64:(g + 1) * 64], start=True, stop=True)))(hh, c, g), [hgB], [bsB])
                dve(lambda e: e.tensor_tensor(out=Pm[:], in0=bs[:, 0:256].rearrange("p (h t) -> p h t", h=4), in1=m64[:].unsqueeze(1).broadcast_to([128, 4, 64]), op=ALU.mult), [bsB, constB], [PmB])
                bo, boB = nb()
                for c in range(2):
                    rs = slice(c * 64, (c + 1) * 64)
                    for hh in range(4):
                        g = hh * 2 + c
                        pe((lambda hh, c, g, rs: (lambda e: e.matmul(bo[:, g * 64:(g + 1) * 64], lhsT=vtok[rs, hh * 128:(hh + 1) * 128], rhs=Pm[rs, hh, :], start=True, stop=False)))(hh, c, g, rs), [vtokB, PmB], [boB])
                        pe((lambda hh, c, g: (lambda e: e.matmul(bo[:, g * 64:(g + 1) * 64], lhsT=Sbf[:, hh, :], rhs=qE[:, g * 64:(g + 1) * 64], start=False, stop=True)))(hh, c, g), [SB_, hgB], [boB])
                    bd, bdB = nb()
                    for hh in range(4):
                        pe((lambda hh, rs: (lambda e: e.matmul(bd[:, hh * 128:(hh + 1) * 128], lhsT=kdtok[rs, hh * 128:(hh + 1) * 128], rhs=vtok[rs, hh * 128:(hh + 1) * 128], start=True, stop=True)))(hh, rs), [kdtokB, vtokB], [bdB])
                    for hh in range(4):
                        g = hh * 2 + c
                        dve((lambda hh, g, bd: (lambda e: e.scalar_tensor_tensor(out=S32[:, hh, :], in0=S32[:, hh, :], scalar=small[:, 8 + g:9 + g], in1=bd[:, hh * 128:(hh + 1) * 128], op0=ALU.mult, op1=ALU.add)))(hh, g, bd), [bdB, eAB], [SB_])
                    act(lambda e: e.activation(out=Sbf[:], in_=S32[:], func=AF.Copy), [], [SB_])
                OS = T[4]
                act(lambda e: e.activation(out=OS[:], in_=bo[:], func=AF.Copy), [boB], [TB[4]])
                SQ = T[5]
                act(lambda e: e.activation(out=SQ[:].bitcast(BF16)[:, 0:512], in_=bo[:], func=AF.Square), [boB], [TB[5]])
                bn, bnB = nb()
                pe(lambda e: e.matmul(bn[:], lhsT=onesb[:], rhs=SQ[:].bitcast(BF16)[:, 0:512], start=True, stop=True), [TB[5], constB], [bnB])
                R = T[2]
                dve(lambda e: e.tensor_scalar(out=R[:], in0=bn[:], scalar1=1.0 / 128, scalar2=EPS, op0=ALU.mult, op1=ALU.add), [bnB], [TB[2]])
                act(lambda e: e.activation(out=R[:], in_=R[:], func=AF.Ln), [], [TB[2]])
                act(lambda e: e.activation(out=R[:], in_=R[:], func=AF.Exp, scale=-0.5), [], [TB[2]])
                dve(lambda e: e.tensor_tensor(out=OS[:], in0=OS[:], in1=R[:], op=ALU.mult), [TB[2]], [TB[4]])
                dve(lambda e: e.tensor_tensor(out=OS[:], in0=OS[:], in1=Gs[:], op=ALU.mult), [GsB], [TB[4]])
                for hh in range(4):
                    dve((lambda hh: (lambda e: e.tensor_scalar(out=yhgT[:, hh, :], in0=OS[:, hh * 128:(hh + 1) * 128], scalar1=pv[:, pvl + PV_HNG + hh: pvl + PV_HNG + hh + 1], scalar2=None, op0=ALU.mult)))(hh), [TB[4], pvB], [yhgB])

                bu, buB = nb(); proj_fm(C_MU, 4, bu, buB)
                bmv, bmvB = nb(); proj_tm(C_MV, 512, bmv, bmvB)
                bmo, bmoB = nb(); proj_tm(C_MO, 512, bmo, bmoB)
                bgt, bgtB = nb(); proj_tm(C_MI, 8, bgt, bgtB)
                act(lambda e: e.activation(out=U[:, :, 3:131], in_=bu[:].rearrange("p (h t) -> p h t", h=4), func=AF.Copy), [buB], [UB])
                CV = T[0]
                for hh in range(4):
                    cw = pvl + PV_CW
                    dve((lambda hh, cw: (lambda e: e.tensor_scalar(out=CV[:, hh * 128:(hh + 1) * 128], in0=U[:, hh, 0:128], scalar1=pv[:, cw + 0 * 4 + hh: cw + 0 * 4 + hh + 1], scalar2=pv[:, pvl + PV_CB + hh: pvl + PV_CB + hh + 1], op0=ALU.mult, op1=ALU.add)))(hh, cw), [UB, pvB], [TB[0]])
                    for j in range(1, 4):
                        dve((lambda hh, cw, j: (lambda e: e.scalar_tensor_tensor(out=CV[:, hh * 128:(hh + 1) * 128], in0=U[:, hh, j:j + 128], scalar=pv[:, cw + j * 4 + hh: cw + j * 4 + hh + 1], in1=CV[:, hh * 128:(hh + 1) * 128], op0=ALU.mult, op1=ALU.add)))(hh, cw, j), [UB, pvB], [TB[0]])
                pool(lambda e: e.tensor_copy(out=U[:, :, 0:3], in_=U[:, :, 128:131]), [], [UB])
                act(lambda e: e.activation(out=ucT[:].rearrange("p h t -> p (h t)"), in_=CV[:], func=AF.Silu), [TB[0]], [ucB])
                act(lambda e: e.activation(out=mosg[:], in_=bmo[:], func=AF.Sigmoid), [bmoB], [mosgB])
                act(lambda e: e.activation(out=vaug[:, :, 0:128], in_=bmv[:].rearrange("p (h t) -> p h t", h=4), func=AF.Copy), [bmvB], [vaugB])
                dve(lambda e: e.memset(vaug[:, :, 128:130], 1.0), [], [vaugB])
                dve(lambda e: e.tensor_copy(out=gts[:, 0:4], in_=bgt[:, 0:4]), [bgtB], [gtsB])
                dve(lambda e: e.tensor_tensor(out=gts[:, 4:8], in0=bgt[:, 4:8], in1=pv[:, pvl + PV_FB: pvl + PV_FB + 4], op=ALU.add), [bgtB, pvB], [gtsB])
                act(lambda e: e.activation(out=gts[:, 4:8], in_=gts[:, 4:8], func=AF.Exp, scale=-1.0), [], [gtsB])
                act(lambda e: e.activation(out=gts[:, 8:12], in_=gts[:, 4:8], func=AF.Ln, bias=1.0), [], [gtsB])
                dve(lambda e: e.tensor_scalar(out=gts[:, 8:12], in0=gts[:, 8:12], scalar1=-1.0, scalar2=None, op0=ALU.mult), [], [gtsB])
                dve(lambda e: e.tensor_copy(out=gthl[:, 0:4], in_=gts[:, 8:12]), [], [gtsB])
                dve(lambda e: e.tensor_tensor(out=gts[:, 32:36], in0=gts[:, 8:12], in1=gthl[:, 0:4], op=ALU.subtract), [], [gtsB])
                dve(lambda e: e.tensor_copy(out=gthl[:, 4:8], in_=gts[:, 32:36]), [], [gtsB])
                bc, bcB = nb()
                pe(lambda e: e.matmul(bc[:, 0:4], lhsT=trib[:], rhs=gthl[:, 0:4], start=True, stop=False), [gtsB, constB], [bcB])
                pe(lambda e: e.matmul(bc[:, 0:4], lhsT=trib[:], rhs=gthl[:, 4:8], start=False, stop=True), [gtsB, constB], [bcB])
                pe(lambda e: e.matmul(bc[:, 4:8], lhsT=onesb[:], rhs=gthl[:, 0:4], start=True, stop=False), [gtsB, constB], [bcB])
                pe(lambda e: e.matmul(bc[:, 4:8], lhsT=onesb[:], rhs=gthl[:, 4:8], start=False, stop=True), [gtsB, constB], [bcB])
                dve(lambda e: e.tensor_copy(out=gts[:, 12:20], in_=bc[:, 0:8]), [bcB], [gtsB])
                dve(lambda e: e.tensor_tensor(out=gts[:, 20:24], in0=gts[:, 0:4], in1=gts[:, 12:16], op=ALU.subtract), [], [gtsB])
                dve(lambda e: e.tensor_tensor(out=gts[:, 28:32], in0=gts[:, 20:24], in1=gts[:, 16:20], op=ALU.add), [], [gtsB])
                dve(lambda e: e.tensor_copy(out=gts[:, 24:28], in_=gts[:, 12:16]), [], [gtsB])
                act(lambda e: e.activation(out=gts[:, 20:32], in_=gts[:, 20:32], func=AF.Exp), [], [gtsB])
                act(lambda e: e.activation(out=gts[:, 36:40], in_=gts[:, 16:20], func=AF.Exp), [], [gtsB])
                bqk, bqkB = nb()
                for hh in range(4):
                    pe((lambda hh: (lambda e: e.matmul(bqk[0:64, hh * 128:(hh + 1) * 128], lhsT=wqk[:, 0, hh, :], rhs=ucT[:, hh, :], start=True, stop=True)))(hh), [wqkB, ucB], [bqkB])
                bkk, bkkB = nb()
                for hh in range(4):
                    pe((lambda hh: (lambda e: e.matmul(bkk[0:64, hh * 128:(hh + 1) * 128], lhsT=wqk[:, 1, hh, :], rhs=ucT[:, hh, :], start=True, stop=True)))(hh), [wqkB, ucB], [bkkB])
                bkt, bktB = nb()
                for hh in range(4):
                    pe((lambda hh: (lambda e: e.matmul(bkt[:, hh * 64:(hh + 1) * 64], lhsT=ucT[:, hh, :], rhs=wqk[:, 1, hh, :], start=True, stop=True)))(hh), [wqkB, ucB], [bktB])
                act(lambda e: e.activation(out=qTm[:].rearrange("p h t -> p (h t)"), in_=bqk[0:64, :], func=AF.Copy, scale=0.125), [bqkB], [qkB])
                act(lambda e: e.activation(out=kTm[:].rearrange("p h t -> p (h t)"), in_=bkk[0:64, :], func=AF.Copy), [bkkB], [qkB])
                dve(lambda e: e.tensor_tensor(out=ktok[:], in0=bkt[:, 0:256].rearrange("p (h e) -> p h e", h=4), in1=gts[:, 28:32].unsqueeze(2).broadcast_to([128, 4, 64]), op=ALU.mult), [bktB, gtsB], [ktokB])
                bsm, bsmB = nb()
                for hh in range(4):
                    pe((lambda hh: (lambda e: e.matmul(bsm[:, hh * 128:(hh + 1) * 128], lhsT=kTm[:, hh, :], rhs=qTm[:, hh, :], start=True, stop=True)))(hh), [qkB], [bsmB])
                for hh in range(4):
                    dve((lambda hh: (lambda e: e.scalar_tensor_tensor(out=Pml[:, hh, :], in0=bsm[:, hh * 128:(hh + 1) * 128], scalar=gts[:, 20 + hh:21 + hh], in1=caus[:], op0=ALU.mult, op1=ALU.mult)))(hh), [bsmB, gtsB, constB], [PmlB])
                HM = T[1]
                bnum = []
                for pair in range(2):
                    bn2, bn2B = nb()
                    bnum.append((bn2, bn2B))
                    for hq in range(2):
                        hh = pair * 2 + hq
                        pe((lambda hh, hq, bn2: (lambda e: e.matmul(bn2[:, hq * 130:hq * 130 + 130], lhsT=Pml[:, hh, :], rhs=vaug[:, hh, :], start=True, stop=False)))(hh, hq, bn2), [PmlB, vaugB], [bn2B])
                        pe((lambda hh, hq, bn2: (lambda e: e.matmul(bn2[:, hq * 130:hq * 130 + 130], lhsT=qTm[:, hh, :], rhs=Cbf[:, hh, :], start=False, stop=True)))(hh, hq, bn2), [qkB, CB_], [bn2B])
                bcs, bcsB = nb()
                for hh in range(4):
                    pe((lambda hh: (lambda e: e.matmul(bcs[0:64, hh * 128:hh * 128 + 128], lhsT=ktok[:, hh, :], rhs=vaug[:, hh, 0:128], start=True, stop=True)))(hh), [ktokB, vaugB], [bcsB])
                bcn, bcnB = nb()
                for hh in range(4):
                    pe((lambda hh: (lambda e: e.matmul(bcn[0:64, hh * 2:hh * 2 + 2], lhsT=ktok[:, hh, :], rhs=vaug[:, hh, 128:130], start=True, stop=True)))(hh), [ktokB, vaugB], [bcnB])
                for hh in range(4):
                    dve((lambda hh: (lambda e: e.scalar_tensor_tensor(out=C32[:, hh, 0:128], in0=C32[:, hh, 0:128], scalar=gts[0:64, 36 + hh:37 + hh], in1=bcs[0:64, hh * 128:(hh + 1) * 128], op0=ALU.mult, op1=ALU.add)))(hh), [bcsB, gtsB], [CB_])
                    dve((lambda hh: (lambda e: e.scalar_tensor_tensor(out=C32[:, hh, 128:130], in0=C32[:, hh, 128:130], scalar=gts[0:64, 36 + hh:37 + hh], in1=bcn[0:64, hh * 2:hh * 2 + 2], op0=ALU.mult, op1=ALU.add)))(hh), [bcnB, gtsB], [CB_])
                for pair in range(2):
                    bn2, bn2B = bnum[pair]
                    for hq in range(2):
                        hh = pair * 2 + hq
                        c0 = hq * 130
                        dve((lambda hh, c0, bn2: (lambda e: e.tensor_scalar(out=gts[:, 52 + hh:53 + hh], in0=bn2[:, c0 + 128:c0 + 129], scalar1=gts[:, 24 + hh:25 + hh], scalar2=None, op0=ALU.mult)))(hh, c0, bn2), [bn2B], [gtsB])
                        dve((lambda hh: (lambda e: e.scalar_tensor_tensor(out=gts[:, 40 + hh:41 + hh], in0=gts[:, 52 + hh:53 + hh], scalar=-1.0, in1=gts[:, 52 + hh:53 + hh], op0=ALU.mult, op1=ALU.max)))(hh), [], [gtsB])
                        dve((lambda hh: (lambda e: e.tensor_scalar(out=gts[:, 40 + hh:41 + hh], in0=gts[:, 40 + hh:41 + hh], scalar1=1.0, scalar2=None, op0=ALU.max)))(hh), [], [gtsB])
                        dve((lambda hh: (lambda e: e.reciprocal(out=gts[:, 44 + hh:45 + hh], in_=gts[:, 40 + hh:41 + hh])))(hh), [], [gtsB])
                        dve((lambda hh: (lambda e: e.tensor_tensor(out=gts[:, 44 + hh:45 + hh], in0=gts[:, 44 + hh:45 + hh], in1=gts[:, 24 + hh:25 + hh], op=ALU.mult)))(hh), [], [gtsB])
                        dve((lambda hh, c0, bn2: (lambda e: e.tensor_scalar(out=HM[:, hh * 128:(hh + 1) * 128], in0=bn2[:, c0:c0 + 128], scalar1=gts[:, 44 + hh:45 + hh], scalar2=None, op0=ALU.mult)))(hh, c0, bn2), [bn2B, gtsB], [TB[1]])
                        act((lambda hh: (lambda e: e.activation(out=T[2][:, hh * 128:(hh + 1) * 128], in_=HM[:, hh * 128:(hh + 1) * 128], func=AF.Square, accum_out=gts[:, 48 + hh:49 + hh])))(hh), [TB[1]], [TB[2], gtsB])
                act(lambda e: e.activation(out=Cbf[:], in_=C32[:], func=AF.Copy), [], [CB_])
                dve(lambda e: e.tensor_scalar(out=gts[:, 48:52], in0=gts[:, 48:52], scalar1=1.0 / 128, scalar2=EPS, op0=ALU.mult, op1=ALU.add), [], [gtsB])
                act(lambda e: e.activation(out=gts[:, 48:52], in_=gts[:, 48:52], func=AF.Ln), [], [gtsB])
                act(lambda e: e.activation(out=gts[:, 48:52], in_=gts[:, 48:52], func=AF.Exp, scale=-0.5), [], [gtsB])
                for hh in range(4):
                    dve((lambda hh: (lambda e: e.scalar_tensor_tensor(out=yml[:, hh * 128:(hh + 1) * 128], in0=HM[:, hh * 128:(hh + 1) * 128], scalar=gts[:, 48 + hh:49 + hh], in1=mosg[:, hh * 128:(hh + 1) * 128], op0=ALU.mult, op1=ALU.mult)))(hh), [TB[1], gtsB, mosgB], [ymlTokB])
                bty, btyB = nb()
                btyb = bty[:].bitcast(BF16)
                for hh in range(4):
                    pe((lambda hh: (lambda e: e.transpose(out=btyb[:, hh * 128:(hh + 1) * 128], in_=yml[:, hh * 128:(hh + 1) * 128], identity=identb[:])))(hh), [ymlTokB, constB], [btyB])
                for hh in range(4):
                    dve((lambda hh: (lambda e: e.tensor_scalar(out=ymlT[:, hh, :], in0=btyb[:, hh * 128:(hh + 1) * 128], scalar1=pv[:, pvl + PV_MNG + hh: pvl + PV_MNG + hh + 1], scalar2=None, op0=ALU.mult)))(hh), [btyB, pvB], [ymlB])

                for (c0, SG) in ((C_GA, SGA), (C_GB, SGB)):
                    for half in range(2):
                        bgx, bgxB = nb()
                        proj_fm(c0 + half * 512, 4, bgx, bgxB)
                        act((lambda SG, half, bgx: (lambda e: e.activation(out=SG[:, half * 4:(half + 1) * 4, :].rearrange("p k t -> p (k t)"), in_=bgx[:], func=AF.Sigmoid)))(SG, half, bgx), [bgxB], [sgB, sgB2])
                for half in range(2):
                    bph, bphB = nb()
                    bpm, bpmB = nb()
                    for jj in range(4):
                        j = half * 4 + jj
                        for k in range(4):
                            pe((lambda j, jj, k, bph: (lambda e: e.matmul(bph[:, jj * 128:(jj + 1) * 128], lhsT=Wbh[:, k, j * 128:(j + 1) * 128], rhs=yhgT[:, k, :], start=(k == 0), stop=(k == 3))))(j, jj, k, bph), [WB[1], yhgB], [bphB])
                        for k in range(4):
                            pe((lambda j, jj, k, bpm: (lambda e: e.matmul(bpm[:, jj * 128:(jj + 1) * 128], lhsT=Wbm[:, k, j * 128:(j + 1) * 128], rhs=ymlT[:, k, :], start=(k == 0), stop=(k == 3))))(j, jj, k, bpm), [WB[1], ymlB], [bpmB])
                    dve((lambda half, bph: (lambda e: e.tensor_tensor(out=T[3][:], in0=bph[:], in1=SGA[:, half * 4:(half + 1) * 4, :].rearrange("p k t -> p (k t)"), op=ALU.mult)))(half, bph), [bphB, sgB, sgB2], [TB[3]])
                    dve((lambda half, bpm: (lambda e: e.tensor_tensor(out=T[5][:], in0=bpm[:], in1=SGB[:, half * 4:(half + 1) * 4, :].rearrange("p k t -> p (k t)"), op=ALU.mult)))(half, bpm), [bpmB, sgB, sgB2], [TB[5]])
                    pool((lambda half: (lambda e: e.tensor_tensor(out=mrg[:, half * 4:(half + 1) * 4, :].rearrange("p k t -> p (k t)"), in0=T[3][:], in1=T[5][:], op=ALU.add)))(half), [TB[3], TB[5]], [mrgB])
                ob0, ob0B = banks[6], bankB[6]
                ob1, ob1B = banks[7], bankB[7]
                for half, ob, obb in ((0, ob0, ob0B), (1, ob1, ob1B)):
                    for k in range(8):
                        pe((lambda half, k, ob: (lambda e: e.matmul(ob[:], lhsT=mrg[:, k, :], rhs=Wo[:, k, half * 512:(half + 1) * 512], start=(k == 0), stop=(k == 7))))(half, k, ob), [mrgB, WB[1]], [obb])
                residual(slot, [ob0, ob1], [ob0B, ob1B])
                store_x(ti, slot, xdB[ti])

        if "A" in phases:
            phase_mixer(0, x_d)
        srcB = out_d if "A" in phases else x_d
        if "B" in phases:
            phase_dense(0, srcB)
        srcC = out_d if ("A" in phases or "B" in phases) else x_d
        if "C" in phases:
            phase_mixer(1, srcC)
        srcD = out_d if any(p in phases for p in "ABC") else x_d
        if "D" in phases:
            if SPARSE_MOE:
                phase_moe_sparse(1, srcD)
            else:
                phase_moe(1, srcD)
        P.final_wait("sp", xdB)
        P.final_wait("pool", dbgB)
        P.emit()
    return nc, P


def _consts():
    cst = np.zeros((128, 1024), np.float32)
    cst[:, 0:128] = np.eye(128, dtype=np.float32)
    s = np.arange(128)[:, None]
    t = np.arange(128)[None, :]
    cst[:, 128:256] = (s <= t)
    cst[:, 256:384] = ((s // 64) == (t // 64))
    cst[:, 384:512] = (s <= t)
    cst[:, 512:576] = ((s % 64) <= np.arange(64)[None, :])
    cst[:, 576:704] = (s < t)
    rst = np.ones((128, 512), np.float32)
    rst[:, 0::64] = 0.0
    return cst, rst


def _fm(v, k):
    return np.ascontiguousarray(np.asarray(v, np.float32).reshape(k, 128).T)


def _pack_pv(inp):
    pv = np.zeros((128, DEPTH * PVL), np.float32)
    for l in range(DEPTH):
        o = l * PVL
        pv[:, o + PV_NMG:o + PV_NMG + 8] = _fm(inp["norm_mix_g"][l], 8)
        pv[:, o + PV_AMB:o + PV_AMB + 24] = _fm(inp["ada_mix_b"][l], 24)
        pv[:, o + PV_NFG:o + PV_NFG + 8] = _fm(inp["norm_ffn_g"][l], 8)
        pv[:, o + PV_AFB:o + PV_AFB + 24] = _fm(inp["ada_ffn_b"][l], 24)
        pv[:, o + PV_LB:o + PV_LB + 4] = _fm(inp["hg_lb_logits"][l], 4)
        pv[:, o + PV_HNG:o + PV_HNG + 4] = _fm(inp["hg_norm_g"][l], 4)
        cw = np.asarray(inp["ml_conv_w"][l], np.float32)
        for j in range(4):
            pv[:, o + PV_CW + j * 4:o + PV_CW + j * 4 + 4] = _fm(cw[j], 4)
        pv[:, o + PV_CB:o + PV_CB + 4] = _fm(inp["ml_conv_b"][l], 4)
        pv[:, o + PV_MNG:o + PV_MNG + 4] = _fm(inp["ml_norm_g"][l], 4)
        pv[:, o + PV_FB:o + PV_FB + 4] = np.broadcast_to(np.asarray(inp["ml_fbias"][l], np.float32)[None, :], (128, 4))
    pv[:, PVL + PV_RB:PVL + PV_RB + NE] = np.broadcast_to(np.asarray(inp["moe_router_b"][0], np.float32)[None, :], (128, NE))
    return pv


_CACHE = {}
LAST_CNT = None


def run(inputs, n_cores, NSEQ, S, phases="ABCD", debug=False):
    key = (NSEQ, S, phases, debug)
    if key not in _CACHE:
        _CACHE[key] = build(NSEQ, S, phases, debug)[0]
    nc = _CACHE[key]
    f32 = lambda a: np.ascontiguousarray(np.asarray(a, np.float32))
    x = f32(inputs["x"]); c = f32(inputs["c"])
    cst, rst = _consts()
    pv = _pack_pv(inputs)
    gbias = np.stack([np.broadcast_to(f32(inputs[nm])[l][2 * D:3 * D][None, :], (128, D))
                      for l in range(DEPTH) for nm in ("ada_mix_b", "ada_ffn_b")]).astype(np.float32)
    fng = np.ascontiguousarray(np.broadcast_to(f32(inputs["final_norm_g"])[None, :], (128, D)))
    shared = {"pv": pv, "cst": cst, "rst": rst, "gbias": np.ascontiguousarray(gbias), "fng": fng}
    for nm in ("ada_mix_w", "ada_ffn_w", "w_in", "ml_wq", "ml_wk", "w_br_hg", "w_br_ml", "w_out", "ffn_w1", "ffn_w3", "ffn_w2",
               "moe_router_w", "moe_w1", "moe_w3", "moe_w2"):
        shared[nm] = f32(inputs[nm])
    in_maps = []
    for i in range(n_cores):
        xs = x[i * NSEQ:(i + 1) * NSEQ].reshape(NSEQ * S, D)
        cs = c[i * NSEQ:(i + 1) * NSEQ]
        cTl = np.ascontiguousarray(cs.reshape(NSEQ, 8, 128).transpose(2, 1, 0))
        m = dict(shared)
        m["x"] = np.ascontiguousarray(xs)
        m["cT"] = cTl
        in_maps.append(m)
    res = run_bass_kernel_spmd(nc, in_maps, core_ids=list(range(n_cores)))
    outs = [r["out"].reshape(NSEQ, S, D) for r in res.results]
    global LAST_CNT
    LAST_CNT = [r["cnt"][0] for r in res.results]
    if debug:
        return np.concatenate(outs, axis=0), res.results[0]
    return np.concatenate(outs, axis=0)


def kernel(**inputs):
    B, S, _ = inputs["x"].shape
    return run(inputs, N_CORES, B // N_CORES, S).astype(np.float32)
```

```python
import contextlib
import numpy as np
import concourse.bass as bass
import concourse.mybir as mybir
from concourse.bass_utils import run_bass_kernel_spmd

F32 = mybir.dt.float32
BF16 = mybir.dt.bfloat16
AF = mybir.ActivationFunctionType
ALU = mybir.AluOpType
AX = mybir.AxisListType

D = 1024
DEPTH = 2
INC = 5640
C_Q, C_F, C_I, C_G, C_MU, C_MV, C_MO, C_MI, C_MF, C_GA, C_GB = 0, 512, 1024, 1536, 2048, 2560, 3072, 3584, 3588, 3592, 4616
DFF = 2816
NE = 8
DFE = 1408
EPS = 1e-6
N_CORES = 8
PE_DRAIN = False
SAME_ENG_DIST = 1 << 30
SPARSE_MOE = True

PV_NMG, PV_AMB, PV_NFG, PV_AFB, PV_LB, PV_HNG, PV_CW, PV_CB, PV_MNG, PV_FB, PV_RB = 0, 8, 32, 40, 64, 68, 72, 88, 92, 96, 100
PVL = 108


class Buf:
    __slots__ = ("name", "w", "r")

    def __init__(self, name):
        self.name = name
        self.w = []
        self.r = []


class _Rec:
    def __init__(self):
        self.call = None

    def __getattr__(self, name):
        def f(*a, **k):
            self.call = (name, a, k)
            return self
        return f


def _record(fn):
    r = _Rec()
    fn(r)
    return r.call


class Prog:
    ENGS = ("pe", "act", "dve", "pool", "sp")

    def __init__(self, nc, n_dma_sems=8):
        self.nc = nc
        self.q = {e: [] for e in self.ENGS}
        self.cnt = {e: 0 for e in self.ENGS}
        self.waited = {e: {} for e in self.ENGS}
        self.n_dma_sems = n_dma_sems
        self.dma_rr = {"sp": 0, "pool": 0, "act": 0}
        self.dma_cnt = {}
        self.n_ins = 0

    def _need(self, eng, tok, waits):
        if tok is None:
            return
        key, val = tok
        if key == ("e", eng) and eng == "pe":
            return
        if key == ("e", eng) and (self.cnt[eng] + 1 - val) > SAME_ENG_DIST:
            return
        if self.waited[eng].get(key, 0) >= val:
            return
        self.waited[eng][key] = val
        waits.append((key, val))

    def _deps(self, eng, reads, writes):
        waits = []
        for b in reads:
            for t in b.w:
                self._need(eng, t, waits)
        for b in writes:
            for t in b.w:
                self._need(eng, t, waits)
            for t in b.r:
                self._need(eng, t, waits)
        return waits

    def _commit(self, tok, reads, writes):
        for b in reads:
            b.r.append(tok)
        for b in writes:
            b.w = [tok]
            b.r = []

    def op(self, eng, fn, reads=(), writes=()):
        waits = self._deps(eng, reads, writes)
        self.cnt[eng] += 1
        tok = (("e", eng), self.cnt[eng])
        self.q[eng].append((waits, _record(fn), ("e", eng), 1))
        self._commit(tok, reads, writes)
        self.n_ins += 1
        return tok

    def dma(self, qeng, fn, reads=(), writes=(), group=None):
        if group is not None:
            waits = []
            for t in group["deps"]:
                self._need(qeng, t, waits)
        else:
            waits = self._deps(qeng, reads, writes)
        k = self.dma_rr[qeng]
        self.dma_rr[qeng] = (k + 1) % self.n_dma_sems
        key = ("d", qeng, k)
        prev = self.dma_cnt.get(key, 0)
        if prev:
            self._need(qeng, (key, prev), waits)
        val = prev + 16
        self.dma_cnt[key] = val
        tok = (key, val)
        self.q[qeng].append((waits, _record(fn), key, 16))
        if group is not None:
            group["toks"].append(tok)
        else:
            self._commit(tok, reads, writes)
        self.n_ins += 1
        return tok

    def group_begin(self, bufs):
        deps = []
        for b in bufs:
            deps += list(b.w) + list(b.r)
        return {"deps": deps, "toks": [], "bufs": bufs}

    def group_end(self, g):
        for b in g["bufs"]:
            b.w = list(g["toks"])
            b.r = []

    def final_wait(self, eng, bufs):
        waits = []
        for b in bufs:
            for t in b.w:
                self._need(eng, t, waits)
        self.q[eng].append((waits, None, None, 0))

    def emit(self):
        nc = self.nc
        with contextlib.ExitStack() as st:
            sems = {}
            for e in self.ENGS:
                sems[("e", e)] = st.enter_context(nc.semaphore("s_" + e))
            for key in self.dma_cnt:
                sems[key] = st.enter_context(nc.semaphore("d_%s_%d" % (key[1], key[2])))
            block = st.enter_context(nc.Block())

            def run(eh, lst, drain=False):
                for waits, fn, skey, inc in lst:
                    for key, val in waits:
                        eh.wait_ge(sems[key], val)
                    if drain and waits:
                        eh.drain()
                    if fn is not None:
                        name, a, k = fn
                        getattr(eh, name)(*a, **k).then_inc(sems[skey], inc)

            block.tensor(lambda t: run(t, self.q["pe"], PE_DRAIN))
            block.scalar(lambda t: run(t, self.q["act"]))
            block.vector(lambda t: run(t, self.q["dve"]))
            block.gpsimd(lambda t: run(t, self.q["pool"]))
            block.sync(lambda t: run(t, self.q["sp"]))


def build(NSEQ, S, phases="ABCD", debug=False):
    NT = S // 128
    NTILES = NSEQ * NT
    nc = bass.Bass("TRN2", target_bir_lowering=False)
    dt_in = lambda name, shape: nc.dram_tensor(name, list(shape), F32, kind="ExternalInput").ap()
    x_d = dt_in("x", [NSEQ * S, D])
    cT_d = dt_in("cT", [128, 8, NSEQ])
    pv_d = dt_in("pv", [128, DEPTH * PVL])
    cst_d = dt_in("cst", [128, 1024])
    rst_d = dt_in("rst", [128, 512])
    gb_d = dt_in("gbias", [2 * DEPTH, 128, D])
    fng_d = dt_in("fng", [128, D])
    ada_mix_w = dt_in("ada_mix_w", [DEPTH, D, 3 * D])
    ada_ffn_w = dt_in("ada_ffn_w", [DEPTH, D, 3 * D])
    w_in = dt_in("w_in", [DEPTH, D, INC])
    ml_wq = dt_in("ml_wq", [DEPTH, 4, 128, 64])
    ml_wk = dt_in("ml_wk", [DEPTH, 4, 128, 64])
    w_br_hg = dt_in("w_br_hg", [DEPTH, 512, D])
    w_br_ml = dt_in("w_br_ml", [DEPTH, 512, D])
    w_out = dt_in("w_out", [DEPTH, D, D])
    ffn_w1 = dt_in("ffn_w1", [1, D, DFF])
    ffn_w3 = dt_in("ffn_w3", [1, D, DFF])
    ffn_w2 = dt_in("ffn_w2", [1, DFF, D])
    moe_rw = dt_in("moe_router_w", [1, D, NE])
    moe_w1 = dt_in("moe_w1", [1, NE, D, DFE])
    moe_w3 = dt_in("moe_w3", [1, NE, D, DFE])
    moe_w2 = dt_in("moe_w2", [1, NE, DFE, D])
    out_d = nc.dram_tensor("out", [NSEQ * S, D], F32, kind="ExternalOutput").ap()
    cnt_d = nc.dram_tensor("cnt", [128, NE], F32, kind="ExternalOutput").ap()
    hsc_d = nc.dram_tensor("hsc", [NTILES, 128, 8 * 128], BF16, kind="Internal").ap()
    csc_d = nc.dram_tensor("csc", [NTILES, 128, NE], F32, kind="Internal").ap()
    CAP = max(128, ((NSEQ * S * 2 // NE) * 15 // 8 + 127) // 128 * 128)
    hg_d = nc.dram_tensor("hg", [NE * CAP, D], BF16, kind="Internal").ap()
    y_d = nc.dram_tensor("yex", [NE * CAP, D], F32, kind="Internal").ap()

    dbg = {}
    if debug:
        for nm, shp in (("dbg_hT", [128, 1024]), ("dbg_mod", [128, 16]), ("dbg_gate", [128, 1024]), ("dbg_act", [128, 1408]), ("dbg_xn", [128, 1024]), ("dbg_ob", [128, 1024])):
            dbg[nm] = nc.dram_tensor(nm, shp, F32, kind="ExternalOutput").ap()
    dbgB = []
    P = Prog(nc)
    st = contextlib.ExitStack()
    with st:
        def sb(name, shape, dt=F32):
            return st.enter_context(nc.sbuf_tensor("sb_" + name, list(shape), dt))

        ARENA_N = 67584
        arena = sb("arena", [128, ARENA_N], BF16)
        slotB = [Buf("slot0"), Buf("slot1")]
        X = [sb("X0", [128, D]), sb("X1", [128, D])]
        XB = [Buf("X0"), Buf("X1")]
        xn = sb("xn", [128, D], BF16); xnB = Buf("xn")
        hT = sb("hT", [128, 8, 128], BF16); hTB = Buf("hT")
        cst = sb("cst", [128, 1024]); cstB = Buf("cst")
        identb = sb("identb", [128, 128], BF16)
        trib = sb("trib", [128, 128], BF16)
        bonesb = sb("bonesb", [128, 128], BF16)
        onesb = sb("onesb", [128, 128], BF16)
        caus = sb("caus", [128, 128], BF16)
        m64 = sb("m64", [128, 64])
        rst = sb("rst", [128, 512], BF16);
        pv = sb("pv", [128, DEPTH * PVL]); pvB = Buf("pv")
        lbc = sb("lbc", [128, 2, 4]); lbB = Buf("lbc")
        cT = sb("cT", [128, 8, NSEQ])
        cact = sb("cact", [128, 8, NSEQ], BF16)
        cbc = sb("cbc", [128, 8, 128], BF16); cbcB = Buf("cbc")
        MS = sb("MS", [128, NSEQ, 8]); SH = sb("SH", [128, NSEQ, 8]); modB = Buf("mod")
        GATEb = sb("GATEb", [128, D]); gateB = Buf("gate")
        small = sb("small", [128, 64]);
        T = [sb("T%d" % i, [128, 512]) for i in range(6)]
        TB = [Buf("T%d" % i) for i in range(6)]
        constB = Buf("consts")

        psum_all = st.enter_context(nc.psum_tensor("psum_all", [128, 4096], F32))
        banks = [psum_all[:, i * 512:(i + 1) * 512] for i in range(8)]
        bankB = [Buf("bank%d" % i) for i in range(8)]
        bank_rr = [0]
        bank_n = [6]

        def nb():
            i = bank_rr[0] % bank_n[0]
            bank_rr[0] = (i + 1) % bank_n[0]
            return banks[i], bankB[i]

        def dve(fn, r=(), w=()):
            return P.op("dve", fn, r, w)

        def act(fn, r=(), w=()):
            return P.op("act", fn, r, w)

        def pe(fn, r=(), w=()):
            return P.op("pe", fn, r, w)

        def pool(fn, r=(), w=()):
            return P.op("pool", fn, r, w)

        P.dma("sp", lambda e: e.dma_start(out=cst[:], in_=cst_d), writes=[cstB])
        P.dma("pool", lambda e: e.dma_start(out=rst[:], in_=rst_d), writes=[constB])
        P.dma("sp", lambda e: e.dma_start(out=pv[:], in_=pv_d), writes=[pvB])
        P.dma("sp", lambda e: e.dma_start(out=cT[:], in_=cT_d), writes=[modB])
        dve(lambda e: e.tensor_copy(out=identb[:], in_=cst[:, 0:128]), [cstB], [constB])
        dve(lambda e: e.tensor_copy(out=trib[:], in_=cst[:, 128:256]), [cstB], [constB])
        dve(lambda e: e.tensor_copy(out=bonesb[:], in_=cst[:, 256:384]), [cstB], [constB])
        dve(lambda e: e.tensor_copy(out=caus[:], in_=cst[:, 384:512]), [cstB], [constB])
        dve(lambda e: e.tensor_copy(out=m64[:], in_=cst[:, 512:576]), [cstB], [constB])
        dve(lambda e: e.memset(onesb[:], 1.0), [], [constB])
        strilb = sb("strilb", [128, 128], BF16)
        dve(lambda e: e.tensor_copy(out=strilb[:], in_=cst[:, 576:704]), [cstB], [constB])
        act(lambda e: e.activation(out=cact[:], in_=cT[:], func=AF.Silu), [modB], [modB])

        def wload(dst_ap, src_ap, grp, ncols):
            K = dst_ap.shape[1]
            for k in range(K):
                for c0 in range(0, ncols, 1024):
                    c1 = min(ncols, c0 + 1024)
                    P.dma("pool", (lambda k, c0, c1: (lambda e: e.dma_start(out=dst_ap[:, k, c0:c1], in_=src_ap[:, k, c0:c1])))(k, c0, c1),
                          group=grp)

        def kview(w2d):
            return w2d.rearrange("(k p) n -> p k n", p=128)

        def ada_setup(ada_w_l, l, which, norm_g_col, ada_b_col, gb_row):
            aw = arena[:, 33792:33792 + 8 * 3072].rearrange("p (k n) -> p k n", k=8)
            g = P.group_begin([slotB[1]])
            wload(aw, kview(ada_w_l), g, 3072)
            P.group_end(g)
            bk, bb = nb()
            for j in range(16):
                for k in range(8):
                    pe((lambda j, k: (lambda e: e.matmul(bk[:, j * NSEQ:(j + 1) * NSEQ], lhsT=aw[:, k, j * 128:(j + 1) * 128], rhs=cact[:, k, :], start=(k == 0), stop=(k == 7))))(j, k),
                       [slotB[1], modB], [bb])
            bv = bk[:, 0:16 * NSEQ].rearrange("p (j s) -> p j s", s=NSEQ)
            for s in range(NSEQ):
                dve((lambda s: (lambda e: e.tensor_tensor(out=SH[:, s, :], in0=bv[:, 0:8, s], in1=pv[:, l * PVL + ada_b_col: l * PVL + ada_b_col + 8], op=ALU.add)))(s), [bb, pvB], [modB])
                dve((lambda s: (lambda e: e.tensor_tensor(out=MS[:, s, :], in0=bv[:, 8:16, s], in1=pv[:, l * PVL + ada_b_col + 8: l * PVL + ada_b_col + 16], op=ALU.add)))(s), [bb, pvB], [modB])
                dve((lambda s: (lambda e: e.scalar_tensor_tensor(out=MS[:, s, :], in0=MS[:, s, :], scalar=1.0, in1=pv[:, l * PVL + norm_g_col: l * PVL + norm_g_col + 8], op0=ALU.add, op1=ALU.mult)))(s), [pvB], [modB])
            return aw

        def gate_setup(aw, s, gb_row):
            dve(lambda e: e.tensor_copy(out=cbc[:], in_=cact[:, :, s:s + 1].broadcast_to([128, 8, 128])), [modB], [cbcB])
            P.dma("sp", lambda e: e.dma_start(out=GATEb[:], in_=gb_d[gb_row]), writes=[gateB])
            for half in range(2):
                bk, bb = nb()
                for k in range(8):
                    pe((lambda k, half: (lambda e: e.matmul(bk[:], lhsT=cbc[:, k, :], rhs=aw[:, k, 2048 + half * 512: 2048 + (half + 1) * 512], start=(k == 0), stop=(k == 7))))(k, half),
                       [cbcB, slotB[1]], [bb])
                dve((lambda half, bk: (lambda e: e.tensor_tensor(out=GATEb[:, half * 512:(half + 1) * 512], in0=GATEb[:, half * 512:(half + 1) * 512], in1=bk[:], op=ALU.add)))(half, bk), [bb], [gateB])

        def load_x(ti, slot, src):
            P.dma("sp", lambda e: e.dma_start(out=X[slot][:], in_=src[ti * 128:(ti + 1) * 128, :]), writes=[XB[slot]])

        def norm_mod(slot, s):
            Xs, Xb = X[slot], XB[slot]
            act(lambda e: e.activation(out=xn[:], in_=Xs[:], func=AF.Square, accum_out=small[:, 0:1]), [Xb], [xnB])
            dve(lambda e: e.tensor_scalar(out=small[:, 1:2], in0=small[:, 0:1], scalar1=1.0 / D, scalar2=EPS, op0=ALU.mult, op1=ALU.add), [xnB], [xnB])
            act(lambda e: e.activation(out=small[:, 1:2], in_=small[:, 1:2], func=AF.Ln), [xnB], [xnB])
            act(lambda e: e.activation(out=small[:, 2:3], in_=small[:, 1:2], func=AF.Exp, scale=-0.5), [xnB], [xnB])
            act(lambda e: e.activation(out=xn[:], in_=Xs[:], func=AF.Copy, scale=small[:, 2:3]), [Xb, xnB], [xnB])
            bk, bb = nb()
            bkb = bk[:].bitcast(BF16)
            for k in range(8):
                pe((lambda k: (lambda e: e.transpose(out=bkb[:, k * 128:(k + 1) * 128], in_=xn[:, k * 128:(k + 1) * 128], identity=identb[:])))(k), [xnB, constB], [bb])
            bv = bkb[:, 0:1024].rearrange("p (k t) -> p k t", k=8)
            dve(lambda e: e.tensor_tensor(out=T[0][:].rearrange("p (k t) -> p k t", k=8)[:, :, 0:64], in0=bv[:, :, 0:64], in1=MS[:, s, :].unsqueeze(2).broadcast_to([128, 8, 64]), op=ALU.mult), [bb, modB], [TB[0]])
            dve(lambda e: e.tensor_tensor(out=T[1][:].rearrange("p (k t) -> p k t", k=8)[:, :, 0:64], in0=bv[:, :, 64:128], in1=MS[:, s, :].unsqueeze(2).broadcast_to([128, 8, 64]), op=ALU.mult), [bb, modB], [TB[1]])
            dve(lambda e: e.tensor_tensor(out=hT[:, :, 0:64], in0=T[0][:].rearrange("p (k t) -> p k t", k=8), in1=SH[:, s, :].unsqueeze(2).broadcast_to([128, 8, 64]), op=ALU.add), [TB[0], modB], [hTB])
            dve(lambda e: e.tensor_tensor(out=hT[:, :, 64:128], in0=T[1][:].rearrange("p (k t) -> p k t", k=8), in1=SH[:, s, :].unsqueeze(2).broadcast_to([128, 8, 64]), op=ALU.add), [TB[1], modB], [hTB])

        actT = sb("actT", [128, 11, 128], BF16); actTB = [Buf("actT_g%d" % i) for i in range(3)]
        actT_l = [actT, sb("actT1", [128, 11, 128], BF16)]; actTB_l = [actTB, [Buf("actT1_g%d" % i) for i in range(3)]]
        hT_l = [hT, sb("hT1", [128, 8, 128], BF16)]; hTB_l = [hTB, Buf("hT1")]
        ffn_grp = [0]

        def ffn_views(slot):
            base = slot * 33792
            w1 = arena[:, base:base + 8 * DFE].rearrange("p (k n) -> p k n", k=8)
            w3 = arena[:, base + 8 * DFE: base + 16 * DFE].rearrange("p (k n) -> p k n", k=8)
            w2 = arena[:, base + 16 * DFE: base + 16 * DFE + 11 * D].rearrange("p (k n) -> p k n", k=11)
            return w1, w3, w2

        def ffn_load(slot, w1src, w3src, w2src):
            w1, w3, w2 = ffn_views(slot)
            g = P.group_begin([slotB[slot]])
            wload(w1, kview(w1src), g, DFE)
            wload(w3, kview(w3src), g, DFE)
            wload(w2, kview(w2src), g, D)
            P.group_end(g)

        def ffn_expert(slot, ob, obB, first, last):
            w1, w3, w2 = ffn_views(slot)
            aT, aTB = actT, actTB
            hTc, hTBc = hT, hTB

            def emit_w2_g(g0, nblk, gB):
                for jj in range(nblk):
                    j = g0 + jj
                    for half in range(2):
                        pe((lambda j, half: (lambda e: e.matmul(ob[half][:], lhsT=aT[:, j, :], rhs=w2[:, j, half * 512:(half + 1) * 512], start=(first and j == 0), stop=(last and j == 10))))(j, half),
                           [gB, slotB[slot]], [obB[half]])

            pending = None
            for g0 in range(0, 11, 4):
                nblk = min(4, 11 - g0)
                ffn_grp[0] += 1
                ts = 2 if ffn_grp[0] % 2 == 0 else 5
                b1, b1B = nb()
                b3, b3B = nb()
                for jj in range(nblk):
                    j = g0 + jj
                    for k in range(8):
                        pe((lambda j, jj, k: (lambda e: e.matmul(b1[:, jj * 128:(jj + 1) * 128], lhsT=w1[:, k, j * 128:(j + 1) * 128], rhs=hTc[:, k, :], start=(k == 0), stop=(k == 7))))(j, jj, k), [slotB[slot], hTBc], [b1B])
                    for k in range(8):
                        pe((lambda j, jj, k: (lambda e: e.matmul(b3[:, jj * 128:(jj + 1) * 128], lhsT=w3[:, k, j * 128:(j + 1) * 128], rhs=hTc[:, k, :], start=(k == 0), stop=(k == 7))))(j, jj, k), [slotB[slot], hTBc], [b3B])
                n = nblk * 128
                act((lambda n, b1: (lambda e: e.activation(out=T[ts][:, 0:n], in_=b1[:, 0:n], func=AF.Silu)))(n, b1), [b1B], [TB[ts]])
                gB = aTB[g0 // 4]
                dve((lambda n, b3, g0: (lambda e: e.tensor_tensor(out=aT[:, g0:g0 + n // 128, :].rearrange("p j t -> p (j t)"), in0=T[ts][:, 0:n], in1=b3[:, 0:n], op=ALU.mult)))(n, b3, g0), [TB[ts], b3B], [gB])
                if pending is not None:
                    emit_w2_g(*pending)
                pending = (g0, nblk, gB)
            emit_w2_g(*pending)

        def residual(slot, ob, obB, comb_ap=None, combB=None):
            Xs, Xb = X[slot], XB[slot]
            for half in range(2):
                sl = slice(half * 512, (half + 1) * 512)
                if comb_ap is None:
                    dve((lambda half, sl: (lambda e: e.tensor_tensor(out=T[3 + half][:], in0=ob[half][:], in1=GATEb[:, sl], op=ALU.mult)))(half, sl), [obB[half], gateB], [TB[3 + half]])
                else:
                    dve((lambda half, sl: (lambda e: e.scalar_tensor_tensor(out=T[3 + half][:], in0=ob[half][:], scalar=comb_ap, in1=GATEb[:, sl], op0=ALU.mult, op1=ALU.mult)))(half, sl), [obB[half], gateB, combB], [TB[3 + half]])
                pool((lambda half, sl: (lambda e: e.tensor_tensor(out=Xs[:, sl], in0=Xs[:, sl], in1=T[3 + half][:], op=ALU.add)))(half, sl), [TB[3 + half], Xb], [Xb])

        def store_x(ti, slot, dstB):
            P.dma("sp", lambda e: e.dma_start(out=out_d[ti * 128:(ti + 1) * 128, :], in_=X[slot][:]), reads=[XB[slot]], writes=[dstB])

        xdB = [Buf("xd%d" % i) for i in range(NTILES)]

        def phase_dense(l, src):
            aw = ada_setup(ada_ffn_w[l], l, "ffn", PV_NFG, PV_AFB, 2 * l + 1)
            gate_rows(aw, 2 * l + 1)
            ffn_load(0, ffn_w1[0][:, 0:DFE], ffn_w3[0][:, 0:DFE], ffn_w2[0][0:DFE, :])
            ffn_load(1, ffn_w1[0][:, DFE:DFF], ffn_w3[0][:, DFE:DFF], ffn_w2[0][DFE:DFF, :])
            bank_n[0] = 4
            load_x(0, 0, src)
            for ti in range(NTILES):
                s = ti // NT
                slot = ti % 2
                set_par(slot)
                if ti + 1 < NTILES:
                    load_x(ti + 1, (ti + 1) % 2, src)
                if ti % NT == 0:
                    gate_fetch(s)
                norm_mod(slot, s)
                ob0, ob0B = banks[4 + 2 * slot], bankB[4 + 2 * slot]
                ob1, ob1B = banks[5 + 2 * slot], bankB[5 + 2 * slot]
                ffn_expert(0, [ob0, ob1], [ob0B, ob1B], True, False)
                ffn_expert(1, [ob0, ob1], [ob0B, ob1B], False, True)
                residual(slot, [ob0, ob1], [ob0B, ob1B])
                store_x(ti, slot, xdB[ti])
            bank_n[0] = 6

        gsc_d = nc.dram_tensor("gsc", [NSEQ, 128, D], F32, kind="Internal").ap()
        gscB = [Buf("gsc%d" % s) for s in range(NSEQ)]

        def gate_rows(aw, gb_row):
            for s in range(NSEQ):
                gate_setup(aw, s, gb_row)
                P.dma("sp", (lambda s: (lambda e: e.dma_start(out=gsc_d[s], in_=GATEb[:])))(s), reads=[gateB], writes=[gscB[s]])

        def gate_fetch(s):
            P.dma("sp", lambda e: e.dma_start(out=GATEb[:], in_=gsc_d[s]), reads=[gscB[s]], writes=[gateB])

        def load_x_dep(ti, slot, src):
            if src is out_d:
                P.dma("sp", lambda e: e.dma_start(out=X[slot][:], in_=src[ti * 128:(ti + 1) * 128, :]), reads=[xdB[ti]], writes=[XB[slot]])
            else:
                P.dma("sp", lambda e: e.dma_start(out=X[slot][:], in_=src[ti * 128:(ti + 1) * 128, :]), writes=[XB[slot]])
        load_x = load_x_dep

        rw = sb("rw", [128, 8, 2 * NE], BF16); rwB = Buf("rw")
        rw32 = sb("rw32", [128, 8, NE])
        hlo = sb("hlo_mrg", [128, 8, 128], BF16); hloB = Buf("hlo")
        comb = sb("comb", [128, 4 * NE]); combB = Buf("comb")
        comb_l = [comb, sb("comb1", [128, 4 * NE])]; combB_l = [combB, Buf("comb1")]
        rt = sb("rt", [128, 64]); rtB = Buf("rt")
        basec = sb("basec", [128, NE])
        capmax = sb("capmax", [128, NE])
        Mb = sb("Mb", [128, NE], BF16)
        RT = sb("RT", [128, NTILES, 4])
        idxu = sb("idxu", [128, NTILES, 2], mybir.dt.uint32)

        def set_par(p):
            nonlocal hT, hTB, actT, actTB, comb, combB
            hT, hTB = hT_l[p], hTB_l[p]
            actT, actTB = actT_l[p], actTB_l[p]
            comb, combB = comb_l[p], combB_l[p]
        fng = cst; fngB = cstB
        hT32 = None

        def router(slot, s, ti):
            for hf in range(2):
                dve((lambda hf: (lambda e: e.tensor_tensor(out=T[hf][:].rearrange("p (k t) -> p k t", k=8), in0=T[hf][:].rearrange("p (k t) -> p k t", k=8), in1=SH[:, s, :].unsqueeze(2).broadcast_to([128, 8, 64]), op=ALU.add)))(hf), [modB, hTB], [TB[hf]])
                dve((lambda hf: (lambda e: e.tensor_tensor(out=hlo[:, :, hf * 64:(hf + 1) * 64], in0=T[hf][:].rearrange("p (k t) -> p k t", k=8), in1=hT[:, :, hf * 64:(hf + 1) * 64], op=ALU.subtract)))(hf), [TB[hf], hTB], [hloB])
            bk, bb = nb()
            n = 0
            for (lh, rc) in ((hT, 0), (hT, NE), (hlo, 0)):
                for k in range(8):
                    pe((lambda lh, rc, k, n: (lambda e: e.matmul(bk[:, 0:NE], lhsT=lh[:, k, :], rhs=rw[:, k, rc:rc + NE], start=(n == 0), stop=(n == 23))))(lh, rc, k, n), [hTB, hloB, rwB], [bb])
                    n += 1
            lg = comb[:, 8:16]
            dve(lambda e: e.tensor_tensor(out=lg, in0=bk[:, 0:NE], in1=pv[:, PVL + PV_RB: PVL + PV_RB + NE], op=ALU.add), [bb, pvB], [combB])
            dve(lambda e: e.max(out=comb[:, 16:24], in_=lg), [], [combB])
            dve(lambda e: e.tensor_scalar(out=comb[:, 24:32], in0=lg, scalar1=comb[:, 16:17], scalar2=None, op0=ALU.subtract), [], [combB])
            act(lambda e: e.activation(out=comb[:, 24:32], in_=comb[:, 24:32], func=AF.Exp), [combB], [combB])
            dve(lambda e: e.tensor_scalar(out=comb[:, 0:8], in0=lg, scalar1=comb[:, 17:18], scalar2=None, op0=ALU.is_ge), [combB], [combB])
            dve(lambda e: e.tensor_tensor(out=comb[:, 0:8], in0=comb[:, 0:8], in1=comb[:, 24:32], op=ALU.mult), [], [combB])
            dve(lambda e: e.reduce_sum(out=small[:, 4:5], in_=comb[:, 0:8], axis=AX.X), [], [combB])
            dve(lambda e: e.reciprocal(out=small[:, 5:6], in_=small[:, 4:5]), [], [combB])
            dve(lambda e: e.tensor_scalar(out=comb[:, 0:8], in0=comb[:, 0:8], scalar1=small[:, 5:6], scalar2=None, op0=ALU.mult), [], [combB])

        hscB = [Buf("hsc%d" % i) for i in range(NTILES)]
        cscB = [Buf("csc%d" % i) for i in range(NTILES)]
        outB = [Buf("out%d" % i) for i in range(NTILES)]

        def phase_moe(l, src):
            aw = ada_setup(ada_ffn_w[l], l, "ffn", PV_NFG, PV_AFB, 2 * l + 1)
            gate_rows(aw, 2 * l + 1)
            P.dma("sp", lambda e: e.dma_start(out=rw32[:], in_=kview(moe_rw[0])), writes=[rwB])
            dve(lambda e: e.tensor_copy(out=rw[:, :, 0:NE], in_=rw32[:]), [rwB], [rwB])
            dve(lambda e: e.tensor_tensor(out=rw32[:], in0=rw32[:], in1=rw[:, :, 0:NE], op=ALU.subtract), [], [rwB])
            dve(lambda e: e.tensor_copy(out=rw[:, :, NE:2 * NE], in_=rw32[:]), [], [rwB])
            P.dma("sp", lambda e: e.dma_start(out=fng[:], in_=fng_d), writes=[fngB])
            ffn_load(0, moe_w1[0, 0], moe_w3[0, 0], moe_w2[0, 0])
            bank_n[0] = 4
            units = [(ex, ti) for ex in range(NE) for ti in range(NTILES)]

            def loads(u):
                ex, ti = units[u]
                p = u % 2
                set_par(p)
                load_x(ti, p, src if ex == 0 else out_d)
                if ex > 0:
                    P.dma("sp", lambda e: e.dma_start(out=hT[:].rearrange("p k t -> p (k t)"), in_=hsc_d[ti]), reads=[hscB[ti]], writes=[hTB])
                    P.dma("sp", lambda e: e.dma_start(out=comb[:, 0:NE], in_=csc_d[ti]), reads=[cscB[ti]], writes=[combB])

            loads(0)
            for u, (ex, ti) in enumerate(units):
                slot_w = ex % 2
                if ti == 0 and ex + 1 < NE:
                    ffn_load((ex + 1) % 2, moe_w1[0, ex + 1], moe_w3[0, ex + 1], moe_w2[0, ex + 1])
                s = ti // NT
                slot = u % 2
                if u + 1 < len(units):
                    loads(u + 1)
                set_par(slot)
                if ti % NT == 0:
                    gate_fetch(s)
                if ex == 0:
                    norm_mod(slot, s)
                    router(slot, s, ti)
                    P.dma("sp", lambda e: e.dma_start(out=hsc_d[ti], in_=hT[:].rearrange("p k t -> p (k t)")), reads=[hTB], writes=[hscB[ti]])
                    P.dma("sp", lambda e: e.dma_start(out=csc_d[ti], in_=comb[:, 0:NE]), reads=[combB], writes=[cscB[ti]])
                ob0, ob0B = banks[4 + 2 * slot], bankB[4 + 2 * slot]
                ob1, ob1B = banks[5 + 2 * slot], bankB[5 + 2 * slot]
                ffn_expert(slot_w, [ob0, ob1], [ob0B, ob1B], True, True)
                residual(slot, [ob0, ob1], [ob0B, ob1B], comb_ap=comb[:, ex:ex + 1], combB=combB)
                if ex < NE - 1:
                    store_x(ti, slot, xdB[ti])
                else:
                    final_norm(ti, slot)
            bank_n[0] = 6

        def phase_moe_sparse(l, src):
            from concourse.bass import IndirectOffsetOnAxis
            U32 = mybir.dt.uint32
            aw = ada_setup(ada_ffn_w[l], l, "ffn", PV_NFG, PV_AFB, 2 * l + 1)
            gate_rows(aw, 2 * l + 1)
            P.dma("sp", lambda e: e.dma_start(out=rw32[:], in_=kview(moe_rw[0])), writes=[rwB])
            dve(lambda e: e.tensor_copy(out=rw[:, :, 0:NE], in_=rw32[:]), [rwB], [rwB])
            dve(lambda e: e.tensor_tensor(out=rw32[:], in0=rw32[:], in1=rw[:, :, 0:NE], op=ALU.subtract), [], [rwB])
            dve(lambda e: e.tensor_copy(out=rw[:, :, NE:2 * NE], in_=rw32[:]), [], [rwB])
            ffn_load(0, moe_w1[0, 0], moe_w3[0, 0], moe_w2[0, 0])
            bank_n[0] = 4
            for e_ in range(NE):
                dve((lambda e_: (lambda e: e.memset(basec[:, e_:e_ + 1], float(e_ * CAP))))(e_), [], [rtB])
                dve((lambda e_: (lambda e: e.memset(capmax[:, e_:e_ + 1], float(e_ * CAP + CAP - 1))))(e_), [], [rtB])
            hgB = Buf("Hg")
            hg_grp = P.group_begin([hgB])
            load_x(0, 0, src)
            for ti in range(NTILES):
                s = ti // NT
                slot = ti % 2
                set_par(slot)
                if ti + 1 < NTILES:
                    load_x(ti + 1, (ti + 1) % 2, src)
                norm_mod(slot, s)
                for hf in range(2):
                    dve((lambda hf: (lambda e: e.tensor_tensor(out=T[hf][:].rearrange("p (k t) -> p k t", k=8), in0=T[hf][:].rearrange("p (k t) -> p k t", k=8), in1=SH[:, s, :].unsqueeze(2).broadcast_to([128, 8, 64]), op=ALU.add)))(hf), [modB, hTB], [TB[hf]])
                    dve((lambda hf: (lambda e: e.tensor_tensor(out=hlo[:, :, hf * 64:(hf + 1) * 64], in0=T[hf][:].rearrange("p (k t) -> p k t", k=8), in1=hT[:, :, hf * 64:(hf + 1) * 64], op=ALU.subtract)))(hf), [TB[hf], hTB], [hloB])
                bk, bb = nb()
                n = 0
                for (lh, rc) in ((hT, 0), (hT, NE), (hlo, 0)):
                    for k in range(8):
                        pe((lambda lh, rc, k, n: (lambda e: e.matmul(bk[:, 0:NE], lhsT=lh[:, k, :], rhs=rw[:, k, rc:rc + NE], start=(n == 0), stop=(n == 23))))(lh, rc, k, n), [hTB, hloB, rwB], [bb])
                        n += 1
                lg = rt[:, 48:56]
                dve(lambda e: e.tensor_tensor(out=lg, in0=bk[:, 0:NE], in1=pv[:, PVL + PV_RB: PVL + PV_RB + NE], op=ALU.add), [bb, pvB], [rtB])
                dve(lambda e: e.max(out=rt[:, 0:8], in_=lg), [], [rtB])
                dve(lambda e: e.tensor_scalar(out=rt[:, 8:16], in0=lg, scalar1=rt[:, 0:1], scalar2=None, op0=ALU.is_equal), [], [rtB])
                dve(lambda e: e.tensor_scalar(out=rt[:, 16:24], in0=lg, scalar1=rt[:, 1:2], scalar2=None, op0=ALU.is_equal), [], [rtB])
                dve(lambda e: e.tensor_tensor(out=Mb[:], in0=rt[:, 8:16], in1=rt[:, 16:24], op=ALU.add), [], [rtB])
                bp, bpB = nb()
                pe(lambda e: e.matmul(bp[:, 0:8], lhsT=strilb[:], rhs=Mb[:], start=True, stop=True), [rtB, constB], [bpB])
                pe(lambda e: e.matmul(bp[:, 8:16], lhsT=onesb[:], rhs=Mb[:], start=True, stop=True), [rtB, constB], [bpB])
                dve(lambda e: e.tensor_tensor(out=rt[:, 24:32], in0=bp[:, 0:8], in1=basec[:], op=ALU.add), [bpB], [rtB])
                dve(lambda e: e.tensor_tensor(out=rt[:, 24:32], in0=rt[:, 24:32], in1=capmax[:], op=ALU.min), [], [rtB])
                dve(lambda e: e.tensor_tensor(out=basec[:], in0=basec[:], in1=bp[:, 8:16], op=ALU.add), [bpB], [rtB])
                dve(lambda e: e.tensor_tensor(out=rt[:, 32:40], in0=rt[:, 8:16], in1=rt[:, 24:32], op=ALU.mult), [], [rtB])
                dve(lambda e: e.reduce_sum(out=RT[:, ti, 0:1], in_=rt[:, 32:40], axis=AX.X), [], [rtB])
                dve(lambda e: e.tensor_tensor(out=rt[:, 32:40], in0=rt[:, 16:24], in1=rt[:, 24:32], op=ALU.mult), [], [rtB])
                dve(lambda e: e.reduce_sum(out=RT[:, ti, 1:2], in_=rt[:, 32:40], axis=AX.X), [], [rtB])
                dve(lambda e: e.tensor_copy(out=idxu[:, ti, :], in_=RT[:, ti, 0:2]), [], [rtB])
                dve(lambda e: e.tensor_tensor(out=rt[:, 40:41], in0=rt[:, 1:2], in1=rt[:, 0:1], op=ALU.subtract), [], [rtB])
                act(lambda e: e.activation(out=rt[:, 41:42], in_=rt[:, 40:41], func=AF.Exp), [rtB], [rtB])
                dve(lambda e: e.tensor_scalar(out=rt[:, 42:43], in0=rt[:, 41:42], scalar1=1.0, scalar2=None, op0=ALU.add), [], [rtB])
                dve(lambda e: e.reciprocal(out=RT[:, ti, 2:3], in_=rt[:, 42:43]), [], [rtB])
                dve(lambda e: e.tensor_tensor(out=RT[:, ti, 3:4], in0=rt[:, 41:42], in1=RT[:, ti, 2:3], op=ALU.mult), [], [rtB])
                bt, btB = nb()
                btb = bt[:].bitcast(BF16)
                for k in range(8):
                    pe((lambda k: (lambda e: e.transpose(out=btb[:, k * 128:(k + 1) * 128], in_=hT[:, k, :], identity=identb[:])))(k), [hTB, constB], [btB])
                act(lambda e: e.activation(out=xn[:], in_=btb[:, 0:1024], func=AF.Copy), [btB], [xnB])
                for k in range(2):
                    P.dma("pool", (lambda k: (lambda e: e.indirect_dma_start(out=hg_d, out_offset=IndirectOffsetOnAxis(ap=idxu[:, ti, k:k + 1], axis=0), in_=xn[:], in_offset=None)))(k), group=hg_grp)
                    for t_ in list(xnB.w) + list(rtB.w):
                        P._need("pool", t_, P.q["pool"][-1][0])
                    xnB.r.append(hg_grp["toks"][-1])
                    rtB.r.append(hg_grp["toks"][-1])
            P.group_end(hg_grp)
            cntB = Buf("cnt"); dbgB.append(cntB)
            P.dma("sp", lambda e: e.dma_start(out=cnt_d, in_=basec[:]), reads=[rtB], writes=[cntB])
            yB = Buf("Y")
            y_grp = P.group_begin([yB])
            units = [(ex, t) for ex in range(NE) for t in range(CAP // 128)]
            stage = [xn[:], hlo[:].rearrange("p k t -> p (k t)")]
            stageB = [xnB, hloB]

            def loads2(u):
                ex, t = units[u]
                p = u % 2
                r0 = ex * CAP + t * 128
                P.dma("sp", lambda e: e.dma_start(out=stage[p], in_=hg_d[r0:r0 + 128, :]), reads=[hgB], writes=[stageB[p]])

            loads2(0)
            for u, (ex, t) in enumerate(units):
                slot_w = ex % 2
                if t == 0 and ex + 1 < NE:
                    ffn_load((ex + 1) % 2, moe_w1[0, ex + 1], moe_w3[0, ex + 1], moe_w2[0, ex + 1])
                p = u % 2
                if u + 1 < len(units):
                    loads2(u + 1)
                set_par(p)
                bt, btB = nb()
                btb = bt[:].bitcast(BF16)
                for k in range(8):
                    pe((lambda k: (lambda e: e.transpose(out=btb[:, k * 128:(k + 1) * 128], in_=stage[p][:, k * 128:(k + 1) * 128], identity=identb[:])))(k), [stageB[p], constB], [btB])
                act(lambda e: e.activation(out=hT[:].rearrange("p k t -> p (k t)"), in_=btb[:, 0:1024], func=AF.Copy), [btB], [hTB])
                ob0, ob0B = banks[4 + 2 * p], bankB[4 + 2 * p]
                ob1, ob1B = banks[5 + 2 * p], bankB[5 + 2 * p]
                ffn_expert(slot_w, [ob0, ob1], [ob0B, ob1B], True, True)
                act(lambda e: e.activation(out=X[p][:, 0:512], in_=ob0[:], func=AF.Copy), [ob0B], [XB[p]])
                dve(lambda e: e.tensor_copy(out=X[p][:, 512:1024], in_=ob1[:]), [ob1B], [XB[p]])
                r0 = ex * CAP + t * 128
                P.dma("sp", lambda e: e.dma_start(out=y_d[r0:r0 + 128, :], in_=X[p][:]), group=y_grp)
                for t_ in list(XB[p].w):
                    P._need("sp", t_, P.q["sp"][-1][0])
                XB[p].r.append(y_grp["toks"][-1])
            P.group_end(y_grp)
            bank_n[0] = 6
            P.dma("sp", lambda e: e.dma_start(out=fng[:], in_=fng_d), writes=[fngB])
            Yv = [[arena[:, (pp * 2 + k) * 2048:(pp * 2 + k + 1) * 2048].bitcast(F32) for k in range(2)] for pp in range(2)]
            YvB = [[Buf("Yv%d%d" % (pp, k)) for k in range(2)] for pp in range(2)]
            for pp in range(2):
                for k in range(2):
                    for sbuf_ in slotB:
                        YvB[pp][k].r += list(sbuf_.r) + list(sbuf_.w)

            def loads3(ti):
                p = ti % 2
                load_x(ti, p, src)
                for k in range(2):
                    P.dma("pool", (lambda k: (lambda e: e.indirect_dma_start(out=Yv[p][k], out_offset=None, in_=y_d, in_offset=IndirectOffsetOnAxis(ap=idxu[:, ti, k:k + 1], axis=0))))(k), reads=[yB, rtB], writes=[YvB[p][k]])

            loads3(0)
            for ti in range(NTILES):
                s = ti // NT
                p = ti % 2
                if ti + 1 < NTILES:
                    loads3(ti + 1)
                if ti % NT == 0:
                    gate_fetch(s)
                Xs, Xb = X[p], XB[p]
                for half in range(2):
                    sl = slice(half * 512, (half + 1) * 512)
                    dve((lambda sl, half: (lambda e: e.tensor_scalar(out=T[half][:], in0=Yv[p][0][:, sl], scalar1=RT[:, ti, 2:3], scalar2=None, op0=ALU.mult)))(sl, half), [YvB[p][0], rtB], [TB[half]])
                    dve((lambda sl, half: (lambda e: e.scalar_tensor_tensor(out=T[half][:], in0=Yv[p][1][:, sl], scalar=RT[:, ti, 3:4], in1=T[half][:], op0=ALU.mult, op1=ALU.add)))(sl, half), [YvB[p][1], rtB], [TB[half]])
                    dve((lambda sl, half: (lambda e: e.tensor_tensor(out=T[half][:], in0=T[half][:], in1=GATEb[:, sl], op=ALU.mult)))(sl, half), [gateB], [TB[half]])
                    pool((lambda sl, half: (lambda e: e.tensor_tensor(out=Xs[:, sl], in0=Xs[:, sl], in1=T[half][:], op=ALU.add)))(sl, half), [TB[half], Xb], [Xb])
                final_norm(ti, p)

        def final_norm(ti, slot):
            Xs, Xb = X[slot], XB[slot]
            act(lambda e: e.activation(out=xn[:], in_=Xs[:], func=AF.Square, accum_out=small[:, 0:1]), [Xb], [xnB])
            dve(lambda e: e.tensor_scalar(out=small[:, 1:2], in0=small[:, 0:1], scalar1=1.0 / D, scalar2=EPS, op0=ALU.mult, op1=ALU.add), [xnB], [xnB])
            act(lambda e: e.activation(out=small[:, 1:2], in_=small[:, 1:2], func=AF.Ln), [xnB], [xnB])
            act(lambda e: e.activation(out=small[:, 2:3], in_=small[:, 1:2], func=AF.Exp, scale=-0.5), [xnB], [xnB])
            dve(lambda e: e.scalar_tensor_tensor(out=Xs[:], in0=Xs[:], scalar=small[:, 2:3], in1=fng[:], op0=ALU.mult, op1=ALU.mult), [xnB, fngB], [Xb])
            P.dma("sp", lambda e: e.dma_start(out=out_d[ti * 128:(ti + 1) * 128, :], in_=Xs[:]), reads=[Xb], writes=[xdB[ti], outB[ti]])

        def mixer_bufs():
            pass

        if "A" in phases or "C" in phases:
            qt = sb("qt", [128, 512], BF16); kt = sb("kt", [128, 512], BF16); qE = sb("qE", [128, 512], BF16); kdT = sb("kdT", [128, 512], BF16)
            hgB = Buf("hgops")
            vtok = sb("vtok", [128, 512], BF16); vtokB = Buf("vtok")
            kdtok = sb("kdtok", [128, 512], BF16); kdtokB = Buf("kdtok")
            Pm = sb("Pm", [128, 4, 64], BF16); PmB = Buf("Pm")
            S32 = sb("S32", [128, 4, 128]); Sbf = sb("Sbf", [128, 4, 128], BF16); SB_ = Buf("S")
            Gs = sb("Gs", [128, 512], BF16); GsB = Buf("Gs")
            yhgT = sb("yhgT", [128, 4, 128], BF16); yhgB = Buf("yhgT")
            ymlT = sb("ymlT", [128, 4, 128], BF16); ymlB = Buf("ymlT")
            eA = sb("eA", [128, 16]); eAB = Buf("eA")
            U = sb("U", [128, 4, 131]); UB = Buf("U")
            ucT = sb("ucT", [128, 4, 128], BF16); ucB = Buf("ucT")
            qTm = sb("qTm", [64, 4, 128], BF16); kTm = sb("kTm", [64, 4, 128], BF16); qkB = Buf("qkm")
            ktok = sb("ktok", [128, 4, 64], BF16); ktokB = Buf("ktok")
            vaug = sb("vaug", [128, 4, 130], BF16); vaugB = Buf("vaug")
            mosg = sb("mosg", [128, 512], BF16); mosgB = Buf("mosg")
            _a1 = actT_l[1][:].rearrange("p j t -> p (j t)")
            Pml = _a1[:, 512:1024].rearrange("p (h t) -> p h t", h=4); PmlB = actTB_l[1][0]
            C32 = sb("C32", [64, 4, 130]); Cbf = sb("Cbf", [64, 4, 130], BF16); CB_ = Buf("C")
            gts = sb("gts", [128, 64]); gtsB = Buf("gts")
            gthl = sb("gthl", [128, 8], BF16);
            yml = _a1[:, 0:512]; ymlTokB = actTB_l[1][0]
            SGA = hT_l[1]; SGB = actT_l[0][:, 0:8, :]; sgB = hTB_l[1]; sgB2 = actTB_l[0][0]; sgB3 = actTB_l[0][1]
            mrg = hlo; mrgB = hloB
            wqk = sb("wqk", [128, 2, 4, 64], BF16); wqkB = Buf("wqk")

        def phase_mixer(l, src):
            aw = ada_setup(ada_mix_w[l], l, "mix", PV_NMG, PV_AMB, 2 * l)
            gate_rows(aw, 2 * l)
            Win = arena[:, 0:8 * INC].rearrange("p (k n) -> p k n", k=8)
            o = 8 * INC
            Wbh = arena[:, o:o + 4096].rearrange("p (k n) -> p k n", k=4)
            Wbm = arena[:, o + 4096:o + 8192].rearrange("p (k n) -> p k n", k=4)
            Wo = arena[:, o + 8192:o + 16384].rearrange("p (k n) -> p k n", k=8)
            WB = slotB
            g = P.group_begin(WB + [wqkB])
            wload(Win, kview(w_in[l]), g, INC)
            wload(Wbh, kview(w_br_hg[l]), g, D)
            wload(Wbm, kview(w_br_ml[l]), g, D)
            wload(Wo, kview(w_out[l]), g, D)
            for hh in range(4):
                P.dma("pool", (lambda hh: (lambda e: e.dma_start(out=wqk[:, 0, hh, :], in_=ml_wq[l, hh])))(hh), group=g)
                P.dma("pool", (lambda hh: (lambda e: e.dma_start(out=wqk[:, 1, hh, :], in_=ml_wk[l, hh])))(hh), group=g)
            P.group_end(g)
            if l == 0:
                dve(lambda e: e.memset(lbc[:, 0, :], 0.0), [], [lbB])
                dve(lambda e: e.memset(lbc[:, 1, :], 1.0), [], [lbB])
            else:
                dve(lambda e: e.tensor_tensor(out=lbc[:, 0, :], in0=pv[:, PVL + PV_LB:PVL + PV_LB + 4], in1=pv[:, PV_LB:PV_LB + 4], op=ALU.subtract), [pvB], [lbB])
                act(lambda e: e.activation(out=lbc[:, 0, :], in_=lbc[:, 0, :], func=AF.Sigmoid), [], [lbB])
                dve(lambda e: e.tensor_scalar(out=lbc[:, 1, :], in0=lbc[:, 0, :], scalar1=-1.0, scalar2=1.0, op0=ALU.mult, op1=ALU.add), [], [lbB])
            pvl = l * PVL

            def proj_fm(c0, nblk, bk, bb):
                for j in range(nblk):
                    for k in range(8):
                        pe((lambda j, k: (lambda e: e.matmul(bk[:, j * 128:(j + 1) * 128], lhsT=Win[:, k, c0 + j * 128:c0 + (j + 1) * 128], rhs=hT[:, k, :], start=(k == 0), stop=(k == 7))))(j, k), [WB[0], hTB], [bb])

            def proj_tm(c0, n, bk, bb):
                for k in range(8):
                    pe((lambda k: (lambda e: e.matmul(bk[:, 0:n], lhsT=hT[:, k, :], rhs=Win[:, k, c0:c0 + n], start=(k == 0), stop=(k == 7))))(k), [WB[0], hTB], [bb])

            set_par(0)
            bank_n[0] = 6
            for ti in range(NTILES):
                s = ti // NT
                slot = ti % 2
                first = (ti % NT == 0)
                if first:
                    gate_fetch(s)
                    dve(lambda e: e.memset(S32[:], 0.0), [], [SB_])
                    dve(lambda e: e.memset(Sbf[:], 0.0), [], [SB_])
                    dve(lambda e: e.memset(C32[:], 0.0), [], [CB_])
                    dve(lambda e: e.memset(Cbf[:], 0.0), [], [CB_])
                    dve(lambda e: e.memset(U[:], 0.0), [], [UB])
                load_x(ti, slot, src)
                norm_mod(slot, s)

                bq, bqB = nb(); proj_fm(C_Q, 4, bq, bqB)
                bf, bfB = nb(); proj_fm(C_F, 4, bf, bfB)
                bg, bgB = nb(); proj_fm(C_G, 4, bg, bgB)
                bv, bvB = nb(); proj_tm(C_I, 512, bv, bvB)
                Q, Fb, LF, KK, E1, E2 = T[0], T[1], T[2], T[3], T[4], T[5]
                act(lambda e: e.activation(out=Q[:], in_=bq[:], func=AF.Silu), [bqB], [TB[0]])
                act(lambda e: e.activation(out=Gs[:], in_=bg[:], func=AF.Silu), [bgB], [GsB])
                act(lambda e: e.activation(out=Fb[:], in_=bf[:], func=AF.Sigmoid), [bfB], [TB[1]])
                act(lambda e: e.activation(out=vtok[:], in_=bv[:], func=AF.Copy), [bvB], [vtokB])
                bu, buB = nb(); proj_fm(C_MU, 4, bu, buB)
                bmv, bmvB = nb(); proj_tm(C_MV, 512, bmv, bmvB)
                bmo, bmoB = nb(); proj_tm(C_MO, 512, bmo, bmoB)
                bgt, bgtB = nb(); proj_tm(C_MI, 8, bgt, bgtB)
                act(lambda e: e.activation(out=U[:, :, 3:131], in_=bu[:].rearrange("p (h t) -> p h t", h=4), func=AF.Copy), [buB], [UB])
                act(lambda e: e.activation(out=mosg[:], in_=bmo[:], func=AF.Sigmoid), [bmoB], [mosgB])
                act(lambda e: e.activation(out=vaug[:, :, 0:128], in_=bmv[:].rearrange("p (h t) -> p h t", h=4), func=AF.Copy), [bmvB], [vaugB])
                dve(lambda e: e.memset(vaug[:, :, 128:130], 1.0), [], [vaugB])
                dve(lambda e: e.tensor_copy(out=gts[:, 0:4], in_=bgt[:, 0:4]), [bgtB], [gtsB])
                dve(lambda e: e.tensor_tensor(out=gts[:, 4:8], in0=bgt[:, 4:8], in1=pv[:, pvl + PV_FB: pvl + PV_FB + 4], op=ALU.add), [bgtB, pvB], [gtsB])
                for (c0, SG) in ((C_GA, SGA), (C_GB, SGB)):
                    for half in range(2):
                        bgx, bgxB = nb()
                        proj_fm(c0 + half * 512, 4, bgx, bgxB)
                        act((lambda SG, half, bgx: (lambda e: e.activation(out=SG[:, half * 4:(half + 1) * 4, :].rearrange("p k t -> p (k t)"), in_=bgx[:], func=AF.Sigmoid)))(SG, half, bgx), [bgxB], [sgB, sgB2, sgB3])
                for hh in range(4):
                    dve((lambda hh: (lambda e: e.tensor_scalar(out=Fb[:, hh * 128:(hh + 1) * 128], in0=Fb[:, hh * 128:(hh + 1) * 128], scalar1=lbc[:, 1, hh:hh + 1], scalar2=lbc[:, 0, hh:hh + 1], op0=ALU.mult, op1=ALU.add)))(hh), [lbB], [TB[1]])
                act(lambda e: e.activation(out=LF[:], in_=Fb[:], func=AF.Ln), [TB[1]], [TB[2]])
                dve(lambda e: e.tensor_scalar(out=KK[:], in0=Fb[:], scalar1=-1.0, scalar2=1.0, op0=ALU.mult, op1=ALU.add), [TB[1]], [TB[3]])
                A_ = Fb
                dve(lambda e: e.tensor_tensor_scan(out=A_[:], data0=rst[:], data1=LF[:], initial=0.0, op0=ALU.mult, op1=ALU.add), [TB[2], constB], [TB[1]])
                A4 = A_[:].rearrange("p (g t) -> p g t", t=64)
                Dd = LF
                dve(lambda e: e.tensor_tensor(out=Dd[:].rearrange("p (g t) -> p g t", t=64), in0=A4, in1=A4[:, :, 31:32].broadcast_to([128, 8, 64]), op=ALU.subtract), [TB[1]], [TB[2]])
                dve(lambda e: e.tensor_copy(out=eA[:, 0:8], in_=A4[:, :, 31]), [TB[1]], [eAB])
                dve(lambda e: e.tensor_copy(out=eA[:, 8:16], in_=Dd[:].rearrange("p (g t) -> p g t", t=64)[:, :, 63]), [TB[2]], [eAB])
                dve(lambda e: e.tensor_copy(out=small[:, 8:16], in_=A4[:, :, 63]), [TB[1]], [eAB])
                dve(lambda e: e.tensor_scalar(out=Dd[:], in0=Dd[:], scalar1=40.0, scalar2=-40.0, op0=ALU.min, op1=ALU.max), [eAB], [TB[2]])
                act(lambda e: e.activation(out=E1[:], in_=Dd[:], func=AF.Exp), [TB[2]], [TB[4]])
                act(lambda e: e.activation(out=E2[:], in_=Dd[:], func=AF.Exp, scale=-1.0), [TB[2]], [TB[5]])
                act(lambda e: e.activation(out=eA[:], in_=eA[:], func=AF.Exp), [eAB], [eAB])
                act(lambda e: e.activation(out=small[:, 8:16], in_=small[:, 8:16], func=AF.Exp), [eAB], [eAB])
                dve(lambda e: e.tensor_tensor(out=Q[:], in0=Q[:], in1=E1[:], op=ALU.mult), [TB[4]], [TB[0]])
                dve(lambda e: e.tensor_tensor(out=KK[:], in0=KK[:], in1=E2[:], op=ALU.mult), [TB[5]], [TB[3]])
                act(lambda e: e.activation(out=qt[:], in_=Q[:], func=AF.Copy), [TB[0]], [hgB])
                act(lambda e: e.activation(out=kt[:], in_=KK[:], func=AF.Copy), [TB[3]], [hgB])
                dve(lambda e: e.tensor_tensor(out=qE[:].rearrange("p (g t) -> p g t", t=64), in0=Q[:].rearrange("p (g t) -> p g t", t=64), in1=eA[:, 0:8].unsqueeze(2).broadcast_to([128, 8, 64]), op=ALU.mult), [TB[0], eAB], [hgB])
                dve(lambda e: e.tensor_tensor(out=kdT[:].rearrange("p (g t) -> p g t", t=64), in0=KK[:].rearrange("p (g t) -> p g t", t=64), in1=eA[:, 8:16].unsqueeze(2).broadcast_to([128, 8, 64]), op=ALU.mult), [TB[3], eAB], [hgB])
                if debug and ti == 1:
                    def dump2(nm, c0, ap, bufs):
                        b = Buf(nm + str(c0)); dbgB.append(b)
                        P.dma("pool", lambda e: e.dma_start(out=dbg[nm][:, c0:c0 + 512], in_=ap), reads=bufs, writes=[b])
                    dump2("dbg_hT", 0, A_[:], [TB[1]])
                    dump2("dbg_hT", 512, Dd[:], [TB[2]])
                    dump2("dbg_gate", 0, Q[:], [TB[0]])
                    dump2("dbg_gate", 512, KK[:], [TB[3]])
                    dump2("dbg_xn", 0, E1[:], [TB[4]])
                    dump2("dbg_xn", 512, E2[:], [TB[5]])
                bt, btB = nb()
                btb = bt[:].bitcast(BF16)
                for hh in range(4):
                    pe((lambda hh: (lambda e: e.transpose(out=btb[:, hh * 128:(hh + 1) * 128], in_=kdT[:, hh * 128:(hh + 1) * 128], identity=identb[:])))(hh), [hgB, constB], [btB])
                act(lambda e: e.activation(out=kdtok[:], in_=btb[:, 0:512], func=AF.Copy), [btB], [kdtokB])
                bs, bsB = nb()
                for hh in range(4):
                    for c in range(2):
                        g = hh * 2 + c
                        pe((lambda hh, c, g: (lambda e: e.matmul(bs[c * 64:(c + 1) * 64, hh * 64:(hh + 1) * 64], lhsT=kt[:, g * 64:(g + 1) * 64], rhs=qt[:, g * 64:(g + 1) * 64], start=True, stop=True)))(hh, c, g), [hgB], [bsB])
                dve(lambda e: e.tensor_tensor(out=Pm[:], in0=bs[:, 0:256].rearrange("p (h t) -> p h t", h=4), in1=m64[:].unsqueeze(1).broadcast_to([128, 4, 64]), op=ALU.mult), [bsB, constB], [PmB])
                bo, boB = nb()
                for c in range(2):
                    rs = slice(c * 64, (c + 1) * 64)
                    for hh in range(4):
                        g = hh * 2 + c
                        pe((lambda hh, c, g, rs: (lambda e: e.matmul(bo[:, g * 64:(g + 1) * 64], lhsT=vtok[rs, hh * 128:(hh + 1) * 128], rhs=Pm[rs, hh, :], start=True, stop=False)))(hh, c, g, rs), [vtokB, PmB], [boB])
                        pe((lambda hh, c, g: (lambda e: e.matmul(bo[:, g * 64:(g + 1) * 64], lhsT=Sbf[:, hh, :], rhs=qE[:, g * 64:(g + 1) * 64], start=False, stop=True)))(hh, c, g), [SB_, hgB], [boB])
                    bd, bdB = nb()
                    for hh in range(4):
                        pe((lambda hh, rs: (lambda e: e.matmul(bd[:, hh * 128:(hh + 1) * 128], lhsT=kdtok[rs, hh * 128:(hh + 1) * 128], rhs=vtok[rs, hh * 128:(hh + 1) * 128], start=True, stop=True)))(hh, rs), [kdtokB, vtokB], [bdB])
                    for hh in range(4):
                        g = hh * 2 + c
                        dve((lambda hh, g, bd: (lambda e: e.scalar_tensor_tensor(out=S32[:, hh, :], in0=S32[:, hh, :], scalar=small[:, 8 + g:9 + g], in1=bd[:, hh * 128:(hh + 1) * 128], op0=ALU.mult, op1=ALU.add)))(hh, g, bd), [bdB, eAB], [SB_])
                    act(lambda e: e.activation(out=Sbf[:], in_=S32[:], func=AF.Copy), [], [SB_])
                OS = T[4]
                act(lambda e: e.activation(out=OS[:], in_=bo[:], func=AF.Copy), [boB], [TB[4]])
                SQ = T[5]
                act(lambda e: e.activation(out=SQ[:].bitcast(BF16)[:, 0:512], in_=bo[:], func=AF.Square), [boB], [TB[5]])
                bn, bnB = nb()
                pe(lambda e: e.matmul(bn[:], lhsT=onesb[:], rhs=SQ[:].bitcast(BF16)[:, 0:512], start=True, stop=True), [TB[5], constB], [bnB])
                R = T[2]
                dve(lambda e: e.tensor_scalar(out=R[:], in0=bn[:], scalar1=1.0 / 128, scalar2=EPS, op0=ALU.mult, op1=ALU.add), [bnB], [TB[2]])
                act(lambda e: e.activation(out=R[:], in_=R[:], func=AF.Ln), [], [TB[2]])
                act(lambda e: e.activation(out=R[:], in_=R[:], func=AF.Exp, scale=-0.5), [], [TB[2]])
                dve(lambda e: e.tensor_tensor(out=OS[:], in0=OS[:], in1=R[:], op=ALU.mult), [TB[2]], [TB[4]])
                dve(lambda e: e.tensor_tensor(out=OS[:], in0=OS[:], in1=Gs[:], op=ALU.mult), [GsB], [TB[4]])
                for hh in range(4):
                    dve((lambda hh: (lambda e: e.tensor_scalar(out=yhgT[:, hh, :], in0=OS[:, hh * 128:(hh + 1) * 128], scalar1=pv[:, pvl + PV_HNG + hh: pvl + PV_HNG + hh + 1], scalar2=None, op0=ALU.mult)))(hh), [TB[4], pvB], [yhgB])

                CV = T[0]
                for hh in range(4):
                    cw = pvl + PV_CW
                    dve((lambda hh, cw: (lambda e: e.tensor_scalar(out=CV[:, hh * 128:(hh + 1) * 128], in0=U[:, hh, 0:128], scalar1=pv[:, cw + 0 * 4 + hh: cw + 0 * 4 + hh + 1], scalar2=pv[:, pvl + PV_CB + hh: pvl + PV_CB + hh + 1], op0=ALU.mult, op1=ALU.add)))(hh, cw), [UB, pvB], [TB[0]])
                    for j in range(1, 4):
                        dve((lambda hh, cw, j: (lambda e: e.scalar_tensor_tensor(out=CV[:, hh * 128:(hh + 1) * 128], in0=U[:, hh, j:j + 128], scalar=pv[:, cw + j * 4 + hh: cw + j * 4 + hh + 1], in1=CV[:, hh * 128:(hh + 1) * 128], op0=ALU.mult, op1=ALU.add)))(hh, cw, j), [UB, pvB], [TB[0]])
                pool(lambda e: e.tensor_copy(out=U[:, :, 0:3], in_=U[:, :, 128:131]), [], [UB])
                act(lambda e: e.activation(out=ucT[:].rearrange("p h t -> p (h t)"), in_=CV[:], func=AF.Silu), [TB[0]], [ucB])
                act(lambda e: e.activation(out=gts[:, 4:8], in_=gts[:, 4:8], func=AF.Exp, scale=-1.0), [], [gtsB])
                act(lambda e: e.activation(out=gts[:, 8:12], in_=gts[:, 4:8], func=AF.Ln, bias=1.0), [], [gtsB])
                dve(lambda e: e.tensor_scalar(out=gts[:, 8:12], in0=gts[:, 8:12], scalar1=-1.0, scalar2=None, op0=ALU.mult), [], [gtsB])
                dve(lambda e: e.tensor_copy(out=gthl[:, 0:4], in_=gts[:, 8:12]), [], [gtsB])
                dve(lambda e: e.tensor_tensor(out=gts[:, 32:36], in0=gts[:, 8:12], in1=gthl[:, 0:4], op=ALU.subtract), [], [gtsB])
                dve(lambda e: e.tensor_copy(out=gthl[:, 4:8], in_=gts[:, 32:36]), [], [gtsB])
                bc, bcB = nb()
                pe(lambda e: e.matmul(bc[:, 0:4], lhsT=trib[:], rhs=gthl[:, 0:4], start=True, stop=False), [gtsB, constB], [bcB])
                pe(lambda e: e.matmul(bc[:, 0:4], lhsT=trib[:], rhs=gthl[:, 4:8], start=False, stop=True), [gtsB, constB], [bcB])
                pe(lambda e: e.matmul(bc[:, 4:8], lhsT=onesb[:], rhs=gthl[:, 0:4], start=True, stop=False), [gtsB, constB], [bcB])
                pe(lambda e: e.matmul(bc[:, 4:8], lhsT=onesb[:], rhs=gthl[:, 4:8], start=False, stop=True), [gtsB, constB], [bcB])
                dve(lambda e: e.tensor_copy(out=gts[:, 12:20], in_=bc[:, 0:8]), [bcB], [gtsB])
                dve(lambda e: e.tensor_tensor(out=gts[:, 20:24], in0=gts[:, 0:4], in1=gts[:, 12:16], op=ALU.subtract), [], [gtsB])
                dve(lambda e: e.tensor_tensor(out=gts[:, 28:32], in0=gts[:, 20:24], in1=gts[:, 16:20], op=ALU.add), [], [gtsB])
                dve(lambda e: e.tensor_copy(out=gts[:, 24:28], in_=gts[:, 12:16]), [], [gtsB])
                act(lambda e: e.activation(out=gts[:, 20:32], in_=gts[:, 20:32], func=AF.Exp), [], [gtsB])
                act(lambda e: e.activation(out=gts[:, 36:40], in_=gts[:, 16:20], func=AF.Exp), [], [gtsB])
                bqk, bqkB = nb()
                for hh in range(4):
                    pe((lambda hh: (lambda e: e.matmul(bqk[0:64, hh * 128:(hh + 1) * 128], lhsT=wqk[:, 0, hh, :], rhs=ucT[:, hh, :], start=True, stop=True)))(hh), [wqkB, ucB], [bqkB])
                bkk, bkkB = nb()
                for hh in range(4):
                    pe((lambda hh: (lambda e: e.matmul(bkk[0:64, hh * 128:(hh + 1) * 128], lhsT=wqk[:, 1, hh, :], rhs=ucT[:, hh, :], start=True, stop=True)))(hh), [wqkB, ucB], [bkkB])
                bkt, bktB = nb()
                for hh in range(4):
                    pe((lambda hh: (lambda e: e.matmul(bkt[:, hh * 64:(hh + 1) * 64], lhsT=ucT[:, hh, :], rhs=wqk[:, 1, hh, :], start=True, stop=True)))(hh), [wqkB, ucB], [bktB])
                act(lambda e: e.activation(out=qTm[:].rearrange("p h t -> p (h t)"), in_=bqk[0:64, :], func=AF.Copy, scale=0.125), [bqkB], [qkB])
                act(lambda e: e.activation(out=kTm[:].rearrange("p h t -> p (h t)"), in_=bkk[0:64, :], func=AF.Copy), [bkkB], [qkB])
                dve(lambda e: e.tensor_tensor(out=ktok[:], in0=bkt[:, 0:256].rearrange("p (h e) -> p h e", h=4), in1=gts[:, 28:32].unsqueeze(2).broadcast_to([128, 4, 64]), op=ALU.mult), [bktB, gtsB], [ktokB])
                bsm, bsmB = nb()
                for hh in range(4):
                    pe((lambda hh: (lambda e: e.matmul(bsm[:, hh * 128:(hh + 1) * 128], lhsT=kTm[:, hh, :], rhs=qTm[:, hh, :], start=True, stop=True)))(hh), [qkB], [bsmB])
                for hh in range(4):
                    dve((lambda hh: (lambda e: e.scalar_tensor_tensor(out=Pml[:, hh, :], in0=bsm[:, hh * 128:(hh + 1) * 128], scalar=gts[:, 20 + hh:21 + hh], in1=caus[:], op0=ALU.mult, op1=ALU.mult)))(hh), [bsmB, gtsB, constB], [PmlB])
                HM = T[1]
                bnum = []
                for pair in range(2):
                    bn2, bn2B = nb()
                    bnum.append((bn2, bn2B))
                    for hq in range(2):
                        hh = pair * 2 + hq
                        pe((lambda hh, hq, bn2: (lambda e: e.matmul(bn2[:, hq * 130:hq * 130 + 130], lhsT=Pml[:, hh, :], rhs=vaug[:, hh, :], start=True, stop=False)))(hh, hq, bn2), [PmlB, vaugB], [bn2B])
                        pe((lambda hh, hq, bn2: (lambda e: e.matmul(bn2[:, hq * 130:hq * 130 + 130], lhsT=qTm[:, hh, :], rhs=Cbf[:, hh, :], start=False, stop=True)))(hh, hq, bn2), [qkB, CB_], [bn2B])
                bcs, bcsB = nb()
                for hh in range(4):
                    pe((lambda hh: (lambda e: e.matmul(bcs[0:64, hh * 128:hh * 128 + 128], lhsT=ktok[:, hh, :], rhs=vaug[:, hh, 0:128], start=True, stop=True)))(hh), [ktokB, vaugB], [bcsB])
                bcn, bcnB = nb()
                for hh in range(4):
                    pe((lambda hh: (lambda e: e.matmul(bcn[0:64, hh * 2:hh * 2 + 2], lhsT=ktok[:, hh, :], rhs=vaug[:, hh, 128:130], start=True, stop=True)))(hh), [ktokB, vaugB], [bcnB])
                for hh in range(4):
                    dve((lambda hh: (lambda e: e.scalar_tensor_tensor(out=C32[:, hh, 0:128], in0=C32[:, hh, 0:128], scalar=gts[0:64, 36 + hh:37 + hh], in1=bcs[0:64, hh * 128:(hh + 1) * 128], op0=ALU.mult, op1=ALU.add)))(hh), [bcsB, gtsB], [CB_])
                    dve((lambda hh: (lambda e: e.scalar_tensor_tensor(out=C32[:, hh, 128:130], in0=C32[:, hh, 128:130], scalar=gts[0:64, 36 + hh:37 + hh], in1=bcn[0:64, hh * 2:hh * 2 + 2], op0=ALU.mult, op1=ALU.add)))(hh), [bcnB, gtsB], [CB_])
                for pair in range(2):
                    bn2, bn2B = bnum[pair]
                    for hq in range(2):
                        hh = pair * 2 + hq
                        c0 = hq * 130
                        dve((lambda hh, c0, bn2: (lambda e: e.tensor_scalar(out=gts[:, 52 + hh:53 + hh], in0=bn2[:, c0 + 128:c0 + 129], scalar1=gts[:, 24 + hh:25 + hh], scalar2=None, op0=ALU.mult)))(hh, c0, bn2), [bn2B], [gtsB])
                        dve((lambda hh: (lambda e: e.scalar_tensor_tensor(out=gts[:, 40 + hh:41 + hh], in0=gts[:, 52 + hh:53 + hh], scalar=-1.0, in1=gts[:, 52 + hh:53 + hh], op0=ALU.mult, op1=ALU.max)))(hh), [], [gtsB])
                        dve((lambda hh: (lambda e: e.tensor_scalar(out=gts[:, 40 + hh:41 + hh], in0=gts[:, 40 + hh:41 + hh], scalar1=1.0, scalar2=None, op0=ALU.max)))(hh), [], [gtsB])
                        dve((lambda hh: (lambda e: e.reciprocal(out=gts[:, 44 + hh:45 + hh], in_=gts[:, 40 + hh:41 + hh])))(hh), [], [gtsB])
                        dve((lambda hh: (lambda e: e.tensor_tensor(out=gts[:, 44 + hh:45 + hh], in0=gts[:, 44 + hh:45 + hh], in1=gts[:, 24 + hh:25 + hh], op=ALU.mult)))(hh), [], [gtsB])
                        dve((lambda hh, c0, bn2: (lambda e: e.tensor_scalar(out=HM[:, hh * 128:(hh + 1) * 128], in0=bn2[:, c0:c0 + 128], scalar1=gts[:, 44 + hh:45 + hh], scalar2=None, op0=ALU.mult)))(hh, c0, bn2), [bn2B, gtsB], [TB[1]])
                        act((lambda hh: (lambda e: e.activation(out=T[2][:, hh * 128:(hh + 1) * 128], in_=HM[:, hh * 128:(hh + 1) * 128], func=AF.Square, accum_out=gts[:, 48 + hh:49 + hh])))(hh), [TB[1]], [TB[2], gtsB])
                act(lambda e: e.activation(out=Cbf[:], in_=C32[:], func=AF.Copy), [], [CB_])
                dve(lambda e: e.tensor_scalar(out=gts[:, 48:52], in0=gts[:, 48:52], scalar1=1.0 / 128, scalar2=EPS, op0=ALU.mult, op1=ALU.add), [], [gtsB])
                act(lambda e: e.activation(out=gts[:, 48:52], in_=gts[:, 48:52], func=AF.Ln), [], [gtsB])
                act(lambda e: e.activation(out=gts[:, 48:52], in_=gts[:, 48:52], func=AF.Exp, scale=-0.5), [], [gtsB])
                for hh in range(4):
                    dve((lambda hh: (lambda e: e.scalar_tensor_tensor(out=yml[:, hh * 128:(hh + 1) * 128], in0=HM[:, hh * 128:(hh + 1) * 128], scalar=gts[:, 48 + hh:49 + hh], in1=mosg[:, hh * 128:(hh + 1) * 128], op0=ALU.mult, op1=ALU.mult)))(hh), [TB[1], gtsB, mosgB], [ymlTokB])
                bty, btyB = nb()
                btyb = bty[:].bitcast(BF16)
                for hh in range(4):
                    pe((lambda hh: (lambda e: e.transpose(out=btyb[:, hh * 128:(hh + 1) * 128], in_=yml[:, hh * 128:(hh + 1) * 128], identity=identb[:])))(hh), [ymlTokB, constB], [btyB])
                for hh in range(4):
                    dve((lambda hh: (lambda e: e.tensor_scalar(out=ymlT[:, hh, :], in0=btyb[:, hh * 128:(hh + 1) * 128], scalar1=pv[:, pvl + PV_MNG + hh: pvl + PV_MNG + hh + 1], scalar2=None, op0=ALU.mult)))(hh), [btyB, pvB], [ymlB])

                for half in range(2):
                    bph, bphB = nb()
                    bpm, bpmB = nb()
                    for jj in range(4):
                        j = half * 4 + jj
                        for k in range(4):
                            pe((lambda j, jj, k, bph: (lambda e: e.matmul(bph[:, jj * 128:(jj + 1) * 128], lhsT=Wbh[:, k, j * 128:(j + 1) * 128], rhs=yhgT[:, k, :], start=(k == 0), stop=(k == 3))))(j, jj, k, bph), [WB[1], yhgB], [bphB])
                        for k in range(4):
                            pe((lambda j, jj, k, bpm: (lambda e: e.matmul(bpm[:, jj * 128:(jj + 1) * 128], lhsT=Wbm[:, k, j * 128:(j + 1) * 128], rhs=ymlT[:, k, :], start=(k == 0), stop=(k == 3))))(j, jj, k, bpm), [WB[1], ymlB], [bpmB])
                    dve((lambda half, bph: (lambda e: e.tensor_tensor(out=T[3][:], in0=bph[:], in1=SGA[:, half * 4:(half + 1) * 4, :].rearrange("p k t -> p (k t)"), op=ALU.mult)))(half, bph), [bphB, sgB, sgB2, sgB3], [TB[3]])
                    dve((lambda half, bpm: (lambda e: e.tensor_tensor(out=T[5][:], in0=bpm[:], in1=SGB[:, half * 4:(half + 1) * 4, :].rearrange("p k t -> p (k t)"), op=ALU.mult)))(half, bpm), [bpmB, sgB, sgB2, sgB3], [TB[5]])
                    pool((lambda half: (lambda e: e.tensor_tensor(out=mrg[:, half * 4:(half + 1) * 4, :].rearrange("p k t -> p (k t)"), in0=T[3][:], in1=T[5][:], op=ALU.add)))(half), [TB[3], TB[5]], [mrgB])
                ob0, ob0B = banks[6], bankB[6]
                ob1, ob1B = banks[7], bankB[7]
                for half, ob, obb in ((0, ob0, ob0B), (1, ob1, ob1B)):
                    for k in range(8):
                        pe((lambda half, k, ob: (lambda e: e.matmul(ob[:], lhsT=mrg[:, k, :], rhs=Wo[:, k, half * 512:(half + 1) * 512], start=(k == 0), stop=(k == 7))))(half, k, ob), [mrgB, WB[1]], [obb])
                residual(slot, [ob0, ob1], [ob0B, ob1B])
                store_x(ti, slot, xdB[ti])

        if "A" in phases:
            phase_mixer(0, x_d)
        srcB = out_d if "A" in phases else x_d
        if "B" in phases:
            phase_dense(0, srcB)
        srcC = out_d if ("A" in phases or "B" in phases) else x_d
        if "C" in phases:
            phase_mixer(1, srcC)
        srcD = out_d if any(p in phases for p in "ABC") else x_d
        if "D" in phases:
            if SPARSE_MOE:
                phase_moe_sparse(1, srcD)
            else:
                phase_moe(1, srcD)
        P.final_wait("sp", xdB)
        P.final_wait("pool", dbgB)
        P.emit()
    return nc, P


def _consts():
    cst = np.zeros((128, 1024), np.float32)
    cst[:, 0:128] = np.eye(128, dtype=np.float32)
    s = np.arange(128)[:, None]
    t = np.arange(128)[None, :]
    cst[:, 128:256] = (s <= t)
    cst[:, 256:384] = ((s // 64) == (t // 64))
    cst[:, 384:512] = (s <= t)
    cst[:, 512:576] = ((s % 64) <= np.arange(64)[None, :])
    cst[:, 576:704] = (s < t)
    rst = np.ones((128, 512), np.float32)
    rst[:, 0::64] = 0.0
    return cst, rst


def _fm(v, k):
    return np.ascontiguousarray(np.asarray(v, np.float32).reshape(k, 128).T)


def _pack_pv(inp):
    pv = np.zeros((128, DEPTH * PVL), np.float32)
    for l in range(DEPTH):
        o = l * PVL
        pv[:, o + PV_NMG:o + PV_NMG + 8] = _fm(inp["norm_mix_g"][l], 8)
        pv[:, o + PV_AMB:o + PV_AMB + 24] = _fm(inp["ada_mix_b"][l], 24)
        pv[:, o + PV_NFG:o + PV_NFG + 8] = _fm(inp["norm_ffn_g"][l], 8)
        pv[:, o + PV_AFB:o + PV_AFB + 24] = _fm(inp["ada_ffn_b"][l], 24)
        pv[:, o + PV_LB:o + PV_LB + 4] = _fm(inp["hg_lb_logits"][l], 4)
        pv[:, o + PV_HNG:o + PV_HNG + 4] = _fm(inp["hg_norm_g"][l], 4)
        cw = np.asarray(inp["ml_conv_w"][l], np.float32)
        for j in range(4):
            pv[:, o + PV_CW + j * 4:o + PV_CW + j * 4 + 4] = _fm(cw[j], 4)
        pv[:, o + PV_CB:o + PV_CB + 4] = _fm(inp["ml_conv_b"][l], 4)
        pv[:, o + PV_MNG:o + PV_MNG + 4] = _fm(inp["ml_norm_g"][l], 4)
        pv[:, o + PV_FB:o + PV_FB + 4] = np.broadcast_to(np.asarray(inp["ml_fbias"][l], np.float32)[None, :], (128, 4))
    pv[:, PVL + PV_RB:PVL + PV_RB + NE] = np.broadcast_to(np.asarray(inp["moe_router_b"][0], np.float32)[None, :], (128, NE))
    return pv


_CACHE = {}
LAST_CNT = None


def run(inputs, n_cores, NSEQ, S, phases="ABCD", debug=False):
    key = (NSEQ, S, phases, debug)
    if key not in _CACHE:
        _CACHE[key] = build(NSEQ, S, phases, debug)[0]
    nc = _CACHE[key]
    f32 = lambda a: np.ascontiguousarray(np.asarray(a, np.float32))
    x = f32(inputs["x"]); c = f32(inputs["c"])
    cst, rst = _consts()
    pv = _pack_pv(inputs)
    gbias = np.stack([np.broadcast_to(f32(inputs[nm])[l][2 * D:3 * D][None, :], (128, D))
                      for l in range(DEPTH) for nm in ("ada_mix_b", "ada_ffn_b")]).astype(np.float32)
    fng = np.ascontiguousarray(np.broadcast_to(f32(inputs["final_norm_g"])[None, :], (128, D)))
    shared = {"pv": pv, "cst": cst, "rst": rst, "gbias": np.ascontiguousarray(gbias), "fng": fng}
    for nm in ("ada_mix_w", "ada_ffn_w", "w_in", "ml_wq", "ml_wk", "w_br_hg", "w_br_ml", "w_out", "ffn_w1", "ffn_w3", "ffn_w2",
               "moe_router_w", "moe_w1", "moe_w3", "moe_w2"):
        shared[nm] = f32(inputs[nm])
    in_maps = []
    for i in range(n_cores):
        xs = x[i * NSEQ:(i + 1) * NSEQ].reshape(NSEQ * S, D)
        cs = c[i * NSEQ:(i + 1) * NSEQ]
        cTl = np.ascontiguousarray(cs.reshape(NSEQ, 8, 128).transpose(2, 1, 0))
        m = dict(shared)
        m["x"] = np.ascontiguousarray(xs)
        m["cT"] = cTl
        in_maps.append(m)
    res = run_bass_kernel_spmd(nc, in_maps, core_ids=list(range(n_cores)))
    outs = [r["out"].reshape(NSEQ, S, D) for r in res.results]
    global LAST_CNT
    LAST_CNT = [r["cnt"][0] for r in res.results]
    if debug:
        return np.concatenate(outs, axis=0), res.results[0]
    return np.concatenate(outs, axis=0)


def kernel(**inputs):
    B, S, _ = inputs["x"].shape
    return run(inputs, N_CORES, B // N_CORES, S).astype(np.float32)
```

```python
import contextlib
import numpy as np
import concourse.bass as bass
import concourse.mybir as mybir
from concourse.bass_utils import run_bass_kernel_spmd

F32 = mybir.dt.float32
BF16 = mybir.dt.bfloat16
AF = mybir.ActivationFunctionType
ALU = mybir.AluOpType
AX = mybir.AxisListType

D = 1024
DEPTH = 2
INC = 5640
C_Q, C_F, C_I, C_G, C_MU, C_MV, C_MO, C_MI, C_MF, C_GA, C_GB = 0, 512, 1024, 1536, 2048, 2560, 3072, 3584, 3588, 3592, 4616
DFF = 2816
NE = 8
DFE = 1408
EPS = 1e-6
N_CORES = 8
PE_DRAIN = False
SAME_ENG_DIST = 1 << 30
SPARSE_MOE = True

PV_NMG, PV_AMB, PV_NFG, PV_AFB, PV_LB, PV_HNG, PV_CW, PV_CB, PV_MNG, PV_FB, PV_RB = 0, 8, 32, 40, 64, 68, 72, 88, 92, 96, 100
PVL = 108


class Buf:
    __slots__ = ("name", "w", "r")

    def __init__(self, name):
        self.name = name
        self.w = []
        self.r = []


class _Rec:
    def __init__(self):
        self.call = None

    def __getattr__(self, name):
        def f(*a, **k):
            self.call = (name, a, k)
            return self
        return f


def _record(fn):
    r = _Rec()
    fn(r)
    return r.call


class Prog:
    ENGS = ("pe", "act", "dve", "pool", "sp")

    def __init__(self, nc, n_dma_sems=8):
        self.nc = nc
        self.q = {e: [] for e in self.ENGS}
        self.cnt = {e: 0 for e in self.ENGS}
        self.waited = {e: {} for e in self.ENGS}
        self.n_dma_sems = n_dma_sems
        self.dma_rr = {"sp": 0, "pool": 0, "act": 0}
        self.dma_cnt = {}
        self.n_ins = 0

    def _need(self, eng, tok, waits):
        if tok is None:
            return
        key, val = tok
        if key == ("e", eng) and eng == "pe":
            return
        if key == ("e", eng) and (self.cnt[eng] + 1 - val) > SAME_ENG_DIST:
            return
        if self.waited[eng].get(key, 0) >= val:
            return
        self.waited[eng][key] = val
        waits.append((key, val))

    def _deps(self, eng, reads, writes):
        waits = []
        for b in reads:
            for t in b.w:
                self._need(eng, t, waits)
        for b in writes:
            for t in b.w:
                self._need(eng, t, waits)
            for t in b.r:
                self._need(eng, t, waits)
        return waits

    def _commit(self, tok, reads, writes):
        for b in reads:
            b.r.append(tok)
        for b in writes:
            b.w = [tok]
            b.r = []

    def op(self, eng, fn, reads=(), writes=()):
        waits = self._deps(eng, reads, writes)
        self.cnt[eng] += 1
        tok = (("e", eng), self.cnt[eng])
        self.q[eng].append((waits, _record(fn), ("e", eng), 1))
        self._commit(tok, reads, writes)
        self.n_ins += 1
        return tok

    def dma(self, qeng, fn, reads=(), writes=(), group=None):
        if group is not None:
            waits = []
            for t in group["deps"]:
                self._need(qeng, t, waits)
        else:
            waits = self._deps(qeng, reads, writes)
        k = self.dma_rr[qeng]
        self.dma_rr[qeng] = (k + 1) % self.n_dma_sems
        key = ("d", qeng, k)
        prev = self.dma_cnt.get(key, 0)
        if prev:
            self._need(qeng, (key, prev), waits)
        val = prev + 16
        self.dma_cnt[key] = val
        tok = (key, val)
        self.q[qeng].append((waits, _record(fn), key, 16))
        if group is not None:
            group["toks"].append(tok)
        else:
            self._commit(tok, reads, writes)
        self.n_ins += 1
        return tok

    def group_begin(self, bufs):
        deps = []
        for b in bufs:
            deps += list(b.w) + list(b.r)
        return {"deps": deps, "toks": [], "bufs": bufs}

    def group_end(self, g):
        for b in g["bufs"]:
            b.w = list(g["toks"])
            b.r = []

    def final_wait(self, eng, bufs):
        waits = []
        for b in bufs:
            for t in b.w:
                self._need(eng, t, waits)
        self.q[eng].append((waits, None, None, 0))

    def emit(self):
        nc = self.nc
        with contextlib.ExitStack() as st:
            sems = {}
            for e in self.ENGS:
                sems[("e", e)] = st.enter_context(nc.semaphore("s_" + e))
            for key in self.dma_cnt:
                sems[key] = st.enter_context(nc.semaphore("d_%s_%d" % (key[1], key[2])))
            block = st.enter_context(nc.Block())

            def run(eh, lst, drain=False):
                for waits, fn, skey, inc in lst:
                    for key, val in waits:
                        eh.wait_ge(sems[key], val)
                    if drain and waits:
                        eh.drain()
                    if fn is not None:
                        name, a, k = fn
                        getattr(eh, name)(*a, **k).then_inc(sems[skey], inc)

            block.tensor(lambda t: run(t, self.q["pe"], PE_DRAIN))
            block.scalar(lambda t: run(t, self.q["act"]))
            block.vector(lambda t: run(t, self.q["dve"]))
            block.gpsimd(lambda t: run(t, self.q["pool"]))
            block.sync(lambda t: run(t, self.q["sp"]))


def build(NSEQ, S, phases="ABCD", debug=False):
    NT = S // 128
    NTILES = NSEQ * NT
    nc = bass.Bass("TRN2", target_bir_lowering=False)
    dt_in = lambda name, shape: nc.dram_tensor(name, list(shape), F32, kind="ExternalInput").ap()
    x_d = dt_in("x", [NSEQ * S, D])
    cT_d = dt_in("cT", [128, 8, NSEQ])
    pv_d = dt_in("pv", [128, DEPTH * PVL])
    cst_d = dt_in("cst", [128, 1024])
    rst_d = dt_in("rst", [128, 512])
    gb_d = dt_in("gbias", [2 * DEPTH, 128, D])
    fng_d = dt_in("fng", [128, D])
    ada_mix_w = dt_in("ada_mix_w", [DEPTH, D, 3 * D])
    ada_ffn_w = dt_in("ada_ffn_w", [DEPTH, D, 3 * D])
    w_in = dt_in("w_in", [DEPTH, D, INC])
    ml_wq = dt_in("ml_wq", [DEPTH, 4, 128, 64])
    ml_wk = dt_in("ml_wk", [DEPTH, 4, 128, 64])
    w_br_hg = dt_in("w_br_hg", [DEPTH, 512, D])
    w_br_ml = dt_in("w_br_ml", [DEPTH, 512, D])
    w_out = dt_in("w_out", [DEPTH, D, D])
    ffn_w1 = dt_in("ffn_w1", [1, D, DFF])
    ffn_w3 = dt_in("ffn_w3", [1, D, DFF])
    ffn_w2 = dt_in("ffn_w2", [1, DFF, D])
    moe_rw = dt_in("moe_router_w", [1, D, NE])
    moe_w1 = dt_in("moe_w1", [1, NE, D, DFE])
    moe_w3 = dt_in("moe_w3", [1, NE, D, DFE])
    moe_w2 = dt_in("moe_w2", [1, NE, DFE, D])
    out_d = nc.dram_tensor("out", [NSEQ * S, D], F32, kind="ExternalOutput").ap()
    cnt_d = nc.dram_tensor("cnt", [128, NE], F32, kind="ExternalOutput").ap()
    hsc_d = nc.dram_tensor("hsc", [NTILES, 128, 8 * 128], BF16, kind="Internal").ap()
    csc_d = nc.dram_tensor("csc", [NTILES, 128, NE], F32, kind="Internal").ap()
    CAP = max(128, ((NSEQ * S * 2 // NE) * 15 // 8 + 127) // 128 * 128)
    hg_d = nc.dram_tensor("hg", [NE * CAP, D], BF16, kind="Internal").ap()
    y_d = nc.dram_tensor("yex", [NE * CAP, D], F32, kind="Internal").ap()

    dbg = {}
    if debug:
        for nm, shp in (("dbg_hT", [128, 1024]), ("dbg_mod", [128, 16]), ("dbg_gate", [128, 1024]), ("dbg_act", [128, 1408]), ("dbg_xn", [128, 1024]), ("dbg_ob", [128, 1024])):
            dbg[nm] = nc.dram_tensor(nm, shp, F32, kind="ExternalOutput").ap()
    dbgB = []
    P = Prog(nc)
    st = contextlib.ExitStack()
    with st:
        def sb(name, shape, dt=F32):
            return st.enter_context(nc.sbuf_tensor("sb_" + name, list(shape), dt))

        ARENA_N = 67584
        arena = sb("arena", [128, ARENA_N], BF16)
        slotB = [Buf("slot0"), Buf("slot1")]
        X = [sb("X0", [128, D]), sb("X1", [128, D])]
        XB = [Buf("X0"), Buf("X1")]
        xn = sb("xn", [128, D], BF16); xnB = Buf("xn")
        hT = sb("hT", [128, 8, 128], BF16); hTB = Buf("hT")
        cst = sb("cst", [128, 1024]); cstB = Buf("cst")
        identb = sb("identb", [128, 128], BF16)
        trib = sb("trib", [128, 128], BF16)
        bonesb = sb("bonesb", [128, 128], BF16)
        onesb = sb("onesb", [128, 128], BF16)
        caus = sb("caus", [128, 128], BF16)
        m64 = sb("m64", [128, 64])
        rst = sb("rst", [128, 512], BF16);
        pv = sb("pv", [128, DEPTH * PVL]); pvB = Buf("pv")
        lbc = sb("lbc", [128, 2, 4]); lbB = Buf("lbc")
        cT = sb("cT", [128, 8, NSEQ])
        cact = sb("cact", [128, 8, NSEQ], BF16)
        cbc = sb("cbc", [128, 8, 128], BF16); cbcB = Buf("cbc")
        MS = sb("MS", [128, NSEQ, 8]); SH = sb("SH", [128, NSEQ, 8]); modB = Buf("mod")
        GATEb = sb("GATEb", [128, D]); gateB = Buf("gate")
        small = sb("small", [128, 64]);
        T = [sb("T%d" % i, [128, 512]) for i in range(6)]
        TB = [Buf("T%d" % i) for i in range(6)]
        constB = Buf("consts")

        psum_all = st.enter_context(nc.psum_tensor("psum_all", [128, 4096], F32))
        banks = [psum_all[:, i * 512:(i + 1) * 512] for i in range(8)]
        bankB = [Buf("bank%d" % i) for i in range(8)]
        bank_rr = [0]
        bank_n = [6]

        def nb():
            i = bank_rr[0] % bank_n[0]
            bank_rr[0] = (i + 1) % bank_n[0]
            return banks[i], bankB[i]

        def dve(fn, r=(), w=()):
            return P.op("dve", fn, r, w)

        def act(fn, r=(), w=()):
            return P.op("act", fn, r, w)

        def pe(fn, r=(), w=()):
            return P.op("pe", fn, r, w)

        def pool(fn, r=(), w=()):
            return P.op("pool", fn, r, w)

        P.dma("sp", lambda e: e.dma_start(out=cst[:], in_=cst_d), writes=[cstB])
        P.dma("pool", lambda e: e.dma_start(out=rst[:], in_=rst_d), writes=[constB])
        P.dma("sp", lambda e: e.dma_start(out=pv[:], in_=pv_d), writes=[pvB])
        P.dma("sp", lambda e: e.dma_start(out=cT[:], in_=cT_d), writes=[modB])
        dve(lambda e: e.tensor_copy(out=identb[:], in_=cst[:, 0:128]), [cstB], [constB])
        dve(lambda e: e.tensor_copy(out=trib[:], in_=cst[:, 128:256]), [cstB], [constB])
        dve(lambda e: e.tensor_copy(out=bonesb[:], in_=cst[:, 256:384]), [cstB], [constB])
        dve(lambda e: e.tensor_copy(out=caus[:], in_=cst[:, 384:512]), [cstB], [constB])
        dve(lambda e: e.tensor_copy(out=m64[:], in_=cst[:, 512:576]), [cstB], [constB])
        dve(lambda e: e.memset(onesb[:], 1.0), [], [constB])
        strilb = sb("strilb", [128, 128], BF16)
        dve(lambda e: e.tensor_copy(out=strilb[:], in_=cst[:, 576:704]), [cstB], [constB])
        act(lambda e: e.activation(out=cact[:], in_=cT[:], func=AF.Silu), [modB], [modB])

        def wload(dst_ap, src_ap, grp, ncols):
            K = dst_ap.shape[1]
            for k in range(K):
                for c0 in range(0, ncols, 1024):
                    c1 = min(ncols, c0 + 1024)
                    P.dma("pool", (lambda k, c0, c1: (lambda e: e.dma_start(out=dst_ap[:, k, c0:c1], in_=src_ap[:, k, c0:c1])))(k, c0, c1),
                          group=grp)

        def kview(w2d):
            return w2d.rearrange("(k p) n -> p k n", p=128)

        def ada_setup(ada_w_l, l, which, norm_g_col, ada_b_col, gb_row):
            aw = arena[:, 33792:33792 + 8 * 3072].rearrange("p (k n) -> p k n", k=8)
            g = P.group_begin([slotB[1]])
            wload(aw, kview(ada_w_l), g, 3072)
            P.group_end(g)
            bk, bb = nb()
            for j in range(16):
                for k in range(8):
                    pe((lambda j, k: (lambda e: e.matmul(bk[:, j * NSEQ:(j + 1) * NSEQ], lhsT=aw[:, k, j * 128:(j + 1) * 128], rhs=cact[:, k, :], start=(k == 0), stop=(k == 7))))(j, k),
                       [slotB[1], modB], [bb])
            bv = bk[:, 0:16 * NSEQ].rearrange("p (j s) -> p j s", s=NSEQ)
            for s in range(NSEQ):
                dve((lambda s: (lambda e: e.tensor_tensor(out=SH[:, s, :], in0=bv[:, 0:8, s], in1=pv[:, l * PVL + ada_b_col: l * PVL + ada_b_col + 8], op=ALU.add)))(s), [bb, pvB], [modB])
                dve((lambda s: (lambda e: e.tensor_tensor(out=MS[:, s, :], in0=bv[:, 8:16, s], in1=pv[:, l * PVL + ada_b_col + 8: l * PVL + ada_b_col + 16], op=ALU.add)))(s), [bb, pvB], [modB])
                dve((lambda s: (lambda e: e.scalar_tensor_tensor(out=MS[:, s, :], in0=MS[:, s, :], scalar=1.0, in1=pv[:, l * PVL + norm_g_col: l * PVL + norm_g_col + 8], op0=ALU.add, op1=ALU.mult)))(s), [pvB], [modB])
            return aw

        def gate_setup(aw, s, gb_row):
            dve(lambda e: e.tensor_copy(out=cbc[:], in_=cact[:, :, s:s + 1].broadcast_to([128, 8, 128])), [modB], [cbcB])
            P.dma("sp", lambda e: e.dma_start(out=GATEb[:], in_=gb_d[gb_row]), writes=[gateB])
            for half in range(2):
                bk, bb = nb()
                for k in range(8):
                    pe((lambda k, half: (lambda e: e.matmul(bk[:], lhsT=cbc[:, k, :], rhs=aw[:, k, 2048 + half * 512: 2048 + (half + 1) * 512], start=(k == 0), stop=(k == 7))))(k, half),
                       [cbcB, slotB[1]], [bb])
                dve((lambda half, bk: (lambda e: e.tensor_tensor(out=GATEb[:, half * 512:(half + 1) * 512], in0=GATEb[:, half * 512:(half + 1) * 512], in1=bk[:], op=ALU.add)))(half, bk), [bb], [gateB])

        def load_x(ti, slot, src):
            P.dma("sp", lambda e: e.dma_start(out=X[slot][:], in_=src[ti * 128:(ti + 1) * 128, :]), writes=[XB[slot]])

        def norm_mod(slot, s):
            Xs, Xb = X[slot], XB[slot]
            act(lambda e: e.activation(out=xn[:], in_=Xs[:], func=AF.Square, accum_out=small[:, 0:1]), [Xb], [xnB])
            dve(lambda e: e.tensor_scalar(out=small[:, 1:2], in0=small[:, 0:1], scalar1=1.0 / D, scalar2=EPS, op0=ALU.mult, op1=ALU.add), [xnB], [xnB])
            act(lambda e: e.activation(out=small[:, 1:2], in_=small[:, 1:2], func=AF.Ln), [xnB], [xnB])
            act(lambda e: e.activation(out=small[:, 2:3], in_=small[:, 1:2], func=AF.Exp, scale=-0.5), [xnB], [xnB])
            act(lambda e: e.activation(out=xn[:], in_=Xs[:], func=AF.Copy, scale=small[:, 2:3]), [Xb, xnB], [xnB])
            bk, bb = nb()
            bkb = bk[:].bitcast(BF16)
            for k in range(8):
                pe((lambda k: (lambda e: e.transpose(out=bkb[:, k * 128:(k + 1) * 128], in_=xn[:, k * 128:(k + 1) * 128], identity=identb[:])))(k), [xnB, constB], [bb])
            bv = bkb[:, 0:1024].rearrange("p (k t) -> p k t", k=8)
            dve(lambda e: e.tensor_tensor(out=T[0][:].rearrange("p (k t) -> p k t", k=8)[:, :, 0:64], in0=bv[:, :, 0:64], in1=MS[:, s, :].unsqueeze(2).broadcast_to([128, 8, 64]), op=ALU.mult), [bb, modB], [TB[0]])
            dve(lambda e: e.tensor_tensor(out=T[1][:].rearrange("p (k t) -> p k t", k=8)[:, :, 0:64], in0=bv[:, :, 64:128], in1=MS[:, s, :].unsqueeze(2).broadcast_to([128, 8, 64]), op=ALU.mult), [bb, modB], [TB[1]])
            dve(lambda e: e.tensor_tensor(out=hT[:, :, 0:64], in0=T[0][:].rearrange("p (k t) -> p k t", k=8), in1=SH[:, s, :].unsqueeze(2).broadcast_to([128, 8, 64]), op=ALU.add), [TB[0], modB], [hTB])
            dve(lambda e: e.tensor_tensor(out=hT[:, :, 64:128], in0=T[1][:].rearrange("p (k t) -> p k t", k=8), in1=SH[:, s, :].unsqueeze(2).broadcast_to([128, 8, 64]), op=ALU.add), [TB[1], modB], [hTB])

        actT = sb("actT", [128, 11, 128], BF16); actTB = [Buf("actT_g%d" % i) for i in range(3)]
        actT_l = [actT, sb("actT1", [128, 11, 128], BF16)]; actTB_l = [actTB, [Buf("actT1_g%d" % i) for i in range(3)]]
        hT_l = [hT, sb("hT1", [128, 8, 128], BF16)]; hTB_l = [hTB, Buf("hT1")]
        ffn_grp = [0]

        def ffn_views(slot):
            base = slot * 33792
            w1 = arena[:, base:base + 8 * DFE].rearrange("p (k n) -> p k n", k=8)
            w3 = arena[:, base + 8 * DFE: base + 16 * DFE].rearrange("p (k n) -> p k n", k=8)
            w2 = arena[:, base + 16 * DFE: base + 16 * DFE + 11 * D].rearrange("p (k n) -> p k n", k=11)
            return w1, w3, w2

        def ffn_load(slot, w1src, w3src, w2src):
            w1, w3, w2 = ffn_views(slot)
            g = P.group_begin([slotB[slot]])
            wload(w1, kview(w1src), g, DFE)
            wload(w3, kview(w3src), g, DFE)
            wload(w2, kview(w2src), g, D)
            P.group_end(g)

        def ffn_expert(slot, ob, obB, first, last):
            w1, w3, w2 = ffn_views(slot)
            aT, aTB = actT, actTB
            hTc, hTBc = hT, hTB

            def emit_w2_g(g0, nblk, gB):
                for jj in range(nblk):
                    j = g0 + jj
                    for half in range(2):
                        pe((lambda j, half: (lambda e: e.matmul(ob[half][:], lhsT=aT[:, j, :], rhs=w2[:, j, half * 512:(half + 1) * 512], start=(first and j == 0), stop=(last and j == 10))))(j, half),
                           [gB, slotB[slot]], [obB[half]])

            pending = None
            for g0 in range(0, 11, 4):
                nblk = min(4, 11 - g0)
                ffn_grp[0] += 1
                ts = 2 if ffn_grp[0] % 2 == 0 else 5
                b1, b1B = nb()
                b3, b3B = nb()
                for jj in range(nblk):
                    j = g0 + jj
                    for k in range(8):
                        pe((lambda j, jj, k: (lambda e: e.matmul(b1[:, jj * 128:(jj + 1) * 128], lhsT=w1[:, k, j * 128:(j + 1) * 128], rhs=hTc[:, k, :], start=(k == 0), stop=(k == 7))))(j, jj, k), [slotB[slot], hTBc], [b1B])
                    for k in range(8):
                        pe((lambda j, jj, k: (lambda e: e.matmul(b3[:, jj * 128:(jj + 1) * 128], lhsT=w3[:, k, j * 128:(j + 1) * 128], rhs=hTc[:, k, :], start=(k == 0), stop=(k == 7))))(j, jj, k), [slotB[slot], hTBc], [b3B])
                n = nblk * 128
                act((lambda n, b1: (lambda e: e.activation(out=T[ts][:, 0:n], in_=b1[:, 0:n], func=AF.Silu)))(n, b1), [b1B], [TB[ts]])
                gB = aTB[g0 // 4]
                dve((lambda n, b3, g0: (lambda e: e.tensor_tensor(out=aT[:, g0:g0 + n // 128, :].rearrange("p j t -> p (j t)"), in0=T[ts][:, 0:n], in1=b3[:, 0:n], op=ALU.mult)))(n, b3, g0), [TB[ts], b3B], [gB])
                if pending is not None:
                    emit_w2_g(*pending)
                pending = (g0, nblk, gB)
            emit_w2_g(*pending)

        def residual(slot, ob, obB, comb_ap=None, combB=None):
            Xs, Xb = X[slot], XB[slot]
            for half in range(2):
                sl = slice(half * 512, (half + 1) * 512)
                if comb_ap is None:
                    dve((lambda half, sl: (lambda e: e.tensor_tensor(out=T[3 + half][:], in0=ob[half][:], in1=GATEb[:, sl], op=ALU.mult)))(half, sl), [obB[half], gateB], [TB[3 + half]])
                else:
                    dve((lambda half, sl: (lambda e: e.scalar_tensor_tensor(out=T[3 + half][:], in0=ob[half][:], scalar=comb_ap, in1=GATEb[:, sl], op0=ALU.mult, op1=ALU.mult)))(half, sl), [obB[half], gateB, combB], [TB[3 + half]])
                pool((lambda half, sl: (lambda e: e.tensor_tensor(out=Xs[:, sl], in0=Xs[:, sl], in1=T[3 + half][:], op=ALU.add)))(half, sl), [TB[3 + half], Xb], [Xb])

        def store_x(ti, slot, dstB):
            P.dma("sp", lambda e: e.dma_start(out=out_d[ti * 128:(ti + 1) * 128, :], in_=X[slot][:]), reads=[XB[slot]], writes=[dstB])

        xdB = [Buf("xd%d" % i) for i in range(NTILES)]

        def phase_dense(l, src):
            aw = ada_setup(ada_ffn_w[l], l, "ffn", PV_NFG, PV_AFB, 2 * l + 1)
            gate_rows(aw, 2 * l + 1)
            ffn_load(0, ffn_w1[0][:, 0:DFE], ffn_w3[0][:, 0:DFE], ffn_w2[0][0:DFE, :])
            ffn_load(1, ffn_w1[0][:, DFE:DFF], ffn_w3[0][:, DFE:DFF], ffn_w2[0][DFE:DFF, :])
            bank_n[0] = 4
            load_x(0, 0, src)
            for ti in range(NTILES):
                s = ti // NT
                slot = ti % 2
                set_par(slot)
                if ti + 1 < NTILES:
                    load_x(ti + 1, (ti + 1) % 2, src)
                if ti % NT == 0:
                    gate_fetch(s)
                norm_mod(slot, s)
                ob0, ob0B = banks[4 + 2 * slot], bankB[4 + 2 * slot]
                ob1, ob1B = banks[5 + 2 * slot], bankB[5 + 2 * slot]
                ffn_expert(0, [ob0, ob1], [ob0B, ob1B], True, False)
                ffn_expert(1, [ob0, ob1], [ob0B, ob1B], False, True)
                residual(slot, [ob0, ob1], [ob0B, ob1B])
                store_x(ti, slot, xdB[ti])
            bank_n[0] = 6

        gsc_d = nc.dram_tensor("gsc", [NSEQ, 128, D], F32, kind="Internal").ap()
        gscB = [Buf("gsc%d" % s) for s in range(NSEQ)]

        def gate_rows(aw, gb_row):
            for s in range(NSEQ):
                gate_setup(aw, s, gb_row)
                P.dma("sp", (lambda s: (lambda e: e.dma_start(out=gsc_d[s], in_=GATEb[:])))(s), reads=[gateB], writes=[gscB[s]])

        def gate_fetch(s):
            P.dma("sp", lambda e: e.dma_start(out=GATEb[:], in_=gsc_d[s]), reads=[gscB[s]], writes=[gateB])

        def load_x_dep(ti, slot, src):
            if src is out_d:
                P.dma("sp", lambda e: e.dma_start(out=X[slot][:], in_=src[ti * 128:(ti + 1) * 128, :]), reads=[xdB[ti]], writes=[XB[slot]])
            else:
                P.dma("sp", lambda e: e.dma_start(out=X[slot][:], in_=src[ti * 128:(ti + 1) * 128, :]), writes=[XB[slot]])
        load_x = load_x_dep

        rw = sb("rw", [128, 8, 2 * NE], BF16); rwB = Buf("rw")
        rw32 = sb("rw32", [128, 8, NE])
        hlo = sb("hlo_mrg", [128, 8, 128], BF16); hloB = Buf("hlo")
        comb = sb("comb", [128, 4 * NE]); combB = Buf("comb")
        comb_l = [comb, sb("comb1", [128, 4 * NE])]; combB_l = [combB, Buf("comb1")]
        rt = sb("rt", [128, 64]); rtB = Buf("rt")
        basec = sb("basec", [128, NE])
        capmax = sb("capmax", [128, NE])
        Mb = sb("Mb", [128, NE], BF16)
        RT = sb("RT", [128, NTILES, 4])
        idxu = sb("idxu", [128, NTILES, 2], mybir.dt.uint32)

        def set_par(p):
            nonlocal hT, hTB, actT, actTB, comb, combB
            hT, hTB = hT_l[p], hTB_l[p]
            actT, actTB = actT_l[p], actTB_l[p]
            comb, combB = comb_l[p], combB_l[p]
        fng = cst; fngB = cstB
        hT32 = None

        def router(slot, s, ti):
            for hf in range(2):
                dve((lambda hf: (lambda e: e.tensor_tensor(out=T[hf][:].rearrange("p (k t) -> p k t", k=8), in0=T[hf][:].rearrange("p (k t) -> p k t", k=8), in1=SH[:, s, :].unsqueeze(2).broadcast_to([128, 8, 64]), op=ALU.add)))(hf), [modB, hTB], [TB[hf]])
                dve((lambda hf: (lambda e: e.tensor_tensor(out=hlo[:, :, hf * 64:(hf + 1) * 64], in0=T[hf][:].rearrange("p (k t) -> p k t", k=8), in1=hT[:, :, hf * 64:(hf + 1) * 64], op=ALU.subtract)))(hf), [TB[hf], hTB], [hloB])
            bk, bb = nb()
            n = 0
            for (lh, rc) in ((hT, 0), (hT, NE), (hlo, 0)):
                for k in range(8):
                    pe((lambda lh, rc, k, n: (lambda e: e.matmul(bk[:, 0:NE], lhsT=lh[:, k, :], rhs=rw[:, k, rc:rc + NE], start=(n == 0), stop=(n == 23))))(lh, rc, k, n), [hTB, hloB, rwB], [bb])
                    n += 1
            lg = comb[:, 8:16]
            dve(lambda e: e.tensor_tensor(out=lg, in0=bk[:, 0:NE], in1=pv[:, PVL + PV_RB: PVL + PV_RB + NE], op=ALU.add), [bb, pvB], [combB])
            dve(lambda e: e.max(out=comb[:, 16:24], in_=lg), [], [combB])
            dve(lambda e: e.tensor_scalar(out=comb[:, 24:32], in0=lg, scalar1=comb[:, 16:17], scalar2=None, op0=ALU.subtract), [], [combB])
            act(lambda e: e.activation(out=comb[:, 24:32], in_=comb[:, 24:32], func=AF.Exp), [combB], [combB])
            dve(lambda e: e.tensor_scalar(out=comb[:, 0:8], in0=lg, scalar1=comb[:, 17:18], scalar2=None, op0=ALU.is_ge), [combB], [combB])
            dve(lambda e: e.tensor_tensor(out=comb[:, 0:8], in0=comb[:, 0:8], in1=comb[:, 24:32], op=ALU.mult), [], [combB])
            dve(lambda e: e.reduce_sum(out=small[:, 4:5], in_=comb[:, 0:8], axis=AX.X), [], [combB])
            dve(lambda e: e.reciprocal(out=small[:, 5:6], in_=small[:, 4:5]), [], [combB])
            dve(lambda e: e.tensor_scalar(out=comb[:, 0:8], in0=comb[:, 0:8], scalar1=small[:, 5:6], scalar2=None, op0=ALU.mult), [], [combB])

        hscB = [Buf("hsc%d" % i) for i in range(NTILES)]
        cscB = [Buf("csc%d" % i) for i in range(NTILES)]
        outB = [Buf("out%d" % i) for i in range(NTILES)]

        def phase_moe(l, src):
            aw = ada_setup(ada_ffn_w[l], l, "ffn", PV_NFG, PV_AFB, 2 * l + 1)
            gate_rows(aw, 2 * l + 1)
            P.dma("sp", lambda e: e.dma_start(out=rw32[:], in_=kview(moe_rw[0])), writes=[rwB])
            dve(lambda e: e.tensor_copy(out=rw[:, :, 0:NE], in_=rw32[:]), [rwB], [rwB])
            dve(lambda e: e.tensor_tensor(out=rw32[:], in0=rw32[:], in1=rw[:, :, 0:NE], op=ALU.subtract), [], [rwB])
            dve(lambda e: e.tensor_copy(out=rw[:, :, NE:2 * NE], in_=rw32[:]), [], [rwB])
            P.dma("sp", lambda e: e.dma_start(out=fng[:], in_=fng_d), writes=[fngB])
            ffn_load(0, moe_w1[0, 0], moe_w3[0, 0], moe_w2[0, 0])
            bank_n[0] = 4
            units = [(ex, ti) for ex in range(NE) for ti in range(NTILES)]

            def loads(u):
                ex, ti = units[u]
                p = u % 2
                set_par(p)
                load_x(ti, p, src if ex == 0 else out_d)
                if ex > 0:
                    P.dma("sp", lambda e: e.dma_start(out=hT[:].rearrange("p k t -> p (k t)"), in_=hsc_d[ti]), reads=[hscB[ti]], writes=[hTB])
                    P.dma("sp", lambda e: e.dma_start(out=comb[:, 0:NE], in_=csc_d[ti]), reads=[cscB[ti]], writes=[combB])

            loads(0)
            for u, (ex, ti) in enumerate(units):
                slot_w = ex % 2
                if ti == 0 and ex + 1 < NE:
                    ffn_load((ex + 1) % 2, moe_w1[0, ex + 1], moe_w3[0, ex + 1], moe_w2[0, ex + 1])
                s = ti // NT
                slot = u % 2
                if u + 1 < len(units):
                    loads(u + 1)
                set_par(slot)
                if ti % NT == 0:
                    gate_fetch(s)
                if ex == 0:
                    norm_mod(slot, s)
                    router(slot, s, ti)
                    P.dma("sp", lambda e: e.dma_start(out=hsc_d[ti], in_=hT[:].rearrange("p k t -> p (k t)")), reads=[hTB], writes=[hscB[ti]])
                    P.dma("sp", lambda e: e.dma_start(out=csc_d[ti], in_=comb[:, 0:NE]), reads=[combB], writes=[cscB[ti]])
                ob0, ob0B = banks[4 + 2 * slot], bankB[4 + 2 * slot]
                ob1, ob1B = banks[5 + 2 * slot], bankB[5 + 2 * slot]
                ffn_expert(slot_w, [ob0, ob1], [ob0B, ob1B], True, True)
                residual(slot, [ob0, ob1], [ob0B, ob1B], comb_ap=comb[:, ex:ex + 1], combB=combB)
                if ex < NE - 1:
                    store_x(ti, slot, xdB[ti])
                else:
                    final_norm(ti, slot)
            bank_n[0] = 6

        def phase_moe_sparse(l, src):
            from concourse.bass import IndirectOffsetOnAxis
            U32 = mybir.dt.uint32
            aw = ada_setup(ada_ffn_w[l], l, "ffn", PV_NFG, PV_AFB, 2 * l + 1)
            gate_rows(aw, 2 * l + 1)
            P.dma("sp", lambda e: e.dma_start(out=rw32[:], in_=kview(moe_rw[0])), writes=[rwB])
            dve(lambda e: e.tensor_copy(out=rw[:, :, 0:NE], in_=rw32[:]), [rwB], [rwB])
            dve(lambda e: e.tensor_tensor(out=rw32[:], in0=rw32[:], in1=rw[:, :, 0:NE], op=ALU.subtract), [], [rwB])
            dve(lambda e: e.tensor_copy(out=rw[:, :, NE:2 * NE], in_=rw32[:]), [], [rwB])
            ffn_load(0, moe_w1[0, 0], moe_w3[0, 0], moe_w2[0, 0])
            bank_n[0] = 4
            for e_ in range(NE):
                dve((lambda e_: (lambda e: e.memset(basec[:, e_:e_ + 1], float(e_ * CAP))))(e_), [], [rtB])
                dve((lambda e_: (lambda e: e.memset(capmax[:, e_:e_ + 1], float(e_ * CAP + CAP - 1))))(e_), [], [rtB])
            hgB = Buf("Hg")
            hg_grp = P.group_begin([hgB])
            load_x(0, 0, src)
            for ti in range(NTILES):
                s = ti // NT
                slot = ti % 2
                set_par(slot)
                if ti + 1 < NTILES:
                    load_x(ti + 1, (ti + 1) % 2, src)
                norm_mod(slot, s)
                for hf in range(2):
                    dve((lambda hf: (lambda e: e.tensor_tensor(out=T[hf][:].rearrange("p (k t) -> p k t", k=8), in0=T[hf][:].rearrange("p (k t) -> p k t", k=8), in1=SH[:, s, :].unsqueeze(2).broadcast_to([128, 8, 64]), op=ALU.add)))(hf), [modB, hTB], [TB[hf]])
                    dve((lambda hf: (lambda e: e.tensor_tensor(out=hlo[:, :, hf * 64:(hf + 1) * 64], in0=T[hf][:].rearrange("p (k t) -> p k t", k=8), in1=hT[:, :, hf * 64:(hf + 1) * 64], op=ALU.subtract)))(hf), [TB[hf], hTB], [hloB])
                bk, bb = nb()
                n = 0
                for (lh, rc) in ((hT, 0), (hT, NE), (hlo, 0)):
                    for k in range(8):
                        pe((lambda lh, rc, k, n: (lambda e: e.matmul(bk[:, 0:NE], lhsT=lh[:, k, :], rhs=rw[:, k, rc:rc + NE], start=(n == 0), stop=(n == 23))))(lh, rc, k, n), [hTB, hloB, rwB], [bb])
                        n += 1
                lg = rt[:, 48:56]
                dve(lambda e: e.tensor_tensor(out=lg, in0=bk[:, 0:NE], in1=pv[:, PVL + PV_RB: PVL + PV_RB + NE], op=ALU.add), [bb, pvB], [rtB])
                dve(lambda e: e.max(out=rt[:, 0:8], in_=lg), [], [rtB])
                dve(lambda e: e.tensor_scalar(out=rt[:, 8:16], in0=lg, scalar1=rt[:, 0:1], scalar2=None, op0=ALU.is_equal), [], [rtB])
                dve(lambda e: e.tensor_scalar(out=rt[:, 16:24], in0=lg, scalar1=rt[:, 1:2], scalar2=None, op0=ALU.is_equal), [], [rtB])
                dve(lambda e: e.tensor_tensor(out=Mb[:], in0=rt[:, 8:16], in1=rt[:, 16:24], op=ALU.add), [], [rtB])
                bp, bpB = nb()
                pe(lambda e: e.matmul(bp[:, 0:8], lhsT=strilb[:], rhs=Mb[:], start=True, stop=True), [rtB, constB], [bpB])
                pe(lambda e: e.matmul(bp[:, 8:16], lhsT=onesb[:], rhs=Mb[:], start=True, stop=True), [rtB, constB], [bpB])
                dve(lambda e: e.tensor_tensor(out=rt[:, 24:32], in0=bp[:, 0:8], in1=basec[:], op=ALU.add), [bpB], [rtB])
                dve(lambda e: e.tensor_tensor(out=rt[:, 24:32], in0=rt[:, 24:32], in1=capmax[:], op=ALU.min), [], [rtB])
                dve(lambda e: e.tensor_tensor(out=basec[:], in0=basec[:], in1=bp[:, 8:16], op=ALU.add), [bpB], [rtB])
                dve(lambda e: e.tensor_tensor(out=rt[:, 32:40], in0=rt[:, 8:16], in1=rt[:, 24:32], op=ALU.mult), [], [rtB])
                dve(lambda e: e.reduce_sum(out=RT[:, ti, 0:1], in_=rt[:, 32:40], axis=AX.X), [], [rtB])
                dve(lambda e: e.tensor_tensor(out=rt[:, 32:40], in0=rt[:, 16:24], in1=rt[:, 24:32], op=ALU.mult), [], [rtB])
                dve(lambda e: e.reduce_sum(out=RT[:, ti, 1:2], in_=rt[:, 32:40], axis=AX.X), [], [rtB])
                dve(lambda e: e.tensor_copy(out=idxu[:, ti, :], in_=RT[:, ti, 0:2]), [], [rtB])
                dve(lambda e: e.tensor_tensor(out=rt[:, 40:41], in0=rt[:, 1:2], in1=rt[:, 0:1], op=ALU.subtract), [], [rtB])
                act(lambda e: e.activation(out=rt[:, 41:42], in_=rt[:, 40:41], func=AF.Exp), [rtB], [rtB])
                dve(lambda e: e.tensor_scalar(out=rt[:, 42:43], in0=rt[:, 41:42], scalar1=1.0, scalar2=None, op0=ALU.add), [], [rtB])
                dve(lambda e: e.reciprocal(out=RT[:, ti, 2:3], in_=rt[:, 42:43]), [], [rtB])
                dve(lambda e: e.tensor_tensor(out=RT[:, ti, 3:4], in0=rt[:, 41:42], in1=RT[:, ti, 2:3], op=ALU.mult), [], [rtB])
                bt, btB = nb()
                btb = bt[:].bitcast(BF16)
                for k in range(8):
                    pe((lambda k: (lambda e: e.transpose(out=btb[:, k * 128:(k + 1) * 128], in_=hT[:, k, :], identity=identb[:])))(k), [hTB, constB], [btB])
                act(lambda e: e.activation(out=xn[:], in_=btb[:, 0:1024], func=AF.Copy), [btB], [xnB])
                for k in range(2):
                    P.dma("pool", (lambda k: (lambda e: e.indirect_dma_start(out=hg_d, out_offset=IndirectOffsetOnAxis(ap=idxu[:, ti, k:k + 1], axis=0), in_=xn[:], in_offset=None)))(k), group=hg_grp)
                    for t_ in list(xnB.w) + list(rtB.w):
                        P._need("pool", t_, P.q["pool"][-1][0])
                    xnB.r.append(hg_grp["toks"][-1])
                    rtB.r.append(hg_grp["toks"][-1])
            P.group_end(hg_grp)
            cntB = Buf("cnt"); dbgB.append(cntB)
            P.dma("sp", lambda e: e.dma_start(out=cnt_d, in_=basec[:]), reads=[rtB], writes=[cntB])
            yB = Buf("Y")
            y_grp = P.group_begin([yB])
            units = [(ex, t) for ex in range(NE) for t in range(CAP // 128)]
            stage = [xn[:], hlo[:].rearrange("p k t -> p (k t)")]
            stageB = [xnB, hloB]

            def loads2(u):
                ex, t = units[u]
                p = u % 2
                r0 = ex * CAP + t * 128
                P.dma("sp", lambda e: e.dma_start(out=stage[p], in_=hg_d[r0:r0 + 128, :]), reads=[hgB], writes=[stageB[p]])

            loads2(0)
            for u, (ex, t) in enumerate(units):
                slot_w = ex % 2
                if t == 0 and ex + 1 < NE:
                    ffn_load((ex + 1) % 2, moe_w1[0, ex + 1], moe_w3[0, ex + 1], moe_w2[0, ex + 1])
                p = u % 2
                if u + 1 < len(units):
                    loads2(u + 1)
                set_par(p)
                bt, btB = nb()
                btb = bt[:].bitcast(BF16)
                for k in range(8):
                    pe((lambda k: (lambda e: e.transpose(out=btb[:, k * 128:(k + 1) * 128], in_=stage[p][:, k * 128:(k + 1) * 128], identity=identb[:])))(k), [stageB[p], constB], [btB])
                act(lambda e: e.activation(out=hT[:].rearrange("p k t -> p (k t)"), in_=btb[:, 0:1024], func=AF.Copy), [btB], [hTB])
                ob0, ob0B = banks[4 + 2 * p], bankB[4 + 2 * p]
                ob1, ob1B = banks[5 + 2 * p], bankB[5 + 2 * p]
                ffn_expert(slot_w, [ob0, ob1], [ob0B, ob1B], True, True)
                act(lambda e: e.activation(out=X[p][:, 0:512], in_=ob0[:], func=AF.Copy), [ob0B], [XB[p]])
                dve(lambda e: e.tensor_copy(out=X[p][:, 512:1024], in_=ob1[:]), [ob1B], [XB[p]])
                r0 = ex * CAP + t * 128
                P.dma("sp", lambda e: e.dma_start(out=y_d[r0:r0 + 128, :], in_=X[p][:]), group=y_grp)
                for t_ in list(XB[p].w):
                    P._need("sp", t_, P.q["sp"][-1][0])
                XB[p].r.append(y_grp["toks"][-1])
            P.group_end(y_grp)
            bank_n[0] = 6
            P.dma("sp", lambda e: e.dma_start(out=fng[:], in_=fng_d), writes=[fngB])
            Yv = [[arena[:, (pp * 2 + k) * 2048:(pp * 2 + k + 1) * 2048].bitcast(F32) for k in range(2)] for pp in range(2)]
            YvB = [[Buf("Yv%d%d" % (pp, k)) for k in range(2)] for pp in range(2)]
            for pp in range(2):
                for k in range(2):
                    for sbuf_ in slotB:
                        YvB[pp][k].r += list(sbuf_.r) + list(sbuf_.w)

            def loads3(ti):
                p = ti % 2
                load_x(ti, p, src)
                for k in range(2):
                    P.dma("pool", (lambda k: (lambda e: e.indirect_dma_start(out=Yv[p][k], out_offset=None, in_=y_d, in_offset=IndirectOffsetOnAxis(ap=idxu[:, ti, k:k + 1], axis=0))))(k), reads=[yB, rtB], writes=[YvB[p][k]])

            loads3(0)
            for ti in range(NTILES):
                s = ti // NT
                p = ti % 2
                if ti + 1 < NTILES:
                    loads3(ti + 1)
                if ti % NT == 0:
                    gate_fetch(s)
                Xs, Xb = X[p], XB[p]
                for half in range(2):
                    sl = slice(half * 512, (half + 1) * 512)
                    dve((lambda sl, half: (lambda e: e.tensor_scalar(out=T[half][:], in0=Yv[p][0][:, sl], scalar1=RT[:, ti, 2:3], scalar2=None, op0=ALU.mult)))(sl, half), [YvB[p][0], rtB], [TB[half]])
                    dve((lambda sl, half: (lambda e: e.scalar_tensor_tensor(out=T[half][:], in0=Yv[p][1][:, sl], scalar=RT[:, ti, 3:4], in1=T[half][:], op0=ALU.mult, op1=ALU.add)))(sl, half), [YvB[p][1], rtB], [TB[half]])
                    dve((lambda sl, half: (lambda e: e.tensor_tensor(out=T[half][:], in0=T[half][:], in1=GATEb[:, sl], op=ALU.mult)))(sl, half), [gateB], [TB[half]])
                    pool((lambda sl, half: (lambda e: e.tensor_tensor(out=Xs[:, sl], in0=Xs[:, sl], in1=T[half][:], op=ALU.add)))(sl, half), [TB[half], Xb], [Xb])
                final_norm(ti, p)

        def final_norm(ti, slot):
            Xs, Xb = X[slot], XB[slot]
            act(lambda e: e.activation(out=xn[:], in_=Xs[:], func=AF.Square, accum_out=small[:, 0:1]), [Xb], [xnB])
            dve(lambda e: e.tensor_scalar(out=small[:, 1:2], in0=small[:, 0:1], scalar1=1.0 / D, scalar2=EPS, op0=ALU.mult, op1=ALU.add), [xnB], [xnB])
            act(lambda e: e.activation(out=small[:, 1:2], in_=small[:, 1:2], func=AF.Ln), [xnB], [xnB])
            act(lambda e: e.activation(out=small[:, 2:3], in_=small[:, 1:2], func=AF.Exp, scale=-0.5), [xnB], [xnB])
            dve(lambda e: e.scalar_tensor_tensor(out=Xs[:], in0=Xs[:], scalar=small[:, 2:3], in1=fng[:], op0=ALU.mult, op1=ALU.mult), [xnB, fngB], [Xb])
            P.dma("sp", lambda e: e.dma_start(out=out_d[ti * 128:(ti + 1) * 128, :], in_=Xs[:]), reads=[Xb], writes=[xdB[ti], outB[ti]])

        def mixer_bufs():
            pass

        if "A" in phases or "C" in phases:
            qt = sb("qt", [128, 512], BF16); kt = sb("kt", [128, 512], BF16); qE = sb("qE", [128, 512], BF16); kdT = sb("kdT", [128, 512], BF16)
            hgB = Buf("hgops")
            vtok = sb("vtok", [128, 512], BF16); vtokB = Buf("vtok")
            kdtok = sb("kdtok", [128, 512], BF16); kdtokB = Buf("kdtok")
            Pm = sb("Pm", [128, 4, 64], BF16); PmB = Buf("Pm")
            S32 = sb("S32", [128, 4, 128]); Sbf = sb("Sbf", [128, 4, 128], BF16); SB_ = Buf("S")
            Gs = sb("Gs", [128, 512], BF16); GsB = Buf("Gs")
            yhgT = sb("yhgT", [128, 4, 128], BF16); yhgB = Buf("yhgT")
            ymlT = sb("ymlT", [128, 4, 128], BF16); ymlB = Buf("ymlT")
            eA = sb("eA", [128, 16]); eAB = Buf("eA")
            U = sb("U", [128, 4, 131]); UB = Buf("U")
            ucT = sb("ucT", [128, 4, 128], BF16); ucB = Buf("ucT")
            qTm = sb("qTm", [64, 4, 128], BF16); kTm = sb("kTm", [64, 4, 128], BF16); qkB = Buf("qkm")
            ktok = sb("ktok", [128, 4, 64], BF16); ktokB = Buf("ktok")
            vaug = sb("vaug", [128, 4, 130], BF16); vaugB = Buf("vaug")
            mosg = sb("mosg", [128, 512], BF16); mosgB = Buf("mosg")
            _a1 = actT_l[1][:].rearrange("p j t -> p (j t)")
            Pml = _a1[:, 512:1024].rearrange("p (h t) -> p h t", h=4); PmlB = actTB_l[1][0]
            C32 = sb("C32", [64, 4, 130]); Cbf = sb("Cbf", [64, 4, 130], BF16); CB_ = Buf("C")
            gts = sb("gts", [128, 64]); gtsB = Buf("gts")
            gthl = sb("gthl", [128, 8], BF16);
            yml = _a1[:, 0:512]; ymlTokB = actTB_l[1][0]
            SGA = hT_l[1]; SGB = actT_l[0][:, 0:8, :]; sgB = hTB_l[1]; sgB2 = actTB_l[0][0]; sgB3 = actTB_l[0][1]
            mrg = hlo; mrgB = hloB
            wqk = sb("wqk", [128, 2, 4, 64], BF16); wqkB = Buf("wqk")

        def phase_mixer(l, src):
            aw = ada_setup(ada_mix_w[l], l, "mix", PV_NMG, PV_AMB, 2 * l)
            gate_rows(aw, 2 * l)
            Win = arena[:, 0:8 * INC].rearrange("p (k n) -> p k n", k=8)
            o = 8 * INC
            Wbh = arena[:, o:o + 4096].rearrange("p (k n) -> p k n", k=4)
            Wbm = arena[:, o + 4096:o + 8192].rearrange("p (k n) -> p k n", k=4)
            Wo = arena[:, o + 8192:o + 16384].rearrange("p (k n) -> p k n", k=8)
            WB = slotB
            g = P.group_begin(WB + [wqkB])
            wload(Win, kview(w_in[l]), g, INC)
            wload(Wbh, kview(w_br_hg[l]), g, D)
            wload(Wbm, kview(w_br_ml[l]), g, D)
            wload(Wo, kview(w_out[l]), g, D)
            for hh in range(4):
                P.dma("pool", (lambda hh: (lambda e: e.dma_start(out=wqk[:, 0, hh, :], in_=ml_wq[l, hh])))(hh), group=g)
                P.dma("pool", (lambda hh: (lambda e: e.dma_start(out=wqk[:, 1, hh, :], in_=ml_wk[l, hh])))(hh), group=g)
            P.group_end(g)
            if l == 0:
                dve(lambda e: e.memset(lbc[:, 0, :], 0.0), [], [lbB])
                dve(lambda e: e.memset(lbc[:, 1, :], 1.0), [], [lbB])
            else:
                dve(lambda e: e.tensor_tensor(out=lbc[:, 0, :], in0=pv[:, PVL + PV_LB:PVL + PV_LB + 4], in1=pv[:, PV_LB:PV_LB + 4], op=ALU.subtract), [pvB], [lbB])
                act(lambda e: e.activation(out=lbc[:, 0, :], in_=lbc[:, 0, :], func=AF.Sigmoid), [], [lbB])
                dve(lambda e: e.tensor_scalar(out=lbc[:, 1, :], in0=lbc[:, 0, :], scalar1=-1.0, scalar2=1.0, op0=ALU.mult, op1=ALU.add), [], [lbB])
            pvl = l * PVL

            def proj_fm(c0, nblk, bk, bb):
                for j in range(nblk):
                    for k in range(8):
                        pe((lambda j, k: (lambda e: e.matmul(bk[:, j * 128:(j + 1) * 128], lhsT=Win[:, k, c0 + j * 128:c0 + (j + 1) * 128], rhs=hT[:, k, :], start=(k == 0), stop=(k == 7))))(j, k), [WB[0], hTB], [bb])

            def proj_tm(c0, n, bk, bb):
                for k in range(8):
                    pe((lambda k: (lambda e: e.matmul(bk[:, 0:n], lhsT=hT[:, k, :], rhs=Win[:, k, c0:c0 + n], start=(k == 0), stop=(k == 7))))(k), [WB[0], hTB], [bb])

            set_par(0)
            bank_n[0] = 6
            for ti in range(NTILES):
                s = ti // NT
                slot = ti % 2
                first = (ti % NT == 0)
                if first:
                    gate_fetch(s)
                    dve(lambda e: e.memset(S32[:], 0.0), [], [SB_])
                    dve(lambda e: e.memset(Sbf[:], 0.0), [], [SB_])
                    dve(lambda e: e.memset(C32[:], 0.0), [], [CB_])
                    dve(lambda e: e.memset(Cbf[:], 0.0), [], [CB_])
                    dve(lambda e: e.memset(U[:], 0.0), [], [UB])
                load_x(ti, slot, src)
                norm_mod(slot, s)

                bq, bqB = nb(); proj_fm(C_Q, 4, bq, bqB)
                bf, bfB = nb(); proj_fm(C_F, 4, bf, bfB)
                bg, bgB = nb(); proj_fm(C_G, 4, bg, bgB)
                bv, bvB = nb(); proj_tm(C_I, 512, bv, bvB)
                Q, Fb, LF, KK, E1, E2 = T[0], T[1], T[2], T[3], T[4], T[5]
                act(lambda e: e.activation(out=Q[:], in_=bq[:], func=AF.Silu), [bqB], [TB[0]])
                act(lambda e: e.activation(out=Gs[:], in_=bg[:], func=AF.Silu), [bgB], [GsB])
                act(lambda e: e.activation(out=Fb[:], in_=bf[:], func=AF.Sigmoid), [bfB], [TB[1]])
                act(lambda e: e.activation(out=vtok[:], in_=bv[:], func=AF.Copy), [bvB], [vtokB])
                bu, buB = nb(); proj_fm(C_MU, 4, bu, buB)
                bmv, bmvB = nb(); proj_tm(C_MV, 512, bmv, bmvB)
                bmo, bmoB = nb(); proj_tm(C_MO, 512, bmo, bmoB)
                bgt, bgtB = nb(); proj_tm(C_MI, 8, bgt, bgtB)
                act(lambda e: e.activation(out=U[:, :, 3:131], in_=bu[:].rearrange("p (h t) -> p h t", h=4), func=AF.Copy), [buB], [UB])
                act(lambda e: e.activation(out=mosg[:], in_=bmo[:], func=AF.Sigmoid), [bmoB], [mosgB])
                act(lambda e: e.activation(out=vaug[:, :, 0:128], in_=bmv[:].rearrange("p (h t) -> p h t", h=4), func=AF.Copy), [bmvB], [vaugB])
                dve(lambda e: e.memset(vaug[:, :, 128:130], 1.0), [], [vaugB])
                dve(lambda e: e.tensor_copy(out=gts[:, 0:4], in_=bgt[:, 0:4]), [bgtB], [gtsB])
                dve(lambda e: e.tensor_tensor(out=gts[:, 4:8], in0=bgt[:, 4:8], in1=pv[:, pvl + PV_FB: pvl + PV_FB + 4], op=ALU.add), [bgtB, pvB], [gtsB])
                for (c0, SG) in ((C_GA, SGA), (C_GB, SGB)):
                    for half in range(2):
                        bgx, bgxB = nb()
                        proj_fm(c0 + half * 512, 4, bgx, bgxB)
                        act((lambda SG, half, bgx: (lambda e: e.activation(out=SG[:, half * 4:(half + 1) * 4, :].rearrange("p k t -> p (k t)"), in_=bgx[:], func=AF.Sigmoid)))(SG, half, bgx), [bgxB], [sgB, sgB2, sgB3])
                rrH = [0]; rrM = [0]

                def nbH():
                    i = rrH[0] % 2
                    rrH[0] += 1
                    return banks[i], bankB[i]

                def nbM():
                    i = 2 + rrM[0] % 3
                    rrM[0] += 1
                    return banks[i], bankB[i]

                def chainH():
                    for hh in range(4):
                        dve((lambda hh: (lambda e: e.tensor_scalar(out=Fb[:, hh * 128:(hh + 1) * 128], in0=Fb[:, hh * 128:(hh + 1) * 128], scalar1=lbc[:, 1, hh:hh + 1], scalar2=lbc[:, 0, hh:hh + 1], op0=ALU.mult, op1=ALU.add)))(hh), [lbB], [TB[1]])
                        yield
                    act(lambda e: e.activation(out=LF[:], in_=Fb[:], func=AF.Ln), [TB[1]], [TB[2]])
                    yield
                    dve(lambda e: e.tensor_scalar(out=KK[:], in0=Fb[:], scalar1=-1.0, scalar2=1.0, op0=ALU.mult, op1=ALU.add), [TB[1]], [TB[3]])
                    yield
                    A_ = Fb
                    dve(lambda e: e.tensor_tensor_scan(out=A_[:], data0=rst[:], data1=LF[:], initial=0.0, op0=ALU.mult, op1=ALU.add), [TB[2], constB], [TB[1]])
                    yield
                    A4 = A_[:].rearrange("p (g t) -> p g t", t=64)
                    Dd = LF
                    dve(lambda e: e.tensor_tensor(out=Dd[:].rearrange("p (g t) -> p g t", t=64), in0=A4, in1=A4[:, :, 31:32].broadcast_to([128, 8, 64]), op=ALU.subtract), [TB[1]], [TB[2]])
                    yield
                    dve(lambda e: e.tensor_copy(out=eA[:, 0:8], in_=A4[:, :, 31]), [TB[1]], [eAB])
                    yield
                    dve(lambda e: e.tensor_copy(out=eA[:, 8:16], in_=Dd[:].rearrange("p (g t) -> p g t", t=64)[:, :, 63]), [TB[2]], [eAB])
                    yield
                    dve(lambda e: e.tensor_copy(out=small[:, 8:16], in_=A4[:, :, 63]), [TB[1]], [eAB])
                    yield
                    dve(lambda e: e.tensor_scalar(out=Dd[:], in0=Dd[:], scalar1=40.0, scalar2=-40.0, op0=ALU.min, op1=ALU.max), [eAB], [TB[2]])
                    yield
                    act(lambda e: e.activation(out=E1[:], in_=Dd[:], func=AF.Exp), [TB[2]], [TB[4]])
                    yield
                    act(lambda e: e.activation(out=E2[:], in_=Dd[:], func=AF.Exp, scale=-1.0), [TB[2]], [TB[5]])
                    yield
                    act(lambda e: e.activation(out=eA[:], in_=eA[:], func=AF.Exp), [eAB], [eAB])
                    yield
                    act(lambda e: e.activation(out=small[:, 8:16], in_=small[:, 8:16], func=AF.Exp), [eAB], [eAB])
                    yield
                    dve(lambda e: e.tensor_tensor(out=Q[:], in0=Q[:], in1=E1[:], op=ALU.mult), [TB[4]], [TB[0]])
                    yield
                    dve(lambda e: e.tensor_tensor(out=KK[:], in0=KK[:], in1=E2[:], op=ALU.mult), [TB[5]], [TB[3]])
                    yield
                    act(lambda e: e.activation(out=qt[:], in_=Q[:], func=AF.Copy), [TB[0]], [hgB])
                    yield
                    act(lambda e: e.activation(out=kt[:], in_=KK[:], func=AF.Copy), [TB[3]], [hgB])
                    yield
                    dve(lambda e: e.tensor_tensor(out=qE[:].rearrange("p (g t) -> p g t", t=64), in0=Q[:].rearrange("p (g t) -> p g t", t=64), in1=eA[:, 0:8].unsqueeze(2).broadcast_to([128, 8, 64]), op=ALU.mult), [TB[0], eAB], [hgB])
                    yield
                    dve(lambda e: e.tensor_tensor(out=kdT[:].rearrange("p (g t) -> p g t", t=64), in0=KK[:].rearrange("p (g t) -> p g t", t=64), in1=eA[:, 8:16].unsqueeze(2).broadcast_to([128, 8, 64]), op=ALU.mult), [TB[3], eAB], [hgB])
                    yield
                    if debug and ti == 1:
                        def dump2(nm, c0, ap, bufs):
                            b = Buf(nm + str(c0)); dbgB.append(b)
                            P.dma("pool", lambda e: e.dma_start(out=dbg[nm][:, c0:c0 + 512], in_=ap), reads=bufs, writes=[b])
                        dump2("dbg_hT", 0, A_[:], [TB[1]])
                        dump2("dbg_hT", 512, Dd[:], [TB[2]])
                        dump2("dbg_gate", 0, Q[:], [TB[0]])
                        dump2("dbg_gate", 512, KK[:], [TB[3]])
                        dump2("dbg_xn", 0, E1[:], [TB[4]])
                        dump2("dbg_xn", 512, E2[:], [TB[5]])
                    bt, btB = nbH()
                    btb = bt[:].bitcast(BF16)
                    for hh in range(4):
                        pe((lambda hh: (lambda e: e.transpose(out=btb[:, hh * 128:(hh + 1) * 128], in_=kdT[:, hh * 128:(hh + 1) * 128], identity=identb[:])))(hh), [hgB, constB], [btB])
                        yield
                    act(lambda e: e.activation(out=kdtok[:], in_=btb[:, 0:512], func=AF.Copy), [btB], [kdtokB])
                    yield
                    bs, bsB = nbH()
                    for hh in range(4):
                        for c in range(2):
                            g = hh * 2 + c
                            pe((lambda hh, c, g: (lambda e: e.matmul(bs[c * 64:(c + 1) * 64, hh * 64:(hh + 1) * 64], lhsT=kt[:, g * 64:(g + 1) * 64], rhs=qt[:, g * 64:(g + 1) * 64], start=True, stop=True)))(hh, c, g), [hgB], [bsB])
                            yield
                    dve(lambda e: e.tensor_tensor(out=Pm[:], in0=bs[:, 0:256].rearrange("p (h t) -> p h t", h=4), in1=m64[:].unsqueeze(1).broadcast_to([128, 4, 64]), op=ALU.mult), [bsB, constB], [PmB])
                    yield
                    bo, boB = banks[6], bankB[6]
                    for c in range(2):
                        rs = slice(c * 64, (c + 1) * 64)
                        for hh in range(4):
                            g = hh * 2 + c
                            pe((lambda hh, c, g, rs: (lambda e: e.matmul(bo[:, g * 64:(g + 1) * 64], lhsT=vtok[rs, hh * 128:(hh + 1) * 128], rhs=Pm[rs, hh, :], start=True, stop=False)))(hh, c, g, rs), [vtokB, PmB], [boB])
                            yield
                            pe((lambda hh, c, g: (lambda e: e.matmul(bo[:, g * 64:(g + 1) * 64], lhsT=Sbf[:, hh, :], rhs=qE[:, g * 64:(g + 1) * 64], start=False, stop=True)))(hh, c, g), [SB_, hgB], [boB])
                            yield
                        bd, bdB = nbH()
                        for hh in range(4):
                            pe((lambda hh, rs: (lambda e: e.matmul(bd[:, hh * 128:(hh + 1) * 128], lhsT=kdtok[rs, hh * 128:(hh + 1) * 128], rhs=vtok[rs, hh * 128:(hh + 1) * 128], start=True, stop=True)))(hh, rs), [kdtokB, vtokB], [bdB])
                            yield
                        for hh in range(4):
                            g = hh * 2 + c
                            dve((lambda hh, g, bd: (lambda e: e.scalar_tensor_tensor(out=S32[:, hh, :], in0=S32[:, hh, :], scalar=small[:, 8 + g:9 + g], in1=bd[:, hh * 128:(hh + 1) * 128], op0=ALU.mult, op1=ALU.add)))(hh, g, bd), [bdB, eAB], [SB_])
                            yield
                        act(lambda e: e.activation(out=Sbf[:], in_=S32[:], func=AF.Copy), [], [SB_])
                        yield
                    OS = T[4]
                    act(lambda e: e.activation(out=OS[:], in_=bo[:], func=AF.Copy), [boB], [TB[4]])
                    yield
                    SQ = T[5]
                    act(lambda e: e.activation(out=SQ[:].bitcast(BF16)[:, 0:512], in_=bo[:], func=AF.Square), [boB], [TB[5]])
                    yield
                    bn, bnB = nbH()
                    pe(lambda e: e.matmul(bn[:], lhsT=onesb[:], rhs=SQ[:].bitcast(BF16)[:, 0:512], start=True, stop=True), [TB[5], constB], [bnB])
                    yield
                    R = T[2]
                    dve(lambda e: e.tensor_scalar(out=R[:], in0=bn[:], scalar1=1.0 / 128, scalar2=EPS, op0=ALU.mult, op1=ALU.add), [bnB], [TB[2]])
                    yield
                    act(lambda e: e.activation(out=R[:], in_=R[:], func=AF.Ln), [], [TB[2]])
                    yield
                    act(lambda e: e.activation(out=R[:], in_=R[:], func=AF.Exp, scale=-0.5), [], [TB[2]])
                    yield
                    dve(lambda e: e.tensor_tensor(out=OS[:], in0=OS[:], in1=R[:], op=ALU.mult), [TB[2]], [TB[4]])
                    yield
                    dve(lambda e: e.tensor_tensor(out=OS[:], in0=OS[:], in1=Gs[:], op=ALU.mult), [GsB], [TB[4]])
                    yield
                    for hh in range(4):
                        dve((lambda hh: (lambda e: e.tensor_scalar(out=yhgT[:, hh, :], in0=OS[:, hh * 128:(hh + 1) * 128], scalar1=pv[:, pvl + PV_HNG + hh: pvl + PV_HNG + hh + 1], scalar2=None, op0=ALU.mult)))(hh), [TB[4], pvB], [yhgB])
                        yield

                    yield
                def chainM():
                    CV = X[1 - slot][:, 0:512]
                    for hh in range(4):
                        cw = pvl + PV_CW
                        dve((lambda hh, cw: (lambda e: e.tensor_scalar(out=CV[:, hh * 128:(hh + 1) * 128], in0=U[:, hh, 0:128], scalar1=pv[:, cw + 0 * 4 + hh: cw + 0 * 4 + hh + 1], scalar2=pv[:, pvl + PV_CB + hh: pvl + PV_CB + hh + 1], op0=ALU.mult, op1=ALU.add)))(hh, cw), [UB, pvB], [XB[1 - slot]])
                        yield
                        for j in range(1, 4):
                            dve((lambda hh, cw, j: (lambda e: e.scalar_tensor_tensor(out=CV[:, hh * 128:(hh + 1) * 128], in0=U[:, hh, j:j + 128], scalar=pv[:, cw + j * 4 + hh: cw + j * 4 + hh + 1], in1=CV[:, hh * 128:(hh + 1) * 128], op0=ALU.mult, op1=ALU.add)))(hh, cw, j), [UB, pvB], [XB[1 - slot]])
                            yield
                    pool(lambda e: e.tensor_copy(out=U[:, :, 0:3], in_=U[:, :, 128:131]), [], [UB])
                    yield
                    act(lambda e: e.activation(out=ucT[:].rearrange("p h t -> p (h t)"), in_=CV[:], func=AF.Silu), [XB[1 - slot]], [ucB])
                    yield
                    act(lambda e: e.activation(out=gts[:, 4:8], in_=gts[:, 4:8], func=AF.Exp, scale=-1.0), [], [gtsB])
                    yield
                    act(lambda e: e.activation(out=gts[:, 8:12], in_=gts[:, 4:8], func=AF.Ln, bias=1.0), [], [gtsB])
                    yield
                    dve(lambda e: e.tensor_scalar(out=gts[:, 8:12], in0=gts[:, 8:12], scalar1=-1.0, scalar2=None, op0=ALU.mult), [], [gtsB])
                    yield
                    dve(lambda e: e.tensor_copy(out=gthl[:, 0:4], in_=gts[:, 8:12]), [], [gtsB])
                    yield
                    dve(lambda e: e.tensor_tensor(out=gts[:, 32:36], in0=gts[:, 8:12], in1=gthl[:, 0:4], op=ALU.subtract), [], [gtsB])
                    yield
                    dve(lambda e: e.tensor_copy(out=gthl[:, 4:8], in_=gts[:, 32:36]), [], [gtsB])
                    yield
                    bc, bcB = nbM()
                    pe(lambda e: e.matmul(bc[:, 0:4], lhsT=trib[:], rhs=gthl[:, 0:4], start=True, stop=False), [gtsB, constB], [bcB])
                    yield
                    pe(lambda e: e.matmul(bc[:, 0:4], lhsT=trib[:], rhs=gthl[:, 4:8], start=False, stop=True), [gtsB, constB], [bcB])
                    yield
                    pe(lambda e: e.matmul(bc[:, 4:8], lhsT=onesb[:], rhs=gthl[:, 0:4], start=True, stop=False), [gtsB, constB], [bcB])
                    yield
                    pe(lambda e: e.matmul(bc[:, 4:8], lhsT=onesb[:], rhs=gthl[:, 4:8], start=False, stop=True), [gtsB, constB], [bcB])
                    yield
                    dve(lambda e: e.tensor_copy(out=gts[:, 12:20], in_=bc[:, 0:8]), [bcB], [gtsB])
                    yield
                    dve(lambda e: e.tensor_tensor(out=gts[:, 20:24], in0=gts[:, 0:4], in1=gts[:, 12:16], op=ALU.subtract), [], [gtsB])
                    yield
                    dve(lambda e: e.tensor_tensor(out=gts[:, 28:32], in0=gts[:, 20:24], in1=gts[:, 16:20], op=ALU.add), [], [gtsB])
                    yield
                    dve(lambda e: e.tensor_copy(out=gts[:, 24:28], in_=gts[:, 12:16]), [], [gtsB])
                    yield
                    act(lambda e: e.activation(out=gts[:, 20:32], in_=gts[:, 20:32], func=AF.Exp), [], [gtsB])
                    yield
                    act(lambda e: e.activation(out=gts[:, 36:40], in_=gts[:, 16:20], func=AF.Exp), [], [gtsB])
                    yield
                    bqk, bqkB = nbM()
                    for hh in range(4):
                        pe((lambda hh: (lambda e: e.matmul(bqk[0:64, hh * 128:(hh + 1) * 128], lhsT=wqk[:, 0, hh, :], rhs=ucT[:, hh, :], start=True, stop=True)))(hh), [wqkB, ucB], [bqkB])
                        yield
                    bkk, bkkB = nbM()
                    for hh in range(4):
                        pe((lambda hh: (lambda e: e.matmul(bkk[0:64, hh * 128:(hh + 1) * 128], lhsT=wqk[:, 1, hh, :], rhs=ucT[:, hh, :], start=True, stop=True)))(hh), [wqkB, ucB], [bkkB])
                        yield
                    bkt, bktB = nbM()
                    for hh in range(4):
                        pe((lambda hh: (lambda e: e.matmul(bkt[:, hh * 64:(hh + 1) * 64], lhsT=ucT[:, hh, :], rhs=wqk[:, 1, hh, :], start=True, stop=True)))(hh), [wqkB, ucB], [bktB])
                        yield
                    act(lambda e: e.activation(out=qTm[:].rearrange("p h t -> p (h t)"), in_=bqk[0:64, :], func=AF.Copy, scale=0.125), [bqkB], [qkB])
                    yield
                    act(lambda e: e.activation(out=kTm[:].rearrange("p h t -> p (h t)"), in_=bkk[0:64, :], func=AF.Copy), [bkkB], [qkB])
                    yield
                    dve(lambda e: e.tensor_tensor(out=ktok[:], in0=bkt[:, 0:256].rearrange("p (h e) -> p h e", h=4), in1=gts[:, 28:32].unsqueeze(2).broadcast_to([128, 4, 64]), op=ALU.mult), [bktB, gtsB], [ktokB])
                    yield
                    bsm, bsmB = nbM()
                    for hh in range(4):
                        pe((lambda hh: (lambda e: e.matmul(bsm[:, hh * 128:(hh + 1) * 128], lhsT=kTm[:, hh, :], rhs=qTm[:, hh, :], start=True, stop=True)))(hh), [qkB], [bsmB])
                        yield
                    for hh in range(4):
                        dve((lambda hh: (lambda e: e.scalar_tensor_tensor(out=Pml[:, hh, :], in0=bsm[:, hh * 128:(hh + 1) * 128], scalar=gts[:, 20 + hh:21 + hh], in1=caus[:], op0=ALU.mult, op1=ALU.mult)))(hh), [bsmB, gtsB, constB], [PmlB])
                        yield
                    HM = X[1 - slot][:, 512:1024]
                    bnum = []
                    for pair in range(2):
                        bn2, bn2B = (banks[7], bankB[7]) if pair == 0 else (banks[5], bankB[5])
                        bnum.append((bn2, bn2B))
                        for hq in range(2):
                            hh = pair * 2 + hq
                            pe((lambda hh, hq, bn2: (lambda e: e.matmul(bn2[:, hq * 130:hq * 130 + 130], lhsT=Pml[:, hh, :], rhs=vaug[:, hh, :], start=True, stop=False)))(hh, hq, bn2), [PmlB, vaugB], [bn2B])
                            yield
                            pe((lambda hh, hq, bn2: (lambda e: e.matmul(bn2[:, hq * 130:hq * 130 + 130], lhsT=qTm[:, hh, :], rhs=Cbf[:, hh, :], start=False, stop=True)))(hh, hq, bn2), [qkB, CB_], [bn2B])
                            yield
                    bcs, bcsB = nbM()
                    for hh in range(4):
                        pe((lambda hh: (lambda e: e.matmul(bcs[0:64, hh * 128:hh * 128 + 128], lhsT=ktok[:, hh, :], rhs=vaug[:, hh, 0:128], start=True, stop=True)))(hh), [ktokB, vaugB], [bcsB])
                        yield
                    bcn, bcnB = nbM()
                    for hh in range(4):
                        pe((lambda hh: (lambda e: e.matmul(bcn[0:64, hh * 2:hh * 2 + 2], lhsT=ktok[:, hh, :], rhs=vaug[:, hh, 128:130], start=True, stop=True)))(hh), [ktokB, vaugB], [bcnB])
                        yield
                    for hh in range(4):
                        dve((lambda hh: (lambda e: e.scalar_tensor_tensor(out=C32[:, hh, 0:128], in0=C32[:, hh, 0:128], scalar=gts[0:64, 36 + hh:37 + hh], in1=bcs[0:64, hh * 128:(hh + 1) * 128], op0=ALU.mult, op1=ALU.add)))(hh), [bcsB, gtsB], [CB_])
                        yield
                        dve((lambda hh: (lambda e: e.scalar_tensor_tensor(out=C32[:, hh, 128:130], in0=C32[:, hh, 128:130], scalar=gts[0:64, 36 + hh:37 + hh], in1=bcn[0:64, hh * 2:hh * 2 + 2], op0=ALU.mult, op1=ALU.add)))(hh), [bcnB, gtsB], [CB_])
                        yield
                    for pair in range(2):
                        bn2, bn2B = bnum[pair]
                        for hq in range(2):
                            hh = pair * 2 + hq
                            c0 = hq * 130
                            dve((lambda hh, c0, bn2: (lambda e: e.tensor_scalar(out=gts[:, 52 + hh:53 + hh], in0=bn2[:, c0 + 128:c0 + 129], scalar1=gts[:, 24 + hh:25 + hh], scalar2=None, op0=ALU.mult)))(hh, c0, bn2), [bn2B], [gtsB])
                            yield
                            dve((lambda hh: (lambda e: e.scalar_tensor_tensor(out=gts[:, 40 + hh:41 + hh], in0=gts[:, 52 + hh:53 + hh], scalar=-1.0, in1=gts[:, 52 + hh:53 + hh], op0=ALU.mult, op1=ALU.max)))(hh), [], [gtsB])
                            yield
                            dve((lambda hh: (lambda e: e.tensor_scalar(out=gts[:, 40 + hh:41 + hh], in0=gts[:, 40 + hh:41 + hh], scalar1=1.0, scalar2=None, op0=ALU.max)))(hh), [], [gtsB])
                            yield
                            dve((lambda hh: (lambda e: e.reciprocal(out=gts[:, 44 + hh:45 + hh], in_=gts[:, 40 + hh:41 + hh])))(hh), [], [gtsB])
                            yield
                            dve((lambda hh: (lambda e: e.tensor_tensor(out=gts[:, 44 + hh:45 + hh], in0=gts[:, 44 + hh:45 + hh], in1=gts[:, 24 + hh:25 + hh], op=ALU.mult)))(hh), [], [gtsB])
                            yield
                            dve((lambda hh, c0, bn2: (lambda e: e.tensor_scalar(out=HM[:, hh * 128:(hh + 1) * 128], in0=bn2[:, c0:c0 + 128], scalar1=gts[:, 44 + hh:45 + hh], scalar2=None, op0=ALU.mult)))(hh, c0, bn2), [bn2B, gtsB], [XB[1 - slot]])
                            yield
                            act((lambda hh: (lambda e: e.activation(out=CV[:, hh * 128:(hh + 1) * 128], in_=HM[:, hh * 128:(hh + 1) * 128], func=AF.Square, accum_out=gts[:, 48 + hh:49 + hh])))(hh), [XB[1 - slot]], [XB[1 - slot], gtsB])
                            yield
                    act(lambda e: e.activation(out=Cbf[:], in_=C32[:], func=AF.Copy), [], [CB_])
                    yield
                    dve(lambda e: e.tensor_scalar(out=gts[:, 48:52], in0=gts[:, 48:52], scalar1=1.0 / 128, scalar2=EPS, op0=ALU.mult, op1=ALU.add), [], [gtsB])
                    yield
                    act(lambda e: e.activation(out=gts[:, 48:52], in_=gts[:, 48:52], func=AF.Ln), [], [gtsB])
                    yield
                    act(lambda e: e.activation(out=gts[:, 48:52], in_=gts[:, 48:52], func=AF.Exp, scale=-0.5), [], [gtsB])
                    yield
                    for hh in range(4):
                        dve((lambda hh: (lambda e: e.scalar_tensor_tensor(out=yml[:, hh * 128:(hh + 1) * 128], in0=HM[:, hh * 128:(hh + 1) * 128], scalar=gts[:, 48 + hh:49 + hh], in1=mosg[:, hh * 128:(hh + 1) * 128], op0=ALU.mult, op1=ALU.mult)))(hh), [XB[1 - slot], gtsB, mosgB], [ymlTokB])
                        yield
                    bty, btyB = nbM()
                    btyb = bty[:].bitcast(BF16)
                    for hh in range(4):
                        pe((lambda hh: (lambda e: e.transpose(out=btyb[:, hh * 128:(hh + 1) * 128], in_=yml[:, hh * 128:(hh + 1) * 128], identity=identb[:])))(hh), [ymlTokB, constB], [btyB])
                        yield
                    for hh in range(4):
                        dve((lambda hh: (lambda e: e.tensor_scalar(out=ymlT[:, hh, :], in0=btyb[:, hh * 128:(hh + 1) * 128], scalar1=pv[:, pvl + PV_MNG + hh: pvl + PV_MNG + hh + 1], scalar2=None, op0=ALU.mult)))(hh), [btyB, pvB], [ymlB])
                        yield

                    yield
                alive = [chainH(), chainM()]
                while alive:
                    for g_ in list(alive):
                        try:
                            next(g_)
                        except StopIteration:
                            alive.remove(g_)

                for half in range(2):
                    bph, bphB = nb()
                    bpm, bpmB = nb()
                    for jj in range(4):
                        j = half * 4 + jj
                        for k in range(4):
                            pe((lambda j, jj, k, bph: (lambda e: e.matmul(bph[:, jj * 128:(jj + 1) * 128], lhsT=Wbh[:, k, j * 128:(j + 1) * 128], rhs=yhgT[:, k, :], start=(k == 0), stop=(k == 3))))(j, jj, k, bph), [WB[1], yhgB], [bphB])
                        for k in range(4):
                            pe((lambda j, jj, k, bpm: (lambda e: e.matmul(bpm[:, jj * 128:(jj + 1) * 128], lhsT=Wbm[:, k, j * 128:(j + 1) * 128], rhs=ymlT[:, k, :], start=(k == 0), stop=(k == 3))))(j, jj, k, bpm), [WB[1], ymlB], [bpmB])
                    dve((lambda half, bph: (lambda e: e.tensor_tensor(out=T[3][:], in0=bph[:], in1=SGA[:, half * 4:(half + 1) * 4, :].rearrange("p k t -> p (k t)"), op=ALU.mult)))(half, bph), [bphB, sgB, sgB2, sgB3], [TB[3]])
                    dve((lambda half, bpm: (lambda e: e.tensor_tensor(out=T[5][:], in0=bpm[:], in1=SGB[:, half * 4:(half + 1) * 4, :].rearrange("p k t -> p (k t)"), op=ALU.mult)))(half, bpm), [bpmB, sgB, sgB2, sgB3], [TB[5]])
                    pool((lambda half: (lambda e: e.tensor_tensor(out=mrg[:, half * 4:(half + 1) * 4, :].rearrange("p k t -> p (k t)"), in0=T[3][:], in1=T[5][:], op=ALU.add)))(half), [TB[3], TB[5]], [mrgB])
                ob0, ob0B = banks[6], bankB[6]
                ob1, ob1B = banks[7], bankB[7]
                for half, ob, obb in ((0, ob0, ob0B), (1, ob1, ob1B)):
                    for k in range(8):
                        pe((lambda half, k, ob: (lambda e: e.matmul(ob[:], lhsT=mrg[:, k, :], rhs=Wo[:, k, half * 512:(half + 1) * 512], start=(k == 0), stop=(k == 7))))(half, k, ob), [mrgB, WB[1]], [obb])
                residual(slot, [ob0, ob1], [ob0B, ob1B])
                store_x(ti, slot, xdB[ti])

        if "A" in phases:
            phase_mixer(0, x_d)
        srcB = out_d if "A" in phases else x_d
        if "B" in phases:
            phase_dense(0, srcB)
        srcC = out_d if ("A" in phases or "B" in phases) else x_d
        if "C" in phases:
            phase_mixer(1, srcC)
        srcD = out_d if any(p in phases for p in "ABC") else x_d
        if "D" in phases:
            if SPARSE_MOE:
                phase_moe_sparse(1, srcD)
            else:
                phase_moe(1, srcD)
        P.final_wait("sp", xdB)
        P.final_wait("pool", dbgB)
        P.emit()
    return nc, P


def _consts():
    cst = np.zeros((128, 1024), np.float32)
    cst[:, 0:128] = np.eye(128, dtype=np.float32)
    s = np.arange(128)[:, None]
    t = np.arange(128)[None, :]
    cst[:, 128:256] = (s <= t)
    cst[:, 256:384] = ((s // 64) == (t // 64))
    cst[:, 384:512] = (s <= t)
    cst[:, 512:576] = ((s % 64) <= np.arange(64)[None, :])
    cst[:, 576:704] = (s < t)
    rst = np.ones((128, 512), np.float32)
    rst[:, 0::64] = 0.0
    return cst, rst


def _fm(v, k):
    return np.ascontiguousarray(np.asarray(v, np.float32).reshape(k, 128).T)


def _pack_pv(inp):
    pv = np.zeros((128, DEPTH * PVL), np.float32)
    for l in range(DEPTH):
        o = l * PVL
        pv[:, o + PV_NMG:o + PV_NMG + 8] = _fm(inp["norm_mix_g"][l], 8)
        pv[:, o + PV_AMB:o + PV_AMB + 24] = _fm(inp["ada_mix_b"][l], 24)
        pv[:, o + PV_NFG:o + PV_NFG + 8] = _fm(inp["norm_ffn_g"][l], 8)
        pv[:, o + PV_AFB:o + PV_AFB + 24] = _fm(inp["ada_ffn_b"][l], 24)
        pv[:, o + PV_LB:o + PV_LB + 4] = _fm(inp["hg_lb_logits"][l], 4)
        pv[:, o + PV_HNG:o + PV_HNG + 4] = _fm(inp["hg_norm_g"][l], 4)
        cw = np.asarray(inp["ml_conv_w"][l], np.float32)
        for j in range(4):
            pv[:, o + PV_CW + j * 4:o + PV_CW + j * 4 + 4] = _fm(cw[j], 4)
        pv[:, o + PV_CB:o + PV_CB + 4] = _fm(inp["ml_conv_b"][l], 4)
        pv[:, o + PV_MNG:o + PV_MNG + 4] = _fm(inp["ml_norm_g"][l], 4)
        pv[:, o + PV_FB:o + PV_FB + 4] = np.broadcast_to(np.asarray(inp["ml_fbias"][l], np.float32)[None, :], (128, 4))
    pv[:, PVL + PV_RB:PVL + PV_RB + NE] = np.broadcast_to(np.asarray(inp["moe_router_b"][0], np.float32)[None, :], (128, NE))
    return pv


_CACHE = {}
LAST_CNT = None


def run(inputs, n_cores, NSEQ, S, phases="ABCD", debug=False):
    key = (NSEQ, S, phases, debug)
    if key not in _CACHE:
        _CACHE[key] = build(NSEQ, S, phases, debug)[0]
    nc = _CACHE[key]
    f32 = lambda a: np.ascontiguousarray(np.asarray(a, np.float32))
    x = f32(inputs["x"]); c = f32(inputs["c"])
    cst, rst = _consts()
    pv = _pack_pv(inputs)
    gbias = np.stack([np.broadcast_to(f32(inputs[nm])[l][2 * D:3 * D][None, :], (128, D))
                      for l in range(DEPTH) for nm in ("ada_mix_b", "ada_ffn_b")]).astype(np.float32)
    fng = np.ascontiguousarray(np.broadcast_to(f32(inputs["final_norm_g"])[None, :], (128, D)))
    shared = {"pv": pv, "cst": cst, "rst": rst, "gbias": np.ascontiguousarray(gbias), "fng": fng}
    for nm in ("ada_mix_w", "ada_ffn_w", "w_in", "ml_wq", "ml_wk", "w_br_hg", "w_br_ml", "w_out", "ffn_w1", "ffn_w3", "ffn_w2",
               "moe_router_w", "moe_w1", "moe_w3", "moe_w2"):
        shared[nm] = f32(inputs[nm])
    in_maps = []
    for i in range(n_cores):
        xs = x[i * NSEQ:(i + 1) * NSEQ].reshape(NSEQ * S, D)
        cs = c[i * NSEQ:(i + 1) * NSEQ]
        cTl = np.ascontiguousarray(cs.reshape(NSEQ, 8, 128).transpose(2, 1, 0))
        m = dict(shared)
        m["x"] = np.ascontiguousarray(xs)
        m["cT"] = cTl
        in_maps.append(m)
    res = run_bass_kernel_spmd(nc, in_maps, core_ids=list(range(n_cores)))
    outs = [r["out"].reshape(NSEQ, S, D) for r in res.results]
    global LAST_CNT
    LAST_CNT = [r["cnt"][0] for r in res.results]
    if debug:
        return np.concatenate(outs, axis=0), res.results[0]
    return np.concatenate(outs, axis=0)


def kernel(**inputs):
    B, S, _ = inputs["x"].shape
    return run(inputs, N_CORES, B // N_CORES, S).astype(np.float32)
```

```python
import contextlib
import numpy as np
import concourse.bass as bass
import concourse.mybir as mybir
from concourse.bass_utils import run_bass_kernel_spmd

F32 = mybir.dt.float32
BF16 = mybir.dt.bfloat16
AF = mybir.ActivationFunctionType
ALU = mybir.AluOpType
AX = mybir.AxisListType

D = 1024
DEPTH = 2
INC = 5640
C_Q, C_F, C_I, C_G, C_MU, C_MV, C_MO, C_MI, C_MF, C_GA, C_GB = 0, 512, 1024, 1536, 2048, 2560, 3072, 3584, 3588, 3592, 4616
DFF = 2816
NE = 8
DFE = 1408
EPS = 1e-6
N_CORES = 8
PE_DRAIN = False
ATTACH_WAITS = True
SAME_ENG_DIST = 1 << 30
SPARSE_MOE = True

PV_NMG, PV_AMB, PV_NFG, PV_AFB, PV_LB, PV_HNG, PV_CW, PV_CB, PV_MNG, PV_FB, PV_RB = 0, 8, 32, 40, 64, 68, 72, 88, 92, 96, 100
PVL = 108


class Buf:
    __slots__ = ("name", "w", "r")

    def __init__(self, name):
        self.name = name
        self.w = []
        self.r = []


class _Rec:
    def __init__(self):
        self.call = None

    def __getattr__(self, name):
        def f(*a, **k):
            self.call = (name, a, k)
            return self
        return f


def _record(fn):
    r = _Rec()
    fn(r)
    return r.call


class Prog:
    ENGS = ("pe", "act", "dve", "pool", "sp")

    def __init__(self, nc, n_dma_sems=16):
        self.nc = nc
        self.q = {e: [] for e in self.ENGS}
        self.cnt = {e: 0 for e in self.ENGS}
        self.waited = {e: {} for e in self.ENGS}
        self.n_dma_sems = n_dma_sems
        self.dma_rr = {"sp": 0, "pool": 0, "act": 0}
        self.dma_cnt = {}
        self.n_ins = 0

    def _need(self, eng, tok, waits):
        if tok is None:
            return
        key, val = tok
        if key == ("e", eng) and eng == "pe":
            return
        if key == ("e", eng) and (self.cnt[eng] + 1 - val) > SAME_ENG_DIST:
            return
        if self.waited[eng].get(key, 0) >= val:
            return
        self.waited[eng][key] = val
        waits.append((key, val))

    def _deps(self, eng, reads, writes):
        waits = []
        for b in reads:
            for t in b.w:
                self._need(eng, t, waits)
        for b in writes:
            for t in b.w:
                self._need(eng, t, waits)
            for t in b.r:
                self._need(eng, t, waits)
        return waits

    def _commit(self, tok, reads, writes):
        for b in reads:
            b.r.append(tok)
        for b in writes:
            b.w = [tok]
            b.r = []

    def op(self, eng, fn, reads=(), writes=()):
        waits = self._deps(eng, reads, writes)
        self.cnt[eng] += 1
        tok = (("e", eng), self.cnt[eng])
        self.q[eng].append((waits, _record(fn), ("e", eng), 1))
        self._commit(tok, reads, writes)
        self.n_ins += 1
        return tok

    def dma(self, qeng, fn, reads=(), writes=(), group=None):
        if group is not None:
            waits = []
            for t in group["deps"]:
                self._need(qeng, t, waits)
        else:
            waits = self._deps(qeng, reads, writes)
        k = self.dma_rr[qeng]
        self.dma_rr[qeng] = (k + 1) % self.n_dma_sems
        key = ("d", qeng, k)
        prev = self.dma_cnt.get(key, 0)
        if prev:
            self._need(qeng, (key, prev), waits)
        val = prev + 16
        self.dma_cnt[key] = val
        tok = (key, val)
        self.q[qeng].append((waits, _record(fn), key, 16))
        if group is not None:
            group["toks"].append(tok)
        else:
            self._commit(tok, reads, writes)
        self.n_ins += 1
        return tok

    def group_begin(self, bufs):
        deps = []
        for b in bufs:
            deps += list(b.w) + list(b.r)
        return {"deps": deps, "toks": [], "bufs": bufs}

    def group_end(self, g):
        for b in g["bufs"]:
            b.w = list(g["toks"])
            b.r = []

    def final_wait(self, eng, bufs):
        waits = []
        for b in bufs:
            for t in b.w:
                self._need(eng, t, waits)
        self.q[eng].append((waits, None, None, 0))

    def emit(self):
        nc = self.nc
        with contextlib.ExitStack() as st:
            sems = {}
            for e in self.ENGS:
                sems[("e", e)] = st.enter_context(nc.semaphore("s_" + e))
            for key in self.dma_cnt:
                sems[key] = st.enter_context(nc.semaphore("d_%s_%d" % (key[1], key[2])))
            block = st.enter_context(nc.Block())

            ATTACH_OK = ("tensor_scalar", "tensor_tensor", "scalar_tensor_tensor", "tensor_copy", "reciprocal",
                         "memset", "tensor_tensor_scan", "activation")

            def run(eh, lst, drain=False, attach=False):
                for waits, fn, skey, inc in lst:
                    att = None
                    if attach and waits and fn is not None and fn[0] in ATTACH_OK and fn[2].get("accum_out") is None:
                        att = waits[-1]
                        waits = waits[:-1]
                    for key, val in waits:
                        eh.wait_ge(sems[key], val)
                    if drain and waits:
                        eh.drain()
                    if fn is not None:
                        name, a, k = fn
                        ins = getattr(eh, name)(*a, **k)
                        if att is not None:
                            ins._wait_ge(sems[att[0]], att[1])
                        ins.then_inc(sems[skey], inc)

            block.tensor(lambda t: run(t, self.q["pe"], PE_DRAIN))
            block.scalar(lambda t: run(t, self.q["act"], attach=ATTACH_WAITS))
            block.vector(lambda t: run(t, self.q["dve"], attach=ATTACH_WAITS))
            block.gpsimd(lambda t: run(t, self.q["pool"], attach=ATTACH_WAITS))
            block.sync(lambda t: run(t, self.q["sp"]))


def build(NSEQ, S, phases="ABCD", debug=False):
    NT = S // 128
    NTILES = NSEQ * NT
    nc = bass.Bass("TRN2", target_bir_lowering=False)
    dt_in = lambda name, shape: nc.dram_tensor(name, list(shape), F32, kind="ExternalInput").ap()
    x_d = dt_in("x", [NSEQ * S, D])
    cT_d = dt_in("cT", [128, 8, NSEQ])
    pv_d = dt_in("pv", [128, DEPTH * PVL])
    cst_d = dt_in("cst", [128, 1024])
    rst_d = dt_in("rst", [128, 512])
    gb_d = dt_in("gbias", [2 * DEPTH, 128, D])
    fng_d = dt_in("fng", [128, D])
    ada_mix_w = dt_in("ada_mix_w", [DEPTH, D, 3 * D])
    ada_ffn_w = dt_in("ada_ffn_w", [DEPTH, D, 3 * D])
    w_in = dt_in("w_in", [DEPTH, D, INC])
    ml_wq = dt_in("ml_wq", [DEPTH, 4, 128, 64])
    ml_wk = dt_in("ml_wk", [DEPTH, 4, 128, 64])
    w_br_hg = dt_in("w_br_hg", [DEPTH, 512, D])
    w_br_ml = dt_in("w_br_ml", [DEPTH, 512, D])
    w_out = dt_in("w_out", [DEPTH, D, D])
    ffn_w1 = dt_in("ffn_w1", [1, D, DFF])
    ffn_w3 = dt_in("ffn_w3", [1, D, DFF])
    ffn_w2 = dt_in("ffn_w2", [1, DFF, D])
    moe_rw = dt_in("moe_router_w", [1, D, NE])
    moe_w1 = dt_in("moe_w1", [1, NE, D, DFE])
    moe_w3 = dt_in("moe_w3", [1, NE, D, DFE])
    moe_w2 = dt_in("moe_w2", [1, NE, DFE, D])
    out_d = nc.dram_tensor("out", [NSEQ * S, D], F32, kind="ExternalOutput").ap()
    cnt_d = nc.dram_tensor("cnt", [128, NE], F32, kind="ExternalOutput").ap()
    hsc_d = nc.dram_tensor("hsc", [NTILES, 128, 8 * 128], BF16, kind="Internal").ap()
    csc_d = nc.dram_tensor("csc", [NTILES, 128, NE], F32, kind="Internal").ap()
    CAP = max(128, ((NSEQ * S * 2 // NE) * 15 // 8 + 127) // 128 * 128)
    hg_d = nc.dram_tensor("hg", [NE * CAP, D], BF16, kind="Internal").ap()
    y_d = nc.dram_tensor("yex", [NE * CAP, D], F32, kind="Internal").ap()

    dbg = {}
    if debug:
        for nm, shp in (("dbg_hT", [128, 1024]), ("dbg_mod", [128, 16]), ("dbg_gate", [128, 1024]), ("dbg_act", [128, 1408]), ("dbg_xn", [128, 1024]), ("dbg_ob", [128, 1024])):
            dbg[nm] = nc.dram_tensor(nm, shp, F32, kind="ExternalOutput").ap()
    dbgB = []
    P = Prog(nc)
    st = contextlib.ExitStack()
    with st:
        def sb(name, shape, dt=F32):
            return st.enter_context(nc.sbuf_tensor("sb_" + name, list(shape), dt))

        ARENA_N = 67584
        arena = sb("arena", [128, ARENA_N], BF16)
        slotB = [Buf("slot0"), Buf("slot1")]
        X = [sb("X0", [128, D]), sb("X1", [128, D])]
        XB = [Buf("X0"), Buf("X1")]
        xn = sb("xn", [128, D], BF16); xnB = Buf("xn")
        hT = sb("hT", [128, 8, 128], BF16); hTB = Buf("hT")
        cst = sb("cst", [128, 1024]); cstB = Buf("cst")
        identb = sb("identb", [128, 128], BF16)
        trib = sb("trib", [128, 128], BF16)
        bonesb = sb("bonesb", [128, 128], BF16)
        onesb = sb("onesb", [128, 128], BF16)
        caus = sb("caus", [128, 128], BF16)
        m64 = sb("m64", [128, 64])
        rst = sb("rst", [128, 512], BF16);
        pv = sb("pv", [128, DEPTH * PVL]); pvB = Buf("pv")
        lbc = sb("lbc", [128, 2, 4]); lbB = Buf("lbc")
        cT = sb("cT", [128, 8, NSEQ])
        cact = sb("cact", [128, 8, NSEQ], BF16)
        cbc = sb("cbc", [128, 8, 128], BF16); cbcB = Buf("cbc")
        MS = sb("MS", [128, NSEQ, 8]); SH = sb("SH", [128, NSEQ, 8]); modB = Buf("mod")
        GATEb = sb("GATEb", [128, D]); gateB = Buf("gate")
        small = sb("small", [128, 64]);
        T = [sb("T%d" % i, [128, 512]) for i in range(6)]
        TB = [Buf("T%d" % i) for i in range(6)]
        constB = Buf("consts")

        psum_all = st.enter_context(nc.psum_tensor("psum_all", [128, 4096], F32))
        banks = [psum_all[:, i * 512:(i + 1) * 512] for i in range(8)]
        bankB = [Buf("bank%d" % i) for i in range(8)]
        bank_rr = [0]
        bank_n = [6]

        def nb():
            i = bank_rr[0] % bank_n[0]
            bank_rr[0] = (i + 1) % bank_n[0]
            return banks[i], bankB[i]

        def dve(fn, r=(), w=()):
            return P.op("dve", fn, r, w)

        def act(fn, r=(), w=()):
            return P.op("act", fn, r, w)

        def pe(fn, r=(), w=()):
            return P.op("pe", fn, r, w)

        def pool(fn, r=(), w=()):
            return P.op("pool", fn, r, w)

        P.dma("sp", lambda e: e.dma_start(out=cst[:], in_=cst_d), writes=[cstB])
        P.dma("pool", lambda e: e.dma_start(out=rst[:], in_=rst_d), writes=[constB])
        P.dma("sp", lambda e: e.dma_start(out=pv[:], in_=pv_d), writes=[pvB])
        P.dma("sp", lambda e: e.dma_start(out=cT[:], in_=cT_d), writes=[modB])
        dve(lambda e: e.tensor_copy(out=identb[:], in_=cst[:, 0:128]), [cstB], [constB])
        dve(lambda e: e.tensor_copy(out=trib[:], in_=cst[:, 128:256]), [cstB], [constB])
        dve(lambda e: e.tensor_copy(out=bonesb[:], in_=cst[:, 256:384]), [cstB], [constB])
        dve(lambda e: e.tensor_copy(out=caus[:], in_=cst[:, 384:512]), [cstB], [constB])
        dve(lambda e: e.tensor_copy(out=m64[:], in_=cst[:, 512:576]), [cstB], [constB])
        dve(lambda e: e.memset(onesb[:], 1.0), [], [constB])
        strilb = sb("strilb", [128, 128], BF16)
        dve(lambda e: e.tensor_copy(out=strilb[:], in_=cst[:, 576:704]), [cstB], [constB])
        act(lambda e: e.activation(out=cact[:], in_=cT[:], func=AF.Silu), [modB], [modB])

        def wload(dst_ap, src_ap, grp, ncols):
            K = dst_ap.shape[1]
            for k in range(K):
                for c0 in range(0, ncols, 1024):
                    c1 = min(ncols, c0 + 1024)
                    P.dma("pool", (lambda k, c0, c1: (lambda e: e.dma_start(out=dst_ap[:, k, c0:c1], in_=src_ap[:, k, c0:c1])))(k, c0, c1),
                          group=grp)

        def kview(w2d):
            return w2d.rearrange("(k p) n -> p k n", p=128)

        def ada_setup(ada_w_l, l, which, norm_g_col, ada_b_col, gb_row):
            aw = arena[:, 33792:33792 + 8 * 3072].rearrange("p (k n) -> p k n", k=8)
            g = P.group_begin([slotB[1]])
            wload(aw, kview(ada_w_l), g, 3072)
            P.group_end(g)
            bk, bb = nb()
            for j in range(16):
                for k in range(8):
                    pe((lambda j, k: (lambda e: e.matmul(bk[:, j * NSEQ:(j + 1) * NSEQ], lhsT=aw[:, k, j * 128:(j + 1) * 128], rhs=cact[:, k, :], start=(k == 0), stop=(k == 7))))(j, k),
                       [slotB[1], modB], [bb])
            bv = bk[:, 0:16 * NSEQ].rearrange("p (j s) -> p j s", s=NSEQ)
            for s in range(NSEQ):
                dve((lambda s: (lambda e: e.tensor_tensor(out=SH[:, s, :], in0=bv[:, 0:8, s], in1=pv[:, l * PVL + ada_b_col: l * PVL + ada_b_col + 8], op=ALU.add)))(s), [bb, pvB], [modB])
                dve((lambda s: (lambda e: e.tensor_tensor(out=MS[:, s, :], in0=bv[:, 8:16, s], in1=pv[:, l * PVL + ada_b_col + 8: l * PVL + ada_b_col + 16], op=ALU.add)))(s), [bb, pvB], [modB])
                dve((lambda s: (lambda e: e.scalar_tensor_tensor(out=MS[:, s, :], in0=MS[:, s, :], scalar=1.0, in1=pv[:, l * PVL + norm_g_col: l * PVL + norm_g_col + 8], op0=ALU.add, op1=ALU.mult)))(s), [pvB], [modB])
            return aw

        def gate_setup(aw, s, gb_row):
            dve(lambda e: e.tensor_copy(out=cbc[:], in_=cact[:, :, s:s + 1].broadcast_to([128, 8, 128])), [modB], [cbcB])
            P.dma("sp", lambda e: e.dma_start(out=GATEb[:], in_=gb_d[gb_row]), writes=[gateB])
            for half in range(2):
                bk, bb = nb()
                for k in range(8):
                    pe((lambda k, half: (lambda e: e.matmul(bk[:], lhsT=cbc[:, k, :], rhs=aw[:, k, 2048 + half * 512: 2048 + (half + 1) * 512], start=(k == 0), stop=(k == 7))))(k, half),
                       [cbcB, slotB[1]], [bb])
                dve((lambda half, bk: (lambda e: e.tensor_tensor(out=GATEb[:, half * 512:(half + 1) * 512], in0=GATEb[:, half * 512:(half + 1) * 512], in1=bk[:], op=ALU.add)))(half, bk), [bb], [gateB])

        def load_x(ti, slot, src):
            P.dma("sp", lambda e: e.dma_start(out=X[slot][:], in_=src[ti * 128:(ti + 1) * 128, :]), writes=[XB[slot]])

        def norm_mod(slot, s):
            Xs, Xb = X[slot], XB[slot]
            act(lambda e: e.activation(out=xn[:], in_=Xs[:], func=AF.Square, accum_out=small[:, 0:1]), [Xb], [xnB])
            dve(lambda e: e.tensor_scalar(out=small[:, 1:2], in0=small[:, 0:1], scalar1=1.0 / D, scalar2=EPS, op0=ALU.mult, op1=ALU.add), [xnB], [xnB])
            act(lambda e: e.activation(out=small[:, 1:2], in_=small[:, 1:2], func=AF.Ln), [xnB], [xnB])
            act(lambda e: e.activation(out=small[:, 2:3], in_=small[:, 1:2], func=AF.Exp, scale=-0.5), [xnB], [xnB])
            act(lambda e: e.activation(out=xn[:], in_=Xs[:], func=AF.Copy, scale=small[:, 2:3]), [Xb, xnB], [xnB])
            bk, bb = nb()
            bkb = bk[:].bitcast(BF16)
            for k in range(8):
                pe((lambda k: (lambda e: e.transpose(out=bkb[:, k * 128:(k + 1) * 128], in_=xn[:, k * 128:(k + 1) * 128], identity=identb[:])))(k), [xnB, constB], [bb])
            bv = bkb[:, 0:1024].rearrange("p (k t) -> p k t", k=8)
            dve(lambda e: e.tensor_tensor(out=T[0][:].rearrange("p (k t) -> p k t", k=8)[:, :, 0:64], in0=bv[:, :, 0:64], in1=MS[:, s, :].unsqueeze(2).broadcast_to([128, 8, 64]), op=ALU.mult), [bb, modB], [TB[0]])
            dve(lambda e: e.tensor_tensor(out=T[1][:].rearrange("p (k t) -> p k t", k=8)[:, :, 0:64], in0=bv[:, :, 64:128], in1=MS[:, s, :].unsqueeze(2).broadcast_to([128, 8, 64]), op=ALU.mult), [bb, modB], [TB[1]])
            dve(lambda e: e.tensor_tensor(out=hT[:, :, 0:64], in0=T[0][:].rearrange("p (k t) -> p k t", k=8), in1=SH[:, s, :].unsqueeze(2).broadcast_to([128, 8, 64]), op=ALU.add), [TB[0], modB], [hTB])
            dve(lambda e: e.tensor_tensor(out=hT[:, :, 64:128], in0=T[1][:].rearrange("p (k t) -> p k t", k=8), in1=SH[:, s, :].unsqueeze(2).broadcast_to([128, 8, 64]), op=ALU.add), [TB[1], modB], [hTB])

        actT = sb("actT", [128, 11, 128], BF16); actTB = [Buf("actT_g%d" % i) for i in range(3)]
        actT_l = [actT, sb("actT1", [128, 11, 128], BF16)]; actTB_l = [actTB, [Buf("actT1_g%d" % i) for i in range(3)]]
        hT_l = [hT, sb("hT1", [128, 8, 128], BF16)]; hTB_l = [hTB, Buf("hT1")]
        ffn_grp = [0]

        def ffn_views(slot):
            base = slot * 33792
            w1 = arena[:, base:base + 8 * DFE].rearrange("p (k n) -> p k n", k=8)
            w3 = arena[:, base + 8 * DFE: base + 16 * DFE].rearrange("p (k n) -> p k n", k=8)
            w2 = arena[:, base + 16 * DFE: base + 16 * DFE + 11 * D].rearrange("p (k n) -> p k n", k=11)
            return w1, w3, w2

        def ffn_load(slot, w1src, w3src, w2src):
            w1, w3, w2 = ffn_views(slot)
            g = P.group_begin([slotB[slot]])
            wload(w1, kview(w1src), g, DFE)
            wload(w3, kview(w3src), g, DFE)
            wload(w2, kview(w2src), g, D)
            P.group_end(g)

        def ffn_expert(slot, ob, obB, first, last):
            w1, w3, w2 = ffn_views(slot)
            aT, aTB = actT, actTB
            hTc, hTBc = hT, hTB

            def emit_w2_g(g0, nblk, gB):
                for jj in range(nblk):
                    j = g0 + jj
                    for half in range(2):
                        pe((lambda j, half: (lambda e: e.matmul(ob[half][:], lhsT=aT[:, j, :], rhs=w2[:, j, half * 512:(half + 1) * 512], start=(first and j == 0), stop=(last and j == 10))))(j, half),
                           [gB, slotB[slot]], [obB[half]])

            pending = None
            for g0 in range(0, 11, 4):
                nblk = min(4, 11 - g0)
                ffn_grp[0] += 1
                ts = 2 if ffn_grp[0] % 2 == 0 else 5
                b1, b1B = nb()
                b3, b3B = nb()
                for jj in range(nblk):
                    j = g0 + jj
                    for k in range(8):
                        pe((lambda j, jj, k: (lambda e: e.matmul(b1[:, jj * 128:(jj + 1) * 128], lhsT=w1[:, k, j * 128:(j + 1) * 128], rhs=hTc[:, k, :], start=(k == 0), stop=(k == 7))))(j, jj, k), [slotB[slot], hTBc], [b1B])
                    for k in range(8):
                        pe((lambda j, jj, k: (lambda e: e.matmul(b3[:, jj * 128:(jj + 1) * 128], lhsT=w3[:, k, j * 128:(j + 1) * 128], rhs=hTc[:, k, :], start=(k == 0), stop=(k == 7))))(j, jj, k), [slotB[slot], hTBc], [b3B])
                n = nblk * 128
                act((lambda n, b1: (lambda e: e.activation(out=T[ts][:, 0:n], in_=b1[:, 0:n], func=AF.Silu)))(n, b1), [b1B], [TB[ts]])
                gB = aTB[g0 // 4]
                dve((lambda n, b3, g0: (lambda e: e.tensor_tensor(out=aT[:, g0:g0 + n // 128, :].rearrange("p j t -> p (j t)"), in0=T[ts][:, 0:n], in1=b3[:, 0:n], op=ALU.mult)))(n, b3, g0), [TB[ts], b3B], [gB])
                if pending is not None:
                    emit_w2_g(*pending)
                pending = (g0, nblk, gB)
            emit_w2_g(*pending)

        def residual(slot, ob, obB, comb_ap=None, combB=None):
            Xs, Xb = X[slot], XB[slot]
            for half in range(2):
                sl = slice(half * 512, (half + 1) * 512)
                if comb_ap is None:
                    dve((lambda half, sl: (lambda e: e.tensor_tensor(out=T[3 + half][:], in0=ob[half][:], in1=GATEb[:, sl], op=ALU.mult)))(half, sl), [obB[half], gateB], [TB[3 + half]])
                else:
                    dve((lambda half, sl: (lambda e: e.scalar_tensor_tensor(out=T[3 + half][:], in0=ob[half][:], scalar=comb_ap, in1=GATEb[:, sl], op0=ALU.mult, op1=ALU.mult)))(half, sl), [obB[half], gateB, combB], [TB[3 + half]])
                pool((lambda half, sl: (lambda e: e.tensor_tensor(out=Xs[:, sl], in0=Xs[:, sl], in1=T[3 + half][:], op=ALU.add)))(half, sl), [TB[3 + half], Xb], [Xb])

        def store_x(ti, slot, dstB):
            P.dma("sp", lambda e: e.dma_start(out=out_d[ti * 128:(ti + 1) * 128, :], in_=X[slot][:]), reads=[XB[slot]], writes=[dstB])

        xdB = [Buf("xd%d" % i) for i in range(NTILES)]

        def phase_dense(l, src):
            aw = ada_setup(ada_ffn_w[l], l, "ffn", PV_NFG, PV_AFB, 2 * l + 1)
            gate_rows(aw, 2 * l + 1)
            ffn_load(0, ffn_w1[0][:, 0:DFE], ffn_w3[0][:, 0:DFE], ffn_w2[0][0:DFE, :])
            ffn_load(1, ffn_w1[0][:, DFE:DFF], ffn_w3[0][:, DFE:DFF], ffn_w2[0][DFE:DFF, :])
            bank_n[0] = 4
            load_x(0, 0, src)
            for ti in range(NTILES):
                s = ti // NT
                slot = ti % 2
                set_par(slot)
                if ti + 1 < NTILES:
                    load_x(ti + 1, (ti + 1) % 2, src)
                if ti % NT == 0:
                    gate_fetch(s)
                norm_mod(slot, s)
                ob0, ob0B = banks[4 + 2 * slot], bankB[4 + 2 * slot]
                ob1, ob1B = banks[5 + 2 * slot], bankB[5 + 2 * slot]
                ffn_expert(0, [ob0, ob1], [ob0B, ob1B], True, False)
                ffn_expert(1, [ob0, ob1], [ob0B, ob1B], False, True)
                residual(slot, [ob0, ob1], [ob0B, ob1B])
                store_x(ti, slot, xdB[ti])
            bank_n[0] = 6

        gsc_d = nc.dram_tensor("gsc", [NSEQ, 128, D], F32, kind="Internal").ap()
        gscB = [Buf("gsc%d" % s) for s in range(NSEQ)]

        def gate_rows(aw, gb_row):
            for s in range(NSEQ):
                gate_setup(aw, s, gb_row)
                P.dma("sp", (lambda s: (lambda e: e.dma_start(out=gsc_d[s], in_=GATEb[:])))(s), reads=[gateB], writes=[gscB[s]])

        def gate_fetch(s):
            P.dma("sp", lambda e: e.dma_start(out=GATEb[:], in_=gsc_d[s]), reads=[gscB[s]], writes=[gateB])

        def load_x_dep(ti, slot, src):
            if src is out_d:
                P.dma("sp", lambda e: e.dma_start(out=X[slot][:], in_=src[ti * 128:(ti + 1) * 128, :]), reads=[xdB[ti]], writes=[XB[slot]])
            else:
                P.dma("sp", lambda e: e.dma_start(out=X[slot][:], in_=src[ti * 128:(ti + 1) * 128, :]), writes=[XB[slot]])
        load_x = load_x_dep

        rw = sb("rw", [128, 8, 2 * NE], BF16); rwB = Buf("rw")
        rw32 = sb("rw32", [128, 8, NE])
        hlo = sb("hlo_mrg", [128, 8, 128], BF16); hloB = Buf("hlo")
        comb = sb("comb", [128, 4 * NE]); combB = Buf("comb")
        comb_l = [comb, sb("comb1", [128, 4 * NE])]; combB_l = [combB, Buf("comb1")]
        rt = sb("rt", [128, 64]); rtB = Buf("rt")
        basec = sb("basec", [128, NE])
        capmax = sb("capmax", [128, NE])
        Mb = sb("Mb", [128, NE], BF16)
        RT = sb("RT", [128, NTILES, 4])
        idxu = sb("idxu", [128, NTILES, 2], mybir.dt.uint32)

        def set_par(p):
            nonlocal hT, hTB, actT, actTB, comb, combB
            hT, hTB = hT_l[p], hTB_l[p]
            actT, actTB = actT_l[p], actTB_l[p]
            comb, combB = comb_l[p], combB_l[p]
        fng = cst; fngB = cstB
        hT32 = None

        def router(slot, s, ti):
            for hf in range(2):
                dve((lambda hf: (lambda e: e.tensor_tensor(out=T[hf][:].rearrange("p (k t) -> p k t", k=8), in0=T[hf][:].rearrange("p (k t) -> p k t", k=8), in1=SH[:, s, :].unsqueeze(2).broadcast_to([128, 8, 64]), op=ALU.add)))(hf), [modB, hTB], [TB[hf]])
                dve((lambda hf: (lambda e: e.tensor_tensor(out=hlo[:, :, hf * 64:(hf + 1) * 64], in0=T[hf][:].rearrange("p (k t) -> p k t", k=8), in1=hT[:, :, hf * 64:(hf + 1) * 64], op=ALU.subtract)))(hf), [TB[hf], hTB], [hloB])
            bk, bb = nb()
            n = 0
            for (lh, rc) in ((hT, 0), (hT, NE), (hlo, 0)):
                for k in range(8):
                    pe((lambda lh, rc, k, n: (lambda e: e.matmul(bk[:, 0:NE], lhsT=lh[:, k, :], rhs=rw[:, k, rc:rc + NE], start=(n == 0), stop=(n == 23))))(lh, rc, k, n), [hTB, hloB, rwB], [bb])
                    n += 1
            lg = comb[:, 8:16]
            dve(lambda e: e.tensor_tensor(out=lg, in0=bk[:, 0:NE], in1=pv[:, PVL + PV_RB: PVL + PV_RB + NE], op=ALU.add), [bb, pvB], [combB])
            dve(lambda e: e.max(out=comb[:, 16:24], in_=lg), [], [combB])
            dve(lambda e: e.tensor_scalar(out=comb[:, 24:32], in0=lg, scalar1=comb[:, 16:17], scalar2=None, op0=ALU.subtract), [], [combB])
            act(lambda e: e.activation(out=comb[:, 24:32], in_=comb[:, 24:32], func=AF.Exp), [combB], [combB])
            dve(lambda e: e.tensor_scalar(out=comb[:, 0:8], in0=lg, scalar1=comb[:, 17:18], scalar2=None, op0=ALU.is_ge), [combB], [combB])
            dve(lambda e: e.tensor_tensor(out=comb[:, 0:8], in0=comb[:, 0:8], in1=comb[:, 24:32], op=ALU.mult), [], [combB])
            dve(lambda e: e.reduce_sum(out=small[:, 4:5], in_=comb[:, 0:8], axis=AX.X), [], [combB])
            dve(lambda e: e.reciprocal(out=small[:, 5:6], in_=small[:, 4:5]), [], [combB])
            dve(lambda e: e.tensor_scalar(out=comb[:, 0:8], in0=comb[:, 0:8], scalar1=small[:, 5:6], scalar2=None, op0=ALU.mult), [], [combB])

        hscB = [Buf("hsc%d" % i) for i in range(NTILES)]
        cscB = [Buf("csc%d" % i) for i in range(NTILES)]
        outB = [Buf("out%d" % i) for i in range(NTILES)]

        def phase_moe(l, src):
            aw = ada_setup(ada_ffn_w[l], l, "ffn", PV_NFG, PV_AFB, 2 * l + 1)
            gate_rows(aw, 2 * l + 1)
            P.dma("sp", lambda e: e.dma_start(out=rw32[:], in_=kview(moe_rw[0])), writes=[rwB])
            dve(lambda e: e.tensor_copy(out=rw[:, :, 0:NE], in_=rw32[:]), [rwB], [rwB])
            dve(lambda e: e.tensor_tensor(out=rw32[:], in0=rw32[:], in1=rw[:, :, 0:NE], op=ALU.subtract), [], [rwB])
            dve(lambda e: e.tensor_copy(out=rw[:, :, NE:2 * NE], in_=rw32[:]), [], [rwB])
            P.dma("sp", lambda e: e.dma_start(out=fng[:], in_=fng_d), writes=[fngB])
            ffn_load(0, moe_w1[0, 0], moe_w3[0, 0], moe_w2[0, 0])
            bank_n[0] = 4
            units = [(ex, ti) for ex in range(NE) for ti in range(NTILES)]

            def loads(u):
                ex, ti = units[u]
                p = u % 2
                set_par(p)
                load_x(ti, p, src if ex == 0 else out_d)
                if ex > 0:
                    P.dma("sp", lambda e: e.dma_start(out=hT[:].rearrange("p k t -> p (k t)"), in_=hsc_d[ti]), reads=[hscB[ti]], writes=[hTB])
                    P.dma("sp", lambda e: e.dma_start(out=comb[:, 0:NE], in_=csc_d[ti]), reads=[cscB[ti]], writes=[combB])

            loads(0)
            for u, (ex, ti) in enumerate(units):
                slot_w = ex % 2
                if ti == 0 and ex + 1 < NE:
                    ffn_load((ex + 1) % 2, moe_w1[0, ex + 1], moe_w3[0, ex + 1], moe_w2[0, ex + 1])
                s = ti // NT
                slot = u % 2
                if u + 1 < len(units):
                    loads(u + 1)
                set_par(slot)
                if ti % NT == 0:
                    gate_fetch(s)
                if ex == 0:
                    norm_mod(slot, s)
                    router(slot, s, ti)
                    P.dma("sp", lambda e: e.dma_start(out=hsc_d[ti], in_=hT[:].rearrange("p k t -> p (k t)")), reads=[hTB], writes=[hscB[ti]])
                    P.dma("sp", lambda e: e.dma_start(out=csc_d[ti], in_=comb[:, 0:NE]), reads=[combB], writes=[cscB[ti]])
                ob0, ob0B = banks[4 + 2 * slot], bankB[4 + 2 * slot]
                ob1, ob1B = banks[5 + 2 * slot], bankB[5 + 2 * slot]
                ffn_expert(slot_w, [ob0, ob1], [ob0B, ob1B], True, True)
                residual(slot, [ob0, ob1], [ob0B, ob1B], comb_ap=comb[:, ex:ex + 1], combB=combB)
                if ex < NE - 1:
                    store_x(ti, slot, xdB[ti])
                else:
                    final_norm(ti, slot)
            bank_n[0] = 6

        def phase_moe_sparse(l, src):
            from concourse.bass import IndirectOffsetOnAxis
            U32 = mybir.dt.uint32
            aw = ada_setup(ada_ffn_w[l], l, "ffn", PV_NFG, PV_AFB, 2 * l + 1)
            gate_rows(aw, 2 * l + 1)
            P.dma("sp", lambda e: e.dma_start(out=rw32[:], in_=kview(moe_rw[0])), writes=[rwB])
            dve(lambda e: e.tensor_copy(out=rw[:, :, 0:NE], in_=rw32[:]), [rwB], [rwB])
            dve(lambda e: e.tensor_tensor(out=rw32[:], in0=rw32[:], in1=rw[:, :, 0:NE], op=ALU.subtract), [], [rwB])
            dve(lambda e: e.tensor_copy(out=rw[:, :, NE:2 * NE], in_=rw32[:]), [], [rwB])
            ffn_load(0, moe_w1[0, 0], moe_w3[0, 0], moe_w2[0, 0])
            bank_n[0] = 4
            for e_ in range(NE):
                dve((lambda e_: (lambda e: e.memset(basec[:, e_:e_ + 1], float(e_ * CAP))))(e_), [], [rtB])
                dve((lambda e_: (lambda e: e.memset(capmax[:, e_:e_ + 1], float(e_ * CAP + CAP - 1))))(e_), [], [rtB])
            hgB = Buf("Hg")
            hg_grp = P.group_begin([hgB])
            load_x(0, 0, src)
            for ti in range(NTILES):
                s = ti // NT
                slot = ti % 2
                set_par(slot)
                if ti + 1 < NTILES:
                    load_x(ti + 1, (ti + 1) % 2, src)
                norm_mod(slot, s)
                for hf in range(2):
                    dve((lambda hf: (lambda e: e.tensor_tensor(out=T[hf][:].rearrange("p (k t) -> p k t", k=8), in0=T[hf][:].rearrange("p (k t) -> p k t", k=8), in1=SH[:, s, :].unsqueeze(2).broadcast_to([128, 8, 64]), op=ALU.add)))(hf), [modB, hTB], [TB[hf]])
                    dve((lambda hf: (lambda e: e.tensor_tensor(out=hlo[:, :, hf * 64:(hf + 1) * 64], in0=T[hf][:].rearrange("p (k t) -> p k t", k=8), in1=hT[:, :, hf * 64:(hf + 1) * 64], op=ALU.subtract)))(hf), [TB[hf], hTB], [hloB])
                bk, bb = nb()
                n = 0
                for (lh, rc) in ((hT, 0), (hT, NE), (hlo, 0)):
                    for k in range(8):
                        pe((lambda lh, rc, k, n: (lambda e: e.matmul(bk[:, 0:NE], lhsT=lh[:, k, :], rhs=rw[:, k, rc:rc + NE], start=(n == 0), stop=(n == 23))))(lh, rc, k, n), [hTB, hloB, rwB], [bb])
                        n += 1
                lg = rt[:, 48:56]
                dve(lambda e: e.tensor_tensor(out=lg, in0=bk[:, 0:NE], in1=pv[:, PVL + PV_RB: PVL + PV_RB + NE], op=ALU.add), [bb, pvB], [rtB])
                dve(lambda e: e.max(out=rt[:, 0:8], in_=lg), [], [rtB])
                dve(lambda e: e.tensor_scalar(out=rt[:, 8:16], in0=lg, scalar1=rt[:, 0:1], scalar2=None, op0=ALU.is_equal), [], [rtB])
                dve(lambda e: e.tensor_scalar(out=rt[:, 16:24], in0=lg, scalar1=rt[:, 1:2], scalar2=None, op0=ALU.is_equal), [], [rtB])
                dve(lambda e: e.tensor_tensor(out=Mb[:], in0=rt[:, 8:16], in1=rt[:, 16:24], op=ALU.add), [], [rtB])
                bp, bpB = nb()
                pe(lambda e: e.matmul(bp[:, 0:8], lhsT=strilb[:], rhs=Mb[:], start=True, stop=True), [rtB, constB], [bpB])
                pe(lambda e: e.matmul(bp[:, 8:16], lhsT=onesb[:], rhs=Mb[:], start=True, stop=True), [rtB, constB], [bpB])
                dve(lambda e: e.tensor_tensor(out=rt[:, 24:32], in0=bp[:, 0:8], in1=basec[:], op=ALU.add), [bpB], [rtB])
                dve(lambda e: e.tensor_tensor(out=rt[:, 24:32], in0=rt[:, 24:32], in1=capmax[:], op=ALU.min), [], [rtB])
                dve(lambda e: e.tensor_tensor(out=basec[:], in0=basec[:], in1=bp[:, 8:16], op=ALU.add), [bpB], [rtB])
                dve(lambda e: e.tensor_tensor(out=rt[:, 32:40], in0=rt[:, 8:16], in1=rt[:, 24:32], op=ALU.mult), [], [rtB])
                dve(lambda e: e.reduce_sum(out=RT[:, ti, 0:1], in_=rt[:, 32:40], axis=AX.X), [], [rtB])
                dve(lambda e: e.tensor_tensor(out=rt[:, 32:40], in0=rt[:, 16:24], in1=rt[:, 24:32], op=ALU.mult), [], [rtB])
                dve(lambda e: e.reduce_sum(out=RT[:, ti, 1:2], in_=rt[:, 32:40], axis=AX.X), [], [rtB])
                dve(lambda e: e.tensor_copy(out=idxu[:, ti, :], in_=RT[:, ti, 0:2]), [], [rtB])
                dve(lambda e: e.tensor_tensor(out=rt[:, 40:41], in0=rt[:, 1:2], in1=rt[:, 0:1], op=ALU.subtract), [], [rtB])
                act(lambda e: e.activation(out=rt[:, 41:42], in_=rt[:, 40:41], func=AF.Exp), [rtB], [rtB])
                dve(lambda e: e.tensor_scalar(out=rt[:, 42:43], in0=rt[:, 41:42], scalar1=1.0, scalar2=None, op0=ALU.add), [], [rtB])
                dve(lambda e: e.reciprocal(out=RT[:, ti, 2:3], in_=rt[:, 42:43]), [], [rtB])
                dve(lambda e: e.tensor_tensor(out=RT[:, ti, 3:4], in0=rt[:, 41:42], in1=RT[:, ti, 2:3], op=ALU.mult), [], [rtB])
                bt, btB = nb()
                btb = bt[:].bitcast(BF16)
                for k in range(8):
                    pe((lambda k: (lambda e: e.transpose(out=btb[:, k * 128:(k + 1) * 128], in_=hT[:, k, :], identity=identb[:])))(k), [hTB, constB], [btB])
                stg = T[4 + ti % 2][:].bitcast(BF16)
                stgB = TB[4 + ti % 2]
                act(lambda e: e.activation(out=stg, in_=btb[:, 0:1024], func=AF.Copy), [btB], [stgB])
                for k in range(2):
                    P.dma("pool", (lambda k: (lambda e: e.indirect_dma_start(out=hg_d, out_offset=IndirectOffsetOnAxis(ap=idxu[:, ti, k:k + 1], axis=0), in_=stg, in_offset=None)))(k), group=hg_grp)
                    for t_ in list(stgB.w) + list(rtB.w):
                        P._need("pool", t_, P.q["pool"][-1][0])
                    stgB.r.append(hg_grp["toks"][-1])
                    rtB.r.append(hg_grp["toks"][-1])
            P.group_end(hg_grp)
            cntB = Buf("cnt"); dbgB.append(cntB)
            P.dma("sp", lambda e: e.dma_start(out=cnt_d, in_=basec[:]), reads=[rtB], writes=[cntB])
            yB = Buf("Y")
            y_grp = P.group_begin([yB])
            units = [(ex, t) for ex in range(NE) for t in range(CAP // 128)]
            stage = [xn[:], hlo[:].rearrange("p k t -> p (k t)")]
            stageB = [xnB, hloB]

            def loads2(u):
                ex, t = units[u]
                p = u % 2
                r0 = ex * CAP + t * 128
                P.dma("sp", lambda e: e.dma_start(out=stage[p], in_=hg_d[r0:r0 + 128, :]), reads=[hgB], writes=[stageB[p]])

            loads2(0)
            for u, (ex, t) in enumerate(units):
                slot_w = ex % 2
                if t == 0 and ex + 1 < NE:
                    ffn_load((ex + 1) % 2, moe_w1[0, ex + 1], moe_w3[0, ex + 1], moe_w2[0, ex + 1])
                p = u % 2
                if u + 1 < len(units):
                    loads2(u + 1)
                set_par(p)
                bt, btB = nb()
                btb = bt[:].bitcast(BF16)
                for k in range(8):
                    pe((lambda k: (lambda e: e.transpose(out=btb[:, k * 128:(k + 1) * 128], in_=stage[p][:, k * 128:(k + 1) * 128], identity=identb[:])))(k), [stageB[p], constB], [btB])
                act(lambda e: e.activation(out=hT[:].rearrange("p k t -> p (k t)"), in_=btb[:, 0:1024], func=AF.Copy), [btB], [hTB])
                ob0, ob0B = banks[4 + 2 * p], bankB[4 + 2 * p]
                ob1, ob1B = banks[5 + 2 * p], bankB[5 + 2 * p]
                ffn_expert(slot_w, [ob0, ob1], [ob0B, ob1B], True, True)
                act(lambda e: e.activation(out=X[p][:, 0:512], in_=ob0[:], func=AF.Copy), [ob0B], [XB[p]])
                dve(lambda e: e.tensor_copy(out=X[p][:, 512:1024], in_=ob1[:]), [ob1B], [XB[p]])
                r0 = ex * CAP + t * 128
                P.dma("sp", lambda e: e.dma_start(out=y_d[r0:r0 + 128, :], in_=X[p][:]), group=y_grp)
                for t_ in list(XB[p].w):
                    P._need("sp", t_, P.q["sp"][-1][0])
                XB[p].r.append(y_grp["toks"][-1])
            P.group_end(y_grp)
            bank_n[0] = 6
            P.dma("sp", lambda e: e.dma_start(out=fng[:], in_=fng_d), writes=[fngB])
            Yv = [[arena[:, (pp * 2 + k) * 2048:(pp * 2 + k + 1) * 2048].bitcast(F32) for k in range(2)] for pp in range(2)]
            YvB = [[Buf("Yv%d%d" % (pp, k)) for k in range(2)] for pp in range(2)]
            for pp in range(2):
                for k in range(2):
                    for sbuf_ in slotB:
                        YvB[pp][k].r += list(sbuf_.r) + list(sbuf_.w)

            def loads3(ti):
                p = ti % 2
                load_x(ti, p, src)
                for k in range(2):
                    P.dma("pool", (lambda k: (lambda e: e.indirect_dma_start(out=Yv[p][k], out_offset=None, in_=y_d, in_offset=IndirectOffsetOnAxis(ap=idxu[:, ti, k:k + 1], axis=0))))(k), reads=[yB, rtB], writes=[YvB[p][k]])

            loads3(0)
            for ti in range(NTILES):
                s = ti // NT
                p = ti % 2
                if ti + 1 < NTILES:
                    loads3(ti + 1)
                if ti % NT == 0:
                    gate_fetch(s)
                Xs, Xb = X[p], XB[p]
                for half in range(2):
                    sl = slice(half * 512, (half + 1) * 512)
                    dve((lambda sl, half: (lambda e: e.tensor_scalar(out=T[half][:], in0=Yv[p][0][:, sl], scalar1=RT[:, ti, 2:3], scalar2=None, op0=ALU.mult)))(sl, half), [YvB[p][0], rtB], [TB[half]])
                    dve((lambda sl, half: (lambda e: e.scalar_tensor_tensor(out=T[half][:], in0=Yv[p][1][:, sl], scalar=RT[:, ti, 3:4], in1=T[half][:], op0=ALU.mult, op1=ALU.add)))(sl, half), [YvB[p][1], rtB], [TB[half]])
                    dve((lambda sl, half: (lambda e: e.tensor_tensor(out=T[half][:], in0=T[half][:], in1=GATEb[:, sl], op=ALU.mult)))(sl, half), [gateB], [TB[half]])
                    pool((lambda sl, half: (lambda e: e.tensor_tensor(out=Xs[:, sl], in0=Xs[:, sl], in1=T[half][:], op=ALU.add)))(sl, half), [TB[half], Xb], [Xb])
                final_norm(ti, p)

        def final_norm(ti, slot):
            Xs, Xb = X[slot], XB[slot]
            act(lambda e: e.activation(out=xn[:], in_=Xs[:], func=AF.Square, accum_out=small[:, 0:1]), [Xb], [xnB])
            dve(lambda e: e.tensor_scalar(out=small[:, 1:2], in0=small[:, 0:1], scalar1=1.0 / D, scalar2=EPS, op0=ALU.mult, op1=ALU.add), [xnB], [xnB])
            act(lambda e: e.activation(out=small[:, 1:2], in_=small[:, 1:2], func=AF.Ln), [xnB], [xnB])
            act(lambda e: e.activation(out=small[:, 2:3], in_=small[:, 1:2], func=AF.Exp, scale=-0.5), [xnB], [xnB])
            dve(lambda e: e.scalar_tensor_tensor(out=Xs[:], in0=Xs[:], scalar=small[:, 2:3], in1=fng[:], op0=ALU.mult, op1=ALU.mult), [xnB, fngB], [Xb])
            P.dma("sp", lambda e: e.dma_start(out=out_d[ti * 128:(ti + 1) * 128, :], in_=Xs[:]), reads=[Xb], writes=[xdB[ti], outB[ti]])

        def mixer_bufs():
            pass

        if "A" in phases or "C" in phases:
            qt = sb("qt", [128, 512], BF16); kt = sb("kt", [128, 512], BF16); qE = sb("qE", [128, 512], BF16); kdT = sb("kdT", [128, 512], BF16)
            hgB = Buf("hgops")
            vtok = sb("vtok", [128, 512], BF16); vtokB = Buf("vtok")
            kdtok = sb("kdtok", [128, 512], BF16); kdtokB = Buf("kdtok")
            Pm = sb("Pm", [128, 4, 64], BF16); PmB = Buf("Pm")
            S32 = sb("S32", [128, 4, 128]); Sbf = sb("Sbf", [128, 4, 128], BF16); SB_ = Buf("S")
            Gs = sb("Gs", [128, 512], BF16); GsB = Buf("Gs")
            yhgT = sb("yhgT", [128, 4, 128], BF16); yhgB = Buf("yhgT")
            ymlT = sb("ymlT", [128, 4, 128], BF16); ymlB = Buf("ymlT")
            eA = sb("eA", [128, 16]); eAB = Buf("eA")
            U = sb("U", [128, 4, 131]); UB = Buf("U")
            ucT = sb("ucT", [128, 4, 128], BF16); ucB = Buf("ucT")
            qTm = sb("qTm", [64, 4, 128], BF16); kTm = sb("kTm", [64, 4, 128], BF16); qkB = Buf("qkm")
            ktok = sb("ktok", [128, 4, 64], BF16); ktokB = Buf("ktok")
            vaug = sb("vaug", [128, 4, 130], BF16); vaugB = Buf("vaug")
            mosg = sb("mosg", [128, 512], BF16); mosgB = Buf("mosg")
            _a1 = actT_l[1][:].rearrange("p j t -> p (j t)")
            Pml = _a1[:, 512:1024].rearrange("p (h t) -> p h t", h=4); PmlB = actTB_l[1][0]
            C32 = sb("C32", [64, 4, 130]); Cbf = sb("Cbf", [64, 4, 130], BF16); CB_ = Buf("C")
            gts = sb("gts", [128, 64]); gtsB = Buf("gts")
            gthl = sb("gthl", [128, 8], BF16);
            yml = _a1[:, 0:512]; ymlTokB = actTB_l[1][0]
            SGA = hT_l[1]; SGB = actT_l[0][:, 0:8, :]; sgB = hTB_l[1]; sgB2 = actTB_l[0][0]; sgB3 = actTB_l[0][1]
            mrg = hlo; mrgB = hloB
            wqk = sb("wqk", [128, 2, 4, 64], BF16); wqkB = Buf("wqk")

        def phase_mixer(l, src):
            aw = ada_setup(ada_mix_w[l], l, "mix", PV_NMG, PV_AMB, 2 * l)
            gate_rows(aw, 2 * l)
            Win = arena[:, 0:8 * INC].rearrange("p (k n) -> p k n", k=8)
            o = 8 * INC
            Wbh = arena[:, o:o + 4096].rearrange("p (k n) -> p k n", k=4)
            Wbm = arena[:, o + 4096:o + 8192].rearrange("p (k n) -> p k n", k=4)
            Wo = arena[:, o + 8192:o + 16384].rearrange("p (k n) -> p k n", k=8)
            WB = slotB
            g = P.group_begin(WB + [wqkB])
            wload(Win, kview(w_in[l]), g, INC)
            wload(Wbh, kview(w_br_hg[l]), g, D)
            wload(Wbm, kview(w_br_ml[l]), g, D)
            wload(Wo, kview(w_out[l]), g, D)
            for hh in range(4):
                P.dma("pool", (lambda hh: (lambda e: e.dma_start(out=wqk[:, 0, hh, :], in_=ml_wq[l, hh])))(hh), group=g)
                P.dma("pool", (lambda hh: (lambda e: e.dma_start(out=wqk[:, 1, hh, :], in_=ml_wk[l, hh])))(hh), group=g)
            P.group_end(g)
            if l == 0:
                dve(lambda e: e.memset(lbc[:, 0, :], 0.0), [], [lbB])
                dve(lambda e: e.memset(lbc[:, 1, :], 1.0), [], [lbB])
            else:
                dve(lambda e: e.tensor_tensor(out=lbc[:, 0, :], in0=pv[:, PVL + PV_LB:PVL + PV_LB + 4], in1=pv[:, PV_LB:PV_LB + 4], op=ALU.subtract), [pvB], [lbB])
                act(lambda e: e.activation(out=lbc[:, 0, :], in_=lbc[:, 0, :], func=AF.Sigmoid), [], [lbB])
                dve(lambda e: e.tensor_scalar(out=lbc[:, 1, :], in0=lbc[:, 0, :], scalar1=-1.0, scalar2=1.0, op0=ALU.mult, op1=ALU.add), [], [lbB])
            pvl = l * PVL

            def proj_fm(c0, nblk, bk, bb):
                for j in range(nblk):
                    for k in range(8):
                        pe((lambda j, k: (lambda e: e.matmul(bk[:, j * 128:(j + 1) * 128], lhsT=Win[:, k, c0 + j * 128:c0 + (j + 1) * 128], rhs=hT[:, k, :], start=(k == 0), stop=(k == 7))))(j, k), [WB[0], hTB], [bb])

            def proj_tm(c0, n, bk, bb):
                for k in range(8):
                    pe((lambda k: (lambda e: e.matmul(bk[:, 0:n], lhsT=hT[:, k, :], rhs=Win[:, k, c0:c0 + n], start=(k == 0), stop=(k == 7))))(k), [WB[0], hTB], [bb])

            set_par(0)
            bank_n[0] = 6
            for ti in range(NTILES):
                s = ti // NT
                slot = ti % 2
                first = (ti % NT == 0)
                if first:
                    gate_fetch(s)
                    dve(lambda e: e.memset(S32[:], 0.0), [], [SB_])
                    dve(lambda e: e.memset(Sbf[:], 0.0), [], [SB_])
                    dve(lambda e: e.memset(C32[:], 0.0), [], [CB_])
                    dve(lambda e: e.memset(Cbf[:], 0.0), [], [CB_])
                    dve(lambda e: e.memset(U[:], 0.0), [], [UB])
                load_x(ti, slot, src)
                norm_mod(slot, s)

                bq, bqB = nb(); proj_fm(C_Q, 4, bq, bqB)
                bf, bfB = nb(); proj_fm(C_F, 4, bf, bfB)
                bg, bgB = nb(); proj_fm(C_G, 4, bg, bgB)
                bv, bvB = nb(); proj_tm(C_I, 512, bv, bvB)
                Q, Fb, LF, KK, E1, E2 = T[0], T[1], T[2], T[3], T[4], T[5]
                act(lambda e: e.activation(out=Q[:], in_=bq[:], func=AF.Silu), [bqB], [TB[0]])
                act(lambda e: e.activation(out=Gs[:], in_=bg[:], func=AF.Silu), [bgB], [GsB])
                act(lambda e: e.activation(out=Fb[:], in_=bf[:], func=AF.Sigmoid), [bfB], [TB[1]])
                act(lambda e: e.activation(out=vtok[:], in_=bv[:], func=AF.Copy), [bvB], [vtokB])
                bu, buB = nb(); proj_fm(C_MU, 4, bu, buB)
                bmv, bmvB = nb(); proj_tm(C_MV, 512, bmv, bmvB)
                bmo, bmoB = nb(); proj_tm(C_MO, 512, bmo, bmoB)
                bgt, bgtB = nb(); proj_tm(C_MI, 8, bgt, bgtB)
                act(lambda e: e.activation(out=U[:, :, 3:131], in_=bu[:].rearrange("p (h t) -> p h t", h=4), func=AF.Copy), [buB], [UB])
                act(lambda e: e.activation(out=mosg[:], in_=bmo[:], func=AF.Sigmoid), [bmoB], [mosgB])
                act(lambda e: e.activation(out=vaug[:, :, 0:128], in_=bmv[:].rearrange("p (h t) -> p h t", h=4), func=AF.Copy), [bmvB], [vaugB])
                dve(lambda e: e.memset(vaug[:, :, 128:130], 1.0), [], [vaugB])
                dve(lambda e: e.tensor_copy(out=gts[:, 0:4], in_=bgt[:, 0:4]), [bgtB], [gtsB])
                dve(lambda e: e.tensor_tensor(out=gts[:, 4:8], in0=bgt[:, 4:8], in1=pv[:, pvl + PV_FB: pvl + PV_FB + 4], op=ALU.add), [bgtB, pvB], [gtsB])
                for (c0, SG) in ((C_GA, SGA), (C_GB, SGB)):
                    for half in range(2):
                        bgx, bgxB = nb()
                        proj_fm(c0 + half * 512, 4, bgx, bgxB)
                        act((lambda SG, half, bgx: (lambda e: e.activation(out=SG[:, half * 4:(half + 1) * 4, :].rearrange("p k t -> p (k t)"), in_=bgx[:], func=AF.Sigmoid)))(SG, half, bgx), [bgxB], [sgB, sgB2, sgB3])
                rrH = [0]; rrM = [0]

                def nbH():
                    i = rrH[0] % 2
                    rrH[0] += 1
                    return banks[i], bankB[i]

                def nbM():
                    i = 2 + rrM[0] % 3
                    rrM[0] += 1
                    return banks[i], bankB[i]

                def chainH():
                    for hh in range(4):
                        dve((lambda hh: (lambda e: e.tensor_scalar(out=Fb[:, hh * 128:(hh + 1) * 128], in0=Fb[:, hh * 128:(hh + 1) * 128], scalar1=lbc[:, 1, hh:hh + 1], scalar2=lbc[:, 0, hh:hh + 1], op0=ALU.mult, op1=ALU.add)))(hh), [lbB], [TB[1]])
                        yield
                    act(lambda e: e.activation(out=LF[:], in_=Fb[:], func=AF.Ln), [TB[1]], [TB[2]])
                    yield
                    dve(lambda e: e.tensor_scalar(out=KK[:], in0=Fb[:], scalar1=-1.0, scalar2=1.0, op0=ALU.mult, op1=ALU.add), [TB[1]], [TB[3]])
                    yield
                    A_ = Fb
                    dve(lambda e: e.tensor_tensor_scan(out=A_[:], data0=rst[:], data1=LF[:], initial=0.0, op0=ALU.mult, op1=ALU.add), [TB[2], constB], [TB[1]])
                    yield
                    A4 = A_[:].rearrange("p (g t) -> p g t", t=64)
                    Dd = LF
                    dve(lambda e: e.tensor_tensor(out=Dd[:].rearrange("p (g t) -> p g t", t=64), in0=A4, in1=A4[:, :, 31:32].broadcast_to([128, 8, 64]), op=ALU.subtract), [TB[1]], [TB[2]])
                    yield
                    dve(lambda e: e.tensor_copy(out=eA[:, 0:8], in_=A4[:, :, 31]), [TB[1]], [eAB])
                    yield
                    dve(lambda e: e.tensor_copy(out=eA[:, 8:16], in_=Dd[:].rearrange("p (g t) -> p g t", t=64)[:, :, 63]), [TB[2]], [eAB])
                    yield
                    dve(lambda e: e.tensor_copy(out=small[:, 8:16], in_=A4[:, :, 63]), [TB[1]], [eAB])
                    yield
                    dve(lambda e: e.tensor_scalar(out=Dd[:], in0=Dd[:], scalar1=40.0, scalar2=-40.0, op0=ALU.min, op1=ALU.max), [eAB], [TB[2]])
                    yield
                    act(lambda e: e.activation(out=E1[:], in_=Dd[:], func=AF.Exp), [TB[2]], [TB[4]])
                    yield
                    act(lambda e: e.activation(out=E2[:], in_=Dd[:], func=AF.Exp, scale=-1.0), [TB[2]], [TB[5]])
                    yield
                    act(lambda e: e.activation(out=eA[:], in_=eA[:], func=AF.Exp), [eAB], [eAB])
                    yield
                    act(lambda e: e.activation(out=small[:, 8:16], in_=small[:, 8:16], func=AF.Exp), [eAB], [eAB])
                    yield
                    dve(lambda e: e.tensor_tensor(out=Q[:], in0=Q[:], in1=E1[:], op=ALU.mult), [TB[4]], [TB[0]])
                    yield
                    dve(lambda e: e.tensor_tensor(out=KK[:], in0=KK[:], in1=E2[:], op=ALU.mult), [TB[5]], [TB[3]])
                    yield
                    act(lambda e: e.activation(out=qt[:], in_=Q[:], func=AF.Copy), [TB[0]], [hgB])
                    yield
                    act(lambda e: e.activation(out=kt[:], in_=KK[:], func=AF.Copy), [TB[3]], [hgB])
                    yield
                    dve(lambda e: e.tensor_tensor(out=qE[:].rearrange("p (g t) -> p g t", t=64), in0=Q[:].rearrange("p (g t) -> p g t", t=64), in1=eA[:, 0:8].unsqueeze(2).broadcast_to([128, 8, 64]), op=ALU.mult), [TB[0], eAB], [hgB])
                    yield
                    dve(lambda e: e.tensor_tensor(out=kdT[:].rearrange("p (g t) -> p g t", t=64), in0=KK[:].rearrange("p (g t) -> p g t", t=64), in1=eA[:, 8:16].unsqueeze(2).broadcast_to([128, 8, 64]), op=ALU.mult), [TB[3], eAB], [hgB])
                    yield
                    if debug and ti == 1:
                        def dump2(nm, c0, ap, bufs):
                            b = Buf(nm + str(c0)); dbgB.append(b)
                            P.dma("pool", lambda e: e.dma_start(out=dbg[nm][:, c0:c0 + 512], in_=ap), reads=bufs, writes=[b])
                        dump2("dbg_hT", 0, A_[:], [TB[1]])
                        dump2("dbg_hT", 512, Dd[:], [TB[2]])
                        dump2("dbg_gate", 0, Q[:], [TB[0]])
                        dump2("dbg_gate", 512, KK[:], [TB[3]])
                        dump2("dbg_xn", 0, E1[:], [TB[4]])
                        dump2("dbg_xn", 512, E2[:], [TB[5]])
                    bt, btB = nbH()
                    btb = bt[:].bitcast(BF16)
                    for hh in range(4):
                        pe((lambda hh: (lambda e: e.transpose(out=btb[:, hh * 128:(hh + 1) * 128], in_=kdT[:, hh * 128:(hh + 1) * 128], identity=identb[:])))(hh), [hgB, constB], [btB])
                        yield
                    act(lambda e: e.activation(out=kdtok[:], in_=btb[:, 0:512], func=AF.Copy), [btB], [kdtokB])
                    yield
                    bs, bsB = nbH()
                    for hh in range(4):
                        for c in range(2):
                            g = hh * 2 + c
                            pe((lambda hh, c, g: (lambda e: e.matmul(bs[c * 64:(c + 1) * 64, hh * 64:(hh + 1) * 64], lhsT=kt[:, g * 64:(g + 1) * 64], rhs=qt[:, g * 64:(g + 1) * 64], start=True, stop=True)))(hh, c, g), [hgB], [bsB])
                            yield
                    dve(lambda e: e.tensor_tensor(out=Pm[:], in0=bs[:, 0:256].rearrange("p (h t) -> p h t", h=4), in1=m64[:].unsqueeze(1).broadcast_to([128, 4, 64]), op=ALU.mult), [bsB, constB], [PmB])
                    yield
                    bo, boB = banks[6], bankB[6]
                    for c in range(2):
                        rs = slice(c * 64, (c + 1) * 64)
                        for hh in range(4):
                            g = hh * 2 + c
                            pe((lambda hh, c, g, rs: (lambda e: e.matmul(bo[:, g * 64:(g + 1) * 64], lhsT=vtok[rs, hh * 128:(hh + 1) * 128], rhs=Pm[rs, hh, :], start=True, stop=False)))(hh, c, g, rs), [vtokB, PmB], [boB])
                            yield
                            pe((lambda hh, c, g: (lambda e: e.matmul(bo[:, g * 64:(g + 1) * 64], lhsT=Sbf[:, hh, :], rhs=qE[:, g * 64:(g + 1) * 64], start=False, stop=True)))(hh, c, g), [SB_, hgB], [boB])
                            yield
                        bd, bdB = nbH()
                        for hh in range(4):
                            pe((lambda hh, rs: (lambda e: e.matmul(bd[:, hh * 128:(hh + 1) * 128], lhsT=kdtok[rs, hh * 128:(hh + 1) * 128], rhs=vtok[rs, hh * 128:(hh + 1) * 128], start=True, stop=True)))(hh, rs), [kdtokB, vtokB], [bdB])
                            yield
                        for hh in range(4):
                            g = hh * 2 + c
                            dve((lambda hh, g, bd: (lambda e: e.scalar_tensor_tensor(out=S32[:, hh, :], in0=S32[:, hh, :], scalar=small[:, 8 + g:9 + g], in1=bd[:, hh * 128:(hh + 1) * 128], op0=ALU.mult, op1=ALU.add)))(hh, g, bd), [bdB, eAB], [SB_])
                            yield
                        act(lambda e: e.activation(out=Sbf[:], in_=S32[:], func=AF.Copy), [], [SB_])
                        yield
                    OS = T[4]
                    act(lambda e: e.activation(out=OS[:], in_=bo[:], func=AF.Copy), [boB], [TB[4]])
                    yield
                    SQ = T[5]
                    act(lambda e: e.activation(out=SQ[:].bitcast(BF16)[:, 0:512], in_=bo[:], func=AF.Square), [boB], [TB[5]])
                    yield
                    bn, bnB = nbH()
                    pe(lambda e: e.matmul(bn[:], lhsT=onesb[:], rhs=SQ[:].bitcast(BF16)[:, 0:512], start=True, stop=True), [TB[5], constB], [bnB])
                    yield
                    R = T[2]
                    dve(lambda e: e.tensor_scalar(out=R[:], in0=bn[:], scalar1=1.0 / 128, scalar2=EPS, op0=ALU.mult, op1=ALU.add), [bnB], [TB[2]])
                    yield
                    act(lambda e: e.activation(out=R[:], in_=R[:], func=AF.Ln), [], [TB[2]])
                    yield
                    act(lambda e: e.activation(out=R[:], in_=R[:], func=AF.Exp, scale=-0.5), [], [TB[2]])
                    yield
                    dve(lambda e: e.tensor_tensor(out=OS[:], in0=OS[:], in1=R[:], op=ALU.mult), [TB[2]], [TB[4]])
                    yield
                    dve(lambda e: e.tensor_tensor(out=OS[:], in0=OS[:], in1=Gs[:], op=ALU.mult), [GsB], [TB[4]])
                    yield
                    for hh in range(4):
                        dve((lambda hh: (lambda e: e.tensor_scalar(out=yhgT[:, hh, :], in0=OS[:, hh * 128:(hh + 1) * 128], scalar1=pv[:, pvl + PV_HNG + hh: pvl + PV_HNG + hh + 1], scalar2=None, op0=ALU.mult)))(hh), [TB[4], pvB], [yhgB])
                        yield

                    yield
                def chainM():
                    CV = X[1 - slot][:, 0:512]
                    for hh in range(4):
                        cw = pvl + PV_CW
                        dve((lambda hh, cw: (lambda e: e.tensor_scalar(out=CV[:, hh * 128:(hh + 1) * 128], in0=U[:, hh, 0:128], scalar1=pv[:, cw + 0 * 4 + hh: cw + 0 * 4 + hh + 1], scalar2=pv[:, pvl + PV_CB + hh: pvl + PV_CB + hh + 1], op0=ALU.mult, op1=ALU.add)))(hh, cw), [UB, pvB], [XB[1 - slot]])
                        yield
                        for j in range(1, 4):
                            dve((lambda hh, cw, j: (lambda e: e.scalar_tensor_tensor(out=CV[:, hh * 128:(hh + 1) * 128], in0=U[:, hh, j:j + 128], scalar=pv[:, cw + j * 4 + hh: cw + j * 4 + hh + 1], in1=CV[:, hh * 128:(hh + 1) * 128], op0=ALU.mult, op1=ALU.add)))(hh, cw, j), [UB, pvB], [XB[1 - slot]])
                            yield
                    pool(lambda e: e.tensor_copy(out=U[:, :, 0:3], in_=U[:, :, 128:131]), [], [UB])
                    yield
                    act(lambda e: e.activation(out=ucT[:].rearrange("p h t -> p (h t)"), in_=CV[:], func=AF.Silu), [XB[1 - slot]], [ucB])
                    yield
                    act(lambda e: e.activation(out=gts[:, 4:8], in_=gts[:, 4:8], func=AF.Exp, scale=-1.0), [], [gtsB])
                    yield
                    act(lambda e: e.activation(out=gts[:, 8:12], in_=gts[:, 4:8], func=AF.Ln, bias=1.0), [], [gtsB])
                    yield
                    dve(lambda e: e.tensor_scalar(out=gts[:, 8:12], in0=gts[:, 8:12], scalar1=-1.0, scalar2=None, op0=ALU.mult), [], [gtsB])
                    yield
                    dve(lambda e: e.tensor_copy(out=gthl[:, 0:4], in_=gts[:, 8:12]), [], [gtsB])
                    yield
                    dve(lambda e: e.tensor_tensor(out=gts[:, 32:36], in0=gts[:, 8:12], in1=gthl[:, 0:4], op=ALU.subtract), [], [gtsB])
                    yield
                    dve(lambda e: e.tensor_copy(out=gthl[:, 4:8], in_=gts[:, 32:36]), [], [gtsB])
                    yield
                    bc, bcB = nbM()
                    pe(lambda e: e.matmul(bc[:, 0:4], lhsT=trib[:], rhs=gthl[:, 0:4], start=True, stop=False), [gtsB, constB], [bcB])
                    yield
                    pe(lambda e: e.matmul(bc[:, 0:4], lhsT=trib[:], rhs=gthl[:, 4:8], start=False, stop=True), [gtsB, constB], [bcB])
                    yield
                    pe(lambda e: e.matmul(bc[:, 4:8], lhsT=onesb[:], rhs=gthl[:, 0:4], start=True, stop=False), [gtsB, constB], [bcB])
                    yield
                    pe(lambda e: e.matmul(bc[:, 4:8], lhsT=onesb[:], rhs=gthl[:, 4:8], start=False, stop=True), [gtsB, constB], [bcB])
                    yield
                    dve(lambda e: e.tensor_copy(out=gts[:, 12:20], in_=bc[:, 0:8]), [bcB], [gtsB])
                    yield
                    dve(lambda e: e.tensor_tensor(out=gts[:, 20:24], in0=gts[:, 0:4], in1=gts[:, 12:16], op=ALU.subtract), [], [gtsB])
                    yield
                    dve(lambda e: e.tensor_tensor(out=gts[:, 28:32], in0=gts[:, 20:24], in1=gts[:, 16:20], op=ALU.add), [], [gtsB])
                    yield
                    dve(lambda e: e.tensor_copy(out=gts[:, 24:28], in_=gts[:, 12:16]), [], [gtsB])
                    yield
                    act(lambda e: e.activation(out=gts[:, 20:32], in_=gts[:, 20:32], func=AF.Exp), [], [gtsB])
                    yield
                    act(lambda e: e.activation(out=gts[:, 36:40], in_=gts[:, 16:20], func=AF.Exp), [], [gtsB])
                    yield
                    bqk, bqkB = nbM()
                    for hh in range(4):
                        pe((lambda hh: (lambda e: e.matmul(bqk[0:64, hh * 128:(hh + 1) * 128], lhsT=wqk[:, 0, hh, :], rhs=ucT[:, hh, :], start=True, stop=True)))(hh), [wqkB, ucB], [bqkB])
                        yield
                    bkk, bkkB = nbM()
                    for hh in range(4):
                        pe((lambda hh: (lambda e: e.matmul(bkk[0:64, hh * 128:(hh + 1) * 128], lhsT=wqk[:, 1, hh, :], rhs=ucT[:, hh, :], start=True, stop=True)))(hh), [wqkB, ucB], [bkkB])
                        yield
                    bkt, bktB = nbM()
                    for hh in range(4):
                        pe((lambda hh: (lambda e: e.matmul(bkt[:, hh * 64:(hh + 1) * 64], lhsT=ucT[:, hh, :], rhs=wqk[:, 1, hh, :], start=True, stop=True)))(hh), [wqkB, ucB], [bktB])
                        yield
                    act(lambda e: e.activation(out=qTm[:].rearrange("p h t -> p (h t)"), in_=bqk[0:64, :], func=AF.Copy, scale=0.125), [bqkB], [qkB])
                    yield
                    act(lambda e: e.activation(out=kTm[:].rearrange("p h t -> p (h t)"), in_=bkk[0:64, :], func=AF.Copy), [bkkB], [qkB])
                    yield
                    dve(lambda e: e.tensor_tensor(out=ktok[:], in0=bkt[:, 0:256].rearrange("p (h e) -> p h e", h=4), in1=gts[:, 28:32].unsqueeze(2).broadcast_to([128, 4, 64]), op=ALU.mult), [bktB, gtsB], [ktokB])
                    yield
                    bsm, bsmB = nbM()
                    for hh in range(4):
                        pe((lambda hh: (lambda e: e.matmul(bsm[:, hh * 128:(hh + 1) * 128], lhsT=kTm[:, hh, :], rhs=qTm[:, hh, :], start=True, stop=True)))(hh), [qkB], [bsmB])
                        yield
                    for hh in range(4):
                        dve((lambda hh: (lambda e: e.scalar_tensor_tensor(out=Pml[:, hh, :], in0=bsm[:, hh * 128:(hh + 1) * 128], scalar=gts[:, 20 + hh:21 + hh], in1=caus[:], op0=ALU.mult, op1=ALU.mult)))(hh), [bsmB, gtsB, constB], [PmlB])
                        yield
                    HM = X[1 - slot][:, 512:1024]
                    bnum = []
                    for pair in range(2):
                        bn2, bn2B = (banks[7], bankB[7]) if pair == 0 else (banks[5], bankB[5])
                        bnum.append((bn2, bn2B))
                        for hq in range(2):
                            hh = pair * 2 + hq
                            pe((lambda hh, hq, bn2: (lambda e: e.matmul(bn2[:, hq * 130:hq * 130 + 130], lhsT=Pml[:, hh, :], rhs=vaug[:, hh, :], start=True, stop=False)))(hh, hq, bn2), [PmlB, vaugB], [bn2B])
                            yield
                            pe((lambda hh, hq, bn2: (lambda e: e.matmul(bn2[:, hq * 130:hq * 130 + 130], lhsT=qTm[:, hh, :], rhs=Cbf[:, hh, :], start=False, stop=True)))(hh, hq, bn2), [qkB, CB_], [bn2B])
                            yield
                    bcs, bcsB = nbM()
                    for hh in range(4):
                        pe((lambda hh: (lambda e: e.matmul(bcs[0:64, hh * 128:hh * 128 + 128], lhsT=ktok[:, hh, :], rhs=vaug[:, hh, 0:128], start=True, stop=True)))(hh), [ktokB, vaugB], [bcsB])
                        yield
                    bcn, bcnB = nbM()
                    for hh in range(4):
                        pe((lambda hh: (lambda e: e.matmul(bcn[0:64, hh * 2:hh * 2 + 2], lhsT=ktok[:, hh, :], rhs=vaug[:, hh, 128:130], start=True, stop=True)))(hh), [ktokB, vaugB], [bcnB])
                        yield
                    for hh in range(4):
                        dve((lambda hh: (lambda e: e.scalar_tensor_tensor(out=C32[:, hh, 0:128], in0=C32[:, hh, 0:128], scalar=gts[0:64, 36 + hh:37 + hh], in1=bcs[0:64, hh * 128:(hh + 1) * 128], op0=ALU.mult, op1=ALU.add)))(hh), [bcsB, gtsB], [CB_])
                        yield
                        dve((lambda hh: (lambda e: e.scalar_tensor_tensor(out=C32[:, hh, 128:130], in0=C32[:, hh, 128:130], scalar=gts[0:64, 36 + hh:37 + hh], in1=bcn[0:64, hh * 2:hh * 2 + 2], op0=ALU.mult, op1=ALU.add)))(hh), [bcnB, gtsB], [CB_])
                        yield
                    for pair in range(2):
                        bn2, bn2B = bnum[pair]
                        for hq in range(2):
                            hh = pair * 2 + hq
                            c0 = hq * 130
                            dve((lambda hh, c0, bn2: (lambda e: e.tensor_scalar(out=gts[:, 52 + hh:53 + hh], in0=bn2[:, c0 + 128:c0 + 129], scalar1=gts[:, 24 + hh:25 + hh], scalar2=None, op0=ALU.mult)))(hh, c0, bn2), [bn2B], [gtsB])
                            yield
                            dve((lambda hh: (lambda e: e.scalar_tensor_tensor(out=gts[:, 40 + hh:41 + hh], in0=gts[:, 52 + hh:53 + hh], scalar=-1.0, in1=gts[:, 52 + hh:53 + hh], op0=ALU.mult, op1=ALU.max)))(hh), [], [gtsB])
                            yield
                            dve((lambda hh: (lambda e: e.tensor_scalar(out=gts[:, 40 + hh:41 + hh], in0=gts[:, 40 + hh:41 + hh], scalar1=1.0, scalar2=None, op0=ALU.max)))(hh), [], [gtsB])
                            yield
                            dve((lambda hh: (lambda e: e.reciprocal(out=gts[:, 44 + hh:45 + hh], in_=gts[:, 40 + hh:41 + hh])))(hh), [], [gtsB])
                            yield
                            dve((lambda hh: (lambda e: e.tensor_tensor(out=gts[:, 44 + hh:45 + hh], in0=gts[:, 44 + hh:45 + hh], in1=gts[:, 24 + hh:25 + hh], op=ALU.mult)))(hh), [], [gtsB])
                            yield
                            dve((lambda hh, c0, bn2: (lambda e: e.tensor_scalar(out=HM[:, hh * 128:(hh + 1) * 128], in0=bn2[:, c0:c0 + 128], scalar1=gts[:, 44 + hh:45 + hh], scalar2=None, op0=ALU.mult)))(hh, c0, bn2), [bn2B, gtsB], [XB[1 - slot]])
                            yield
                            act((lambda hh: (lambda e: e.activation(out=CV[:, hh * 128:(hh + 1) * 128], in_=HM[:, hh * 128:(hh + 1) * 128], func=AF.Square, accum_out=gts[:, 48 + hh:49 + hh])))(hh), [XB[1 - slot]], [XB[1 - slot], gtsB])
                            yield
                    act(lambda e: e.activation(out=Cbf[:], in_=C32[:], func=AF.Copy), [], [CB_])
                    yield
                    dve(lambda e: e.tensor_scalar(out=gts[:, 48:52], in0=gts[:, 48:52], scalar1=1.0 / 128, scalar2=EPS, op0=ALU.mult, op1=ALU.add), [], [gtsB])
                    yield
                    act(lambda e: e.activation(out=gts[:, 48:52], in_=gts[:, 48:52], func=AF.Ln), [], [gtsB])
                    yield
                    act(lambda e: e.activation(out=gts[:, 48:52], in_=gts[:, 48:52], func=AF.Exp, scale=-0.5), [], [gtsB])
                    yield
                    for hh in range(4):
                        dve((lambda hh: (lambda e: e.scalar_tensor_tensor(out=yml[:, hh * 128:(hh + 1) * 128], in0=HM[:, hh * 128:(hh + 1) * 128], scalar=gts[:, 48 + hh:49 + hh], in1=mosg[:, hh * 128:(hh + 1) * 128], op0=ALU.mult, op1=ALU.mult)))(hh), [XB[1 - slot], gtsB, mosgB], [ymlTokB])
                        yield
                    bty, btyB = nbM()
                    btyb = bty[:].bitcast(BF16)
                    for hh in range(4):
                        pe((lambda hh: (lambda e: e.transpose(out=btyb[:, hh * 128:(hh + 1) * 128], in_=yml[:, hh * 128:(hh + 1) * 128], identity=identb[:])))(hh), [ymlTokB, constB], [btyB])
                        yield
                    for hh in range(4):
                        dve((lambda hh: (lambda e: e.tensor_scalar(out=ymlT[:, hh, :], in0=btyb[:, hh * 128:(hh + 1) * 128], scalar1=pv[:, pvl + PV_MNG + hh: pvl + PV_MNG + hh + 1], scalar2=None, op0=ALU.mult)))(hh), [btyB, pvB], [ymlB])
                        yield

                    yield
                alive = [chainH(), chainM()]
                while alive:
                    for g_ in list(alive):
                        try:
                            next(g_)
                        except StopIteration:
                            alive.remove(g_)

                for half in range(2):
                    bph, bphB = nb()
                    bpm, bpmB = nb()
                    for jj in range(4):
                        j = half * 4 + jj
                        for k in range(4):
                            pe((lambda j, jj, k, bph: (lambda e: e.matmul(bph[:, jj * 128:(jj + 1) * 128], lhsT=Wbh[:, k, j * 128:(j + 1) * 128], rhs=yhgT[:, k, :], start=(k == 0), stop=(k == 3))))(j, jj, k, bph), [WB[1], yhgB], [bphB])
                        for k in range(4):
                            pe((lambda j, jj, k, bpm: (lambda e: e.matmul(bpm[:, jj * 128:(jj + 1) * 128], lhsT=Wbm[:, k, j * 128:(j + 1) * 128], rhs=ymlT[:, k, :], start=(k == 0), stop=(k == 3))))(j, jj, k, bpm), [WB[1], ymlB], [bpmB])
                    dve((lambda half, bph: (lambda e: e.tensor_tensor(out=T[3][:], in0=bph[:], in1=SGA[:, half * 4:(half + 1) * 4, :].rearrange("p k t -> p (k t)"), op=ALU.mult)))(half, bph), [bphB, sgB, sgB2, sgB3], [TB[3]])
                    dve((lambda half, bpm: (lambda e: e.tensor_tensor(out=T[5][:], in0=bpm[:], in1=SGB[:, half * 4:(half + 1) * 4, :].rearrange("p k t -> p (k t)"), op=ALU.mult)))(half, bpm), [bpmB, sgB, sgB2, sgB3], [TB[5]])
                    pool((lambda half: (lambda e: e.tensor_tensor(out=mrg[:, half * 4:(half + 1) * 4, :].rearrange("p k t -> p (k t)"), in0=T[3][:], in1=T[5][:], op=ALU.add)))(half), [TB[3], TB[5]], [mrgB])
                ob0, ob0B = banks[6], bankB[6]
                ob1, ob1B = banks[7], bankB[7]
                for half, ob, obb in ((0, ob0, ob0B), (1, ob1, ob1B)):
                    for k in range(8):
                        pe((lambda half, k, ob: (lambda e: e.matmul(ob[:], lhsT=mrg[:, k, :], rhs=Wo[:, k, half * 512:(half + 1) * 512], start=(k == 0), stop=(k == 7))))(half, k, ob), [mrgB, WB[1]], [obb])
                residual(slot, [ob0, ob1], [ob0B, ob1B])
                store_x(ti, slot, xdB[ti])

        if "A" in phases:
            phase_mixer(0, x_d)
        srcB = out_d if "A" in phases else x_d
        if "B" in phases:
            phase_dense(0, srcB)
        srcC = out_d if ("A" in phases or "B" in phases) else x_d
        if "C" in phases:
            phase_mixer(1, srcC)
        srcD = out_d if any(p in phases for p in "ABC") else x_d
        if "D" in phases:
            if SPARSE_MOE:
                phase_moe_sparse(1, srcD)
            else:
                phase_moe(1, srcD)
        P.final_wait("sp", xdB)
        P.final_wait("pool", dbgB)
        P.emit()
    return nc, P


def _consts():
    cst = np.zeros((128, 1024), np.float32)
    cst[:, 0:128] = np.eye(128, dtype=np.float32)
    s = np.arange(128)[:, None]
    t = np.arange(128)[None, :]
    cst[:, 128:256] = (s <= t)
    cst[:, 256:384] = ((s // 64) == (t // 64))
    cst[:, 384:512] = (s <= t)
    cst[:, 512:576] = ((s % 64) <= np.arange(64)[None, :])
    cst[:, 576:704] = (s < t)
    rst = np.ones((128, 512), np.float32)
    rst[:, 0::64] = 0.0
    return cst, rst


def _fm(v, k):
    return np.ascontiguousarray(np.asarray(v, np.float32).reshape(k, 128).T)


def _pack_pv(inp):
    pv = np.zeros((128, DEPTH * PVL), np.float32)
    for l in range(DEPTH):
        o = l * PVL
        pv[:, o + PV_NMG:o + PV_NMG + 8] = _fm(inp["norm_mix_g"][l], 8)
        pv[:, o + PV_AMB:o + PV_AMB + 24] = _fm(inp["ada_mix_b"][l], 24)
        pv[:, o + PV_NFG:o + PV_NFG + 8] = _fm(inp["norm_ffn_g"][l], 8)
        pv[:, o + PV_AFB:o + PV_AFB + 24] = _fm(inp["ada_ffn_b"][l], 24)
        pv[:, o + PV_LB:o + PV_LB + 4] = _fm(inp["hg_lb_logits"][l], 4)
        pv[:, o + PV_HNG:o + PV_HNG + 4] = _fm(inp["hg_norm_g"][l], 4)
        cw = np.asarray(inp["ml_conv_w"][l], np.float32)
        for j in range(4):
            pv[:, o + PV_CW + j * 4:o + PV_CW + j * 4 + 4] = _fm(cw[j], 4)
        pv[:, o + PV_CB:o + PV_CB + 4] = _fm(inp["ml_conv_b"][l], 4)
        pv[:, o + PV_MNG:o + PV_MNG + 4] = _fm(inp["ml_norm_g"][l], 4)
        pv[:, o + PV_FB:o + PV_FB + 4] = np.broadcast_to(np.asarray(inp["ml_fbias"][l], np.float32)[None, :], (128, 4))
    pv[:, PVL + PV_RB:PVL + PV_RB + NE] = np.broadcast_to(np.asarray(inp["moe_router_b"][0], np.float32)[None, :], (128, NE))
    return pv


_CACHE = {}
LAST_CNT = None


def run(inputs, n_cores, NSEQ, S, phases="ABCD", debug=False):
    key = (NSEQ, S, phases, debug)
    if key not in _CACHE:
        _CACHE[key] = build(NSEQ, S, phases, debug)[0]
    nc = _CACHE[key]
    f32 = lambda a: np.ascontiguousarray(np.asarray(a, np.float32))
    x = f32(inputs["x"]); c = f32(inputs["c"])
    cst, rst = _consts()
    pv = _pack_pv(inputs)
    gbias = np.stack([np.broadcast_to(f32(inputs[nm])[l][2 * D:3 * D][None, :], (128, D))
                      for l in range(DEPTH) for nm in ("ada_mix_b", "ada_ffn_b")]).astype(np.float32)
    fng = np.ascontiguousarray(np.broadcast_to(f32(inputs["final_norm_g"])[None, :], (128, D)))
    shared = {"pv": pv, "cst": cst, "rst": rst, "gbias": np.ascontiguousarray(gbias), "fng": fng}
    for nm in ("ada_mix_w", "ada_ffn_w", "w_in", "ml_wq", "ml_wk", "w_br_hg", "w_br_ml", "w_out", "ffn_w1", "ffn_w3", "ffn_w2",
               "moe_router_w", "moe_w1", "moe_w3", "moe_w2"):
        shared[nm] = f32(inputs[nm])
    in_maps = []
    for i in range(n_cores):
        xs = x[i * NSEQ:(i + 1) * NSEQ].reshape(NSEQ * S, D)
        cs = c[i * NSEQ:(i + 1) * NSEQ]
        cTl = np.ascontiguousarray(cs.reshape(NSEQ, 8, 128).transpose(2, 1, 0))
        m = dict(shared)
        m["x"] = np.ascontiguousarray(xs)
        m["cT"] = cTl
        in_maps.append(m)
    res = run_bass_kernel_spmd(nc, in_maps, core_ids=list(range(n_cores)))
    outs = [r["out"].reshape(NSEQ, S, D) for r in res.results]
    global LAST_CNT
    LAST_CNT = [r["cnt"][0] for r in res.results]
    if debug:
        return np.concatenate(outs, axis=0), res.results[0]
    return np.concatenate(outs, axis=0)


def kernel(**inputs):
    B, S, _ = inputs["x"].shape
    return run(inputs, N_CORES, B // N_CORES, S).astype(np.float32)
```
